# Optimizing a Trainium2 kernel written in Bass

```python
import math
import jax, jax.numpy as jnp
from jax import lax
import numpy as np

D_MODEL = 2048
BATCH = 4
SEQ = 2048
DEPTH = 4

CHUNK = 64
Q_BLOCK = 128
N_MIXERS = 2
ROPE_THETA = 500000.0
RMS_EPS = 1e-6
N_MOD = 6

DIFF_HEAD_DIM = 128
DIFF_HEADS = D_MODEL // (2 * DIFF_HEAD_DIM)
DIFF_ROT = DIFF_HEAD_DIM // 4

MLA_V = 128
MLA_HEADS = D_MODEL // MLA_V
MLA_Q_RANK = 512
MLA_KV_RANK = 512
MLA_NOPE = 128
MLA_ROPE = 64

N_EXPERTS = 32
TOP_K = 4
D_FF_EXPERT = D_MODEL // 2
SWIGLU_LIMIT = 7.0
SWIGLU_ALPHA = 1.702
EXPERT_BLOCK = 128

N_DIFF_LAYERS = (DEPTH + 1) // 2
N_MLA_LAYERS = DEPTH // 2

kernel_name = "hybrid_diffattn_mla_moe_adaln_chunk_causal"


def _rms_norm(x, g):
    xf = x.astype(jnp.float32)
    y = xf * lax.rsqrt(jnp.mean(xf * xf, axis=-1, keepdims=True) + RMS_EPS)
    return (y * g.astype(jnp.float32)).astype(x.dtype)


def _rope_tables(positions, rot_dim):
    inv_freq = ROPE_THETA ** (-jnp.arange(0, rot_dim, 2, dtype=jnp.float32) / rot_dim)
    ang = positions.astype(jnp.float32)[..., None] * inv_freq
    return jnp.cos(ang), jnp.sin(ang)


def _apply_rope(x, cos, sin):
    half = x.shape[-1] // 2
    x1 = x[..., :half].astype(jnp.float32)
    x2 = x[..., half:].astype(jnp.float32)
    cs = cos[:, :, None, :]
    sn = sin[:, :, None, :]
    return jnp.concatenate([x1 * cs - x2 * sn, x1 * sn + x2 * cs], axis=-1).astype(x.dtype)


def _partial_rope(x, cos, sin, rot_dim):
    return jnp.concatenate([_apply_rope(x[..., :rot_dim], cos, sin), x[..., rot_dim:]], axis=-1)


def _chunk_mask(start, end):
    q_chunk = jnp.arange(start, end) // CHUNK
    k_chunk = jnp.arange(end) // CHUNK
    return k_chunk[None, :] <= q_chunk[:, None]


def _masked_softmax(scores, mask):
    s = jnp.where(mask, scores.astype(jnp.float32), -jnp.inf)
    return jax.nn.softmax(s, axis=-1)


def _sweep_query_blocks(block_fn, seq):
    outs = [block_fn(s0, min(s0 + Q_BLOCK, seq)) for s0 in range(0, seq, Q_BLOCK)]
    return jnp.concatenate(outs, axis=1)


def _diff_attention(u, w_in, lam, subln_g, w_out, cos, sin, lambda_init):
    B, S, _ = u.shape
    H, Dh = DIFF_HEADS, DIFF_HEAD_DIM
    q, k, v = jnp.split(u @ w_in, 3, axis=-1)
    q = _partial_rope(q.reshape(B, S, 2 * H, Dh), cos, sin, DIFF_ROT).reshape(B, S, H, 2, Dh)
    k = _partial_rope(k.reshape(B, S, 2 * H, Dh), cos, sin, DIFF_ROT).reshape(B, S, H, 2, Dh)
    v = v.reshape(B, S, H, 2 * Dh)
    q1, q2 = q[..., 0, :], q[..., 1, :]
    k1, k2 = k[..., 0, :], k[..., 1, :]
    lam = lam.astype(jnp.float32)
    lam_full = jnp.exp(jnp.sum(lam[0] * lam[1])) - jnp.exp(jnp.sum(lam[2] * lam[3])) + lambda_init
    scale = Dh ** -0.5

    def block(start, end):
        mask = _chunk_mask(start, end)
        p1 = _masked_softmax(jnp.einsum('bqhd,bkhd->bhqk', q1[:, start:end], k1[:, :end]) * scale, mask)
        p2 = _masked_softmax(jnp.einsum('bqhd,bkhd->bhqk', q2[:, start:end], k2[:, :end]) * scale, mask)
        p = (p1 - lam_full * p2).astype(v.dtype)
        return jnp.einsum('bhqk,bkhd->bqhd', p, v[:, :end])

    o = _sweep_query_blocks(block, S)
    o = _rms_norm(o, subln_g) * (1.0 - lambda_init)
    return o.reshape(B, S, H * 2 * Dh) @ w_out


def _mla(u, w_in, q_norm_g, kv_norm_g, w_uq, w_ukv, w_out, cos, sin):
    B, S, _ = u.shape
    H = MLA_HEADS
    lat = u @ w_in
    q_lat = lat[..., :MLA_Q_RANK]
    kv_lat = lat[..., MLA_Q_RANK:MLA_Q_RANK + MLA_KV_RANK]
    k_rope = lat[..., MLA_Q_RANK + MLA_KV_RANK:]
    q = (_rms_norm(q_lat, q_norm_g) @ w_uq).reshape(B, S, H, MLA_NOPE + MLA_ROPE)
    q_nope = q[..., :MLA_NOPE]
    q_rope = _apply_rope(q[..., MLA_NOPE:], cos, sin)
    kv = (_rms_norm(kv_lat, kv_norm_g) @ w_ukv).reshape(B, S, H, MLA_NOPE + MLA_V)
    k_nope = kv[..., :MLA_NOPE]
    v = kv[..., MLA_NOPE:]
    k_rope = _apply_rope(k_rope[:, :, None, :], cos, sin)[:, :, 0, :]
    scale = (MLA_NOPE + MLA_ROPE) ** -0.5

    def block(start, end):
        mask = _chunk_mask(start, end)
        s = (jnp.einsum('bqhd,bkhd->bhqk', q_nope[:, start:end], k_nope[:, :end])
             + jnp.einsum('bqhr,bkr->bhqk', q_rope[:, start:end], k_rope[:, :end]))
        p = _masked_softmax(s * scale, mask).astype(v.dtype)
        return jnp.einsum('bhqk,bkhd->bqhd', p, v[:, :end])

    o = _sweep_query_blocks(block, S)
    return o.reshape(B, S, H * MLA_V) @ w_out


def _moe(u, w_router, b_router, w_gu, b_gu, w_down, b_down):
    B, S, D = u.shape
    xf = u.reshape(-1, D)
    N = xf.shape[0]
    logits = (xf @ w_router).astype(jnp.float32) + b_router.astype(jnp.float32)
    top_val, top_idx = lax.top_k(logits, TOP_K)
    gate_w = jax.nn.softmax(top_val, axis=-1)
    A = N * TOP_K
    flat_e = top_idx.reshape(-1)
    flat_tok = jnp.repeat(jnp.arange(N, dtype=jnp.int32), TOP_K)
    flat_w = gate_w.reshape(-1)
    order = jnp.argsort(flat_e)
    sorted_e = flat_e[order]
    counts = jnp.bincount(flat_e, length=N_EXPERTS)
    padded = (counts + EXPERT_BLOCK - 1) // EXPERT_BLOCK * EXPERT_BLOCK
    group_start = jnp.cumsum(counts) - counts
    pad_end = jnp.cumsum(padded)
    pad_start = pad_end - padded
    dest = pad_start[sorted_e] + (jnp.arange(A, dtype=jnp.int32) - group_start[sorted_e])
    n_blocks = -(-(A + N_EXPERTS * (EXPERT_BLOCK - 1)) // EXPERT_BLOCK)
    P = n_blocks * EXPERT_BLOCK
    row_tok = jnp.zeros((P,), jnp.int32).at[dest].set(flat_tok[order])
    row_w = jnp.zeros((P,), jnp.float32).at[dest].set(flat_w[order])
    block_e = jnp.minimum(
        jnp.searchsorted(pad_end, jnp.arange(n_blocks, dtype=jnp.int32) * EXPERT_BLOCK, side='right'),
        N_EXPERTS - 1)
    xb = xf[row_tok].reshape(n_blocks, EXPERT_BLOCK, D)

    def expert_block(args):
        xblk, e = args
        gu = xblk @ w_gu[e] + b_gu[e]
        gate = jnp.minimum(gu[..., :D_FF_EXPERT], SWIGLU_LIMIT)
        up = jnp.clip(gu[..., D_FF_EXPERT:], -SWIGLU_LIMIT, SWIGLU_LIMIT)
        glu = gate * jax.nn.sigmoid(gate * SWIGLU_ALPHA)
        return ((up + 1.0) * glu) @ w_down[e] + b_down[e]

    yb = lax.map(expert_block, (xb, block_e)).reshape(P, D)
    y = jnp.zeros_like(xf).at[row_tok].add((yb * row_w[:, None].astype(yb.dtype)).astype(xf.dtype))
    return y.reshape(B, S, D)


def setup_inputs(seed: int = 0) -> dict:
    key = jax.random.key(seed)
    ks = jax.random.split(key, 32)
    f32 = jnp.float32
    D = D_MODEL
    ND, NM = N_DIFF_LAYERS, N_MLA_LAYERS
    qkv_w = 3 * 2 * DIFF_HEADS * DIFF_HEAD_DIM
    mla_in = MLA_Q_RANK + MLA_KV_RANK + MLA_ROPE

    def nrm(k, shape, scale):
        return jax.random.normal(k, shape, f32) * scale

    offset = jax.random.randint(ks[2], (BATCH, 1), 0, 64, dtype=jnp.int32) * CHUNK
    positions = offset + jnp.arange(SEQ, dtype=jnp.int32)[None, :]
    return {
        "x": nrm(ks[0], (BATCH, SEQ, D), 1.0),
        "c": nrm(ks[1], (BATCH, D), 1.0),
        "positions": positions,
        "ada_w": nrm(ks[3], (DEPTH, D, N_MOD * D), 0.5 * D ** -0.5),
        "ada_b": nrm(ks[4], (DEPTH, N_MOD * D), 0.01),
        "mix_norm_g": 1.0 + nrm(ks[5], (DEPTH, D), 0.01),
        "ffn_norm_g": 1.0 + nrm(ks[6], (DEPTH, D), 0.01),
        "final_norm_g": 1.0 + nrm(ks[7], (D,), 0.01),
        "diff_w_in": nrm(ks[8], (ND, D, qkv_w), D ** -0.5),
        "diff_lambda": nrm(ks[9], (ND, 4, DIFF_HEAD_DIM), 0.1),
        "diff_subln_g": 1.0 + nrm(ks[10], (ND, 2 * DIFF_HEAD_DIM), 0.01),
        "diff_w_out": nrm(ks[11], (ND, DIFF_HEADS * 2 * DIFF_HEAD_DIM, D), (DIFF_HEADS * 2 * DIFF_HEAD_DIM) ** -0.5),
        "mla_w_in": nrm(ks[12], (NM, D, mla_in), D ** -0.5),
        "mla_q_norm_g": 1.0 + nrm(ks[13], (NM, MLA_Q_RANK), 0.01),
        "mla_kv_norm_g": 1.0 + nrm(ks[14], (NM, MLA_KV_RANK), 0.01),
        "mla_w_uq": nrm(ks[15], (NM, MLA_Q_RANK, MLA_HEADS * (MLA_NOPE + MLA_ROPE)), MLA_Q_RANK ** -0.5),
        "mla_w_ukv": nrm(ks[16], (NM, MLA_KV_RANK, MLA_HEADS * (MLA_NOPE + MLA_V)), MLA_KV_RANK ** -0.5),
        "mla_w_out": nrm(ks[17], (NM, MLA_HEADS * MLA_V, D), (MLA_HEADS * MLA_V) ** -0.5),
        "moe_w_router": nrm(ks[18], (DEPTH, D, N_EXPERTS), D ** -0.5),
        "moe_b_router": nrm(ks[19], (DEPTH, N_EXPERTS), 0.01),
        "moe_w_gate_up": nrm(ks[20], (DEPTH, N_EXPERTS, D, 2 * D_FF_EXPERT), D ** -0.5),
        "moe_b_gate_up": nrm(ks[21], (DEPTH, N_EXPERTS, 2 * D_FF_EXPERT), 0.01),
        "moe_w_down": nrm(ks[22], (DEPTH, N_EXPERTS, D_FF_EXPERT, D), D_FF_EXPERT ** -0.5),
        "moe_b_down": nrm(ks[23], (DEPTH, N_EXPERTS, D), 0.01),
    }


def reference(x, c, positions, ada_w, ada_b, mix_norm_g, ffn_norm_g, final_norm_g,
              diff_w_in, diff_lambda, diff_subln_g, diff_w_out,
              mla_w_in, mla_q_norm_g, mla_kv_norm_g, mla_w_uq, mla_w_ukv, mla_w_out,
              moe_w_router, moe_b_router, moe_w_gate_up, moe_b_gate_up, moe_w_down, moe_b_down):
    cos_d, sin_d = _rope_tables(positions, DIFF_ROT)
    cos_m, sin_m = _rope_tables(positions, MLA_ROPE)
    c_act = jax.nn.silu(c)
    h = x
    for i in range(DEPTH):
        mod = c_act @ ada_w[i] + ada_b[i]
        shift_a, scale_a, gate_a, shift_f, scale_f, gate_f = [m[:, None, :] for m in jnp.split(mod, N_MOD, axis=-1)]
        u = _rms_norm(h, mix_norm_g[i]) * (1.0 + scale_a) + shift_a
        j = i // N_MIXERS
        if i % N_MIXERS == 0:
            lambda_init = 0.8 - 0.6 * math.exp(-0.3 * i)
            y = _diff_attention(u, diff_w_in[j], diff_lambda[j], diff_subln_g[j], diff_w_out[j],
                                cos_d, sin_d, lambda_init)
        else:
            y = _mla(u, mla_w_in[j], mla_q_norm_g[j], mla_kv_norm_g[j], mla_w_uq[j], mla_w_ukv[j],
                     mla_w_out[j], cos_m, sin_m)
        h = h + gate_a * y
        u = _rms_norm(h, ffn_norm_g[i]) * (1.0 + scale_f) + shift_f
        h = h + gate_f * _moe(u, moe_w_router[i], moe_b_router[i], moe_w_gate_up[i], moe_b_gate_up[i],
                              moe_w_down[i], moe_b_down[i])
    return _rms_norm(h, final_norm_g)
```

```python
import math
from contextlib import ExitStack

import numpy as np
import ml_dtypes
import concourse.bass as bass
import concourse.mybir as mybir
from concourse.bass_utils import run_bass_kernel_spmd

F32 = mybir.dt.float32
BF16 = mybir.dt.bfloat16
I32 = mybir.dt.int32
AF = mybir.ActivationFunctionType
ALU = mybir.AluOpType
AX = mybir.AxisListType

D = 2048
T = 1024
KC = 16
EPS = 1e-6
NE = 32
ENGS = ["pe", "act", "dve", "pool", "sp"]
NSLOT = 8
EPOCH = 12000
NEPOCH = 24
NTF = 10
NTB = 8
TWO_PI = 2.0 * math.pi


class Prog:
    def __init__(self, nc):
        self.nc = nc
        self.ops = []

    def add(self, eng, fn, reads=(), writes=(), dma=False):
        self.ops.append(dict(eng=eng, fn=fn, reads=tuple(reads), writes=tuple(writes), dma=dma, extra=set()))

    def pe(self, fn, reads=(), writes=()):
        self.add("pe", fn, reads, writes)

    def act(self, fn, reads=(), writes=()):
        self.add("act", fn, reads, writes)

    def dve(self, fn, reads=(), writes=()):
        self.add("dve", fn, reads, writes)

    def pool(self, fn, reads=(), writes=()):
        self.add("pool", fn, reads, writes)

    def dma(self, fn, reads=(), writes=(), q="sp"):
        self.add(q, fn, reads, writes, dma=True)

    def barrier(self):
        self.ops.append(dict(bar=True))

    def analyze(self):
        last_w = {}
        readers = {}
        ops = [o for o in self.ops]
        real = []
        pending_bar = None
        last_comp = {}
        last_dmas = {e: [] for e in ENGS}
        seen_after_bar = set()
        for o in ops:
            if o.get("bar"):
                pending_bar = set()
                for e in ENGS:
                    if e in last_comp:
                        pending_bar.add(last_comp[e])
                    pending_bar.update(last_dmas[e][-NSLOT:])
                seen_after_bar = set()
                continue
            i = len(real)
            real.append(o)
            deps = set()
            for k in o["reads"]:
                if k in last_w:
                    deps.add(last_w[k])
            for k in o["writes"]:
                if k in last_w:
                    deps.add(last_w[k])
                rd = readers.get(k)
                if rd:
                    deps.update(rd[0].values())
                    deps.update(rd[1])
            deps.discard(i)
            if o["eng"] == "pe":
                deps = {d for d in deps if real[d]["eng"] != "pe" or real[d]["dma"]}
            if pending_bar is not None and o["eng"] not in seen_after_bar:
                deps.update(pending_bar)
                seen_after_bar.add(o["eng"])
            o["deps"] = deps
            for k in o["writes"]:
                last_w[k] = i
                readers[k] = ({}, [])
            for k in o["reads"]:
                rd = readers.setdefault(k, ({}, []))
                if o["dma"]:
                    rd[1].append(i)
                else:
                    rd[0][o["eng"]] = i
            if o["dma"]:
                last_dmas[o["eng"]].append(i)
            else:
                last_comp[o["eng"]] = i
        self.real = real
        for o in real:
            o["sig"] = False
        for o in real:
            for d in o["deps"]:
                real[d]["sig"] = True
        cnt = {e: 0 for e in ENGS}
        dcnt = {e: 0 for e in ENGS}
        for o in real:
            e = o["eng"]
            if o["dma"]:
                j = dcnt[e]
                dcnt[e] += 1
                o["slot"] = j % NSLOT
                o["tok"] = ("d", e, j % NSLOT, 16 * (j // NSLOT + 1))
                o["prev"] = 16 * (j // NSLOT)
            elif o["sig"]:
                c = cnt[e]
                cnt[e] += 1
                o["tok"] = ("c", e, c // EPOCH, c % EPOCH + 1)
        self.cnt = cnt
        self.dcnt = dcnt
        for e in ENGS:
            assert cnt[e] < EPOCH * NEPOCH, (e, cnt[e])

    def emit(self, block, sems, dsems):
        nc = self.nc
        ops = self.real
        engobj = {"pe": nc.tensor, "act": nc.scalar, "dve": nc.vector, "pool": nc.gpsimd, "sp": nc.sync}

        def semof(key):
            kind, e, idx = key
            return sems[e][idx] if kind == "c" else dsems[e][idx]

        def run_engine(ename):
            eng = engobj[ename]
            waited = {}
            last_dma_tok = {}
            for op in ops:
                if op["eng"] != ename:
                    continue
                need = {}
                for d in op["deps"]:
                    tok = ops[d]["tok"]
                    key = tok[:3]
                    need[key] = max(need.get(key, 0), tok[3])
                if op["dma"] and op["prev"] > 0:
                    key = ("d", ename, op["slot"])
                    need[key] = max(need.get(key, 0), op["prev"])
                for key, val in need.items():
                    if waited.get(key, 0) >= val:
                        continue
                    eng.wait_ge(semof(key), val)
                    waited[key] = val
                ins = op["fn"](eng)
                if op["dma"]:
                    ins.then_inc(semof(op["tok"][:3]), 16)
                    last_dma_tok[op["slot"]] = op["tok"]
                elif op["sig"]:
                    ins.then_inc(semof(op["tok"][:3]), 1)
            for slot, tok in last_dma_tok.items():
                eng.wait_ge(semof(tok[:3]), tok[3])

        @block.tensor
        def _(e):
            run_engine("pe")

        @block.scalar
        def _(e):
            run_engine("act")

        @block.vector
        def _(e):
            run_engine("dve")

        @block.gpsimd
        def _(e):
            run_engine("pool")

        @block.sync
        def _(e):
            run_engine("sp")


class Bld:
    def __init__(self, nc, es):
        self.nc = nc
        self.P = Prog(nc)
        sb = lambda name, shape, dt: es.enter_context(nc.sbuf_tensor("sb_" + name, shape, dt))
        self.hT = sb("hT", [128, KC, T], F32)
        self.actT = sb("actT", [128, KC, T], BF16)
        self.wst = [sb(f"wst{i}", [128, 4096], F32) for i in range(2)]
        self.wbf = [sb(f"wbf{i}", [128, 4096], BF16) for i in range(2)]
        self.arena = sb("arena", [128, 10240], BF16)
        self.tmpf = [sb(f"tf{i}", [128, 512], F32) for i in range(NTF)]
        self.tmpb = [sb(f"tb{i}", [128, 512], BF16) for i in range(NTB)]
        self.auxf = sb("auxf", [128, 2 * T], F32)
        self.cosT = self.auxf[:, 0:T]
        self.sinT = self.auxf[:, T:2 * T]
        self.gb = [self.auxf[:, 0:T], self.auxf[:, T:2 * T]]
        self.rstd = [sb(f"rstd{i}", [128, 512], F32) for i in range(2)]
        self.rsi = 0
        self.modT = sb("modT", [128, 96], F32)
        self.small = sb("small", [128, 256], F32)
        self.small_b = sb("small_b", [128, 64], BF16)
        self.ones_bf = sb("ones_bf", [128, 128], BF16)
        self.ones_f = sb("ones_f", [128, 128], F32)
        self.rotT = sb("rotT", [64, 96], BF16)
        self.invf = sb("invf", [64, 2], F32)
        self.ident = sb("ident", [128, 128], F32)
        self.pidx = sb("pidx", [32, 128], F32)
        self.bgu = sb("bgu", [128, NE * 16], F32)
        self.wr = sb("wr", [128, KC * NE], F32)
        self.brr = sb("brr", [128, NE], F32)
        self.GT = sb("GT", [32, T], F32)
        self.sel = sb("sel", [32, 128], F32)
        self.rt = sb("rt", [128, 64], F32)
        self.ps = [es.enter_context(nc.psum_tensor(f"ps{i}", [128, 512], F32)) for i in range(8)]
        self.tfi = 0
        self.tbi = 0
        self.wslot = 0
        self.mmb = 0
        self.uid = 0

    def tf(self):
        i = self.tfi
        self.tfi = (i + 1) % NTF
        return self.tmpf[i], ("tf", i)

    def rs_tile(self):
        i = self.rsi
        self.rsi ^= 1
        return self.rstd[i], ("rstd", i)

    def tb(self):
        i = self.tbi
        self.tbi = (i + 1) % NTB
        return self.tmpb[i], ("tb", i)

    def mmbank(self, lo=0, n=4):
        b = lo + self.mmb % n
        self.mmb += 1
        return b

    def load(self, dst, src, wkeys, rkeys=(), q="sp"):
        self.P.dma(lambda e, dst=dst, src=src: e.dma_start(out=dst, in_=src), reads=rkeys, writes=wkeys, q=q)

    def wload(self, regions, n_kc):
        P = self.P
        slot = self.wslot
        self.wslot ^= 1
        off = 0
        views = []
        for ri, r in enumerate(regions):
            w = r.shape[-1]
            n = n_kc * w
            dst = self.wst[slot][:, off:off + n].rearrange("p (k w) -> p k w", w=w)
            P.dma(lambda e, dst=dst, r=r: e.dma_start(out=dst, in_=r), writes=[("wst", slot, ri)])
            views.append(self.wbf[slot][:, off:off + n].rearrange("p (k w) -> p k w", w=w))
            off += n
        assert off <= 4096
        P.act(lambda e, slot=slot, off=off: e.copy(out=self.wbf[slot][:, :off], in_=self.wst[slot][:, :off]),
              reads=[("wst", slot, i) for i in range(4)], writes=[("wbf", slot)])
        return views, ("wbf", slot)

    def linear_fm(self, Wv, n_kc, chunks, rhs_fn, n_tiles, evac, banks=(0, 4)):
        P = self.P
        maxcols = 4096 // n_kc
        i = 0
        while i < len(chunks):
            c0 = chunks[i][0]
            j = i
            while j + 1 < len(chunks) and chunks[j + 1][0] + chunks[j + 1][1] - c0 <= maxcols \
                    and chunks[j + 1][0] == chunks[j][0] + chunks[j][1]:
                j += 1
            c1 = chunks[j][0] + chunks[j][1]
            (wv,), wk = self.wload([Wv[:, :, c0:c1]], n_kc)
            for ci in range(i, j + 1):
                col0, width = chunks[ci]
                o = col0 - c0
                for tt in range(n_tiles):
                    b = self.mmbank(*banks)
                    ncol = None
                    for kc in range(n_kc):
                        rhs, rk = rhs_fn(kc, tt)
                        ncol = rhs.shape[-1]
                        P.pe(lambda e, b=b, kc=kc, o=o, width=width, rhs=rhs, ncol=ncol, wv=wv:
                             e.matmul(self.ps[b][:width, :ncol], lhsT=wv[:rhs.shape[0], kc, o:o + width], rhs=rhs,
                                      start=(kc == 0), stop=(kc == n_kc - 1)),
                             reads=[wk] + list(rk), writes=[("ps", b)])
                    evac(ci, tt, self.ps[b][:width, :ncol], ("ps", b))
            i = j + 1

    def consts(self, dr):
        P = self.P
        P.dve(lambda e: e.memset(self.ones_bf[:], 1.0), writes=[("ones",)])
        P.dve(lambda e: e.memset(self.ones_f[:], 1.0), writes=[("onesf",)])
        self.load(self.rotT[:], dr["rotT"], [("rotT",)])
        self.load(self.invf[:], dr["invf"], [("invf",)])
        self.load(self.ident[:], dr["ident"], [("ident",)])
        self.load(self.pidx[:], dr["pidx"], [("pidx",)])

    def load_hT(self, src):
        v = src.rearrange("(kc p) t -> p kc t", p=128)
        for kc in range(KC):
            self.load(self.hT[:, kc, :], v[:, kc, :], [("hT", kc, 0), ("hT", kc, 1)])

    def store_hT(self, dst):
        v = dst.rearrange("(kc p) t -> p kc t", p=128)
        for kc in range(KC):
            self.load(v[:, kc, :], self.hT[:, kc, :], [("dram", "h", id(dst), kc)], rkeys=[("hT", kc, 0), ("hT", kc, 1)], q="pool")

    def norm_adaln(self, Acol, Bcol, abkeys, want32=None):
        P = self.P
        for tt in range(2):
            cs = slice(tt * 512, (tt + 1) * 512)
            sb_ = 6 + tt
            for kc in range(KC):
                sq, sqk = self.tf()
                P.act(lambda e, sq=sq, kc=kc, cs=cs: e.activation(out=sq[:], in_=self.hT[:, kc, cs], func=AF.Square),
                      reads=[("hT", kc, tt)], writes=[sqk])
                P.pe(lambda e, sq=sq, kc=kc, sb_=sb_: e.matmul(self.ps[sb_][:], lhsT=self.ones_f[:], rhs=sq[:],
                                                              start=(kc == 0), stop=(kc == KC - 1)),
                     reads=[sqk, ("onesf",)], writes=[("ps", sb_)])
            rs, rsk = self.rs_tile()
            P.act(lambda e, rs=rs, sb_=sb_: e.activation(out=rs[:], in_=self.ps[sb_][:], func=AF.Sqrt, scale=1.0 / D, bias=self.small[:, 255:256]),
                  reads=[("ps", sb_), ("epscol",)], writes=[rsk])
            P.dve(lambda e, rs=rs: e.reciprocal(out=rs[:], in_=rs[:]), reads=[rsk], writes=[rsk])
            for kc in range(KC):
                t, tk = self.tf()
                P.dve(lambda e, t=t, kc=kc, cs=cs, rs=rs: e.tensor_tensor(out=t[:], in0=self.hT[:, kc, cs], in1=rs[:], op=ALU.mult),
                      reads=[("hT", kc, tt), rsk], writes=[tk])
                if want32 is None:
                    P.pool(lambda e, t=t, kc=kc, cs=cs: e.tensor_scalar(out=self.actT[:, kc, cs], in0=t[:], scalar1=Acol[:, kc:kc + 1],
                                                                        scalar2=Bcol[:, kc:kc + 1], op0=ALU.mult, op1=ALU.add),
                           reads=[tk] + list(abkeys), writes=[("actT", kc, tt)])
                else:
                    u32, uk = self.tf()
                    P.pool(lambda e, t=t, kc=kc, u32=u32: e.tensor_scalar(out=u32[:], in0=t[:], scalar1=Acol[:, kc:kc + 1],
                                                                          scalar2=Bcol[:, kc:kc + 1], op0=ALU.mult, op1=ALU.add),
                           reads=[tk] + list(abkeys), writes=[uk])
                    P.act(lambda e, u32=u32, kc=kc, cs=cs: e.copy(out=self.actT[:, kc, cs], in_=u32[:]), reads=[uk], writes=[("actT", kc, tt)])
                    want32(kc, tt, u32, uk)

    def eps_col(self):
        self.P.dve(lambda e: e.memset(self.small[:, 255:256], EPS), writes=[("epscol",)])

    def rhs_act(self, kc, tt):
        return self.actT[:, kc, tt * 512:(tt + 1) * 512], [("actT", kc, tt)]

    def compute_mod(self, dr):
        P = self.P
        ccol = self.small[:, 0:16]
        cs_ = self.small[:, 16:32]
        self.load(ccol, dr["ccol"], [("ccol",)])
        P.act(lambda e: e.activation(out=cs_, in_=ccol, func=AF.Silu), reads=[("ccol",)], writes=[("csil",)])
        cb = self.small_b[:, 0:16]
        P.dve(lambda e: e.tensor_copy(out=cb, in_=cs_), reads=[("csil",)], writes=[("cbf",)])
        adab = self.small[:, 32:128]
        self.load(adab, dr["adabT"], [("adab",)])
        Wv = dr["ada_w"].rearrange("(kc p) n -> p kc n", p=128)

        def rhs(kc, tt):
            return cb[:, kc:kc + 1], [("cbf",)]

        def evac(ci, tt, ps, pk):
            P.dve(lambda e, ci=ci, ps=ps: e.tensor_tensor(out=self.modT[:, ci:ci + 1], in0=ps, in1=adab[:, ci:ci + 1], op=ALU.add),
                  reads=[pk, ("adab",)], writes=[("modT", ci)])

        self.linear_fm(Wv, KC, [(j * 128, 128) for j in range(96)], rhs, 1, evac)

    def adaln_cols(self, gT_dram, which):
        P = self.P
        base = 0 if which == 0 else 48
        g = self.small[:, 128:144]
        A = self.small[:, 144:160]
        self.load(g, gT_dram, [("gcol",)])
        P.dve(lambda e: e.tensor_scalar(out=A, in0=self.modT[:, base + 16:base + 32], scalar1=1.0, scalar2=None, op0=ALU.add),
              reads=[("modT", j) for j in range(base + 16, base + 32)], writes=[("Acol",)])
        P.dve(lambda e: e.tensor_tensor(out=A, in0=A, in1=g, op=ALU.mult), reads=[("Acol",), ("gcol",)], writes=[("Acol",)])
        return A, self.modT[:, base:base + 16], [("Acol",)] + [("modT", j) for j in range(base, base + 16)]

    def rope_tables(self, dr, col, R):
        P = self.P
        for tt in range(2):
            cs = slice(tt * 512, (tt + 1) * 512)
            pi_, pik = self.tf()
            pint = pi_.bitcast(I32)
            self.load(pint[:64, :], dr["pos"][:, cs], [pik])
            for which, shift, dst in ((0, 0.0, self.sinT), (1, math.pi / 2, self.cosT)):
                a, ak = self.tf()
                P.dve(lambda e, a=a, pint=pint: e.tensor_copy(out=a[:R, :], in_=pint[:R, :]), reads=[pik], writes=[ak])
                P.dve(lambda e, a=a, shift=shift: e.tensor_scalar(out=a[:R, :], in0=a[:R, :], scalar1=self.invf[:R, col:col + 1], scalar2=shift,
                                                                  op0=ALU.mult, op1=ALU.add), reads=[ak, ("invf",)], writes=[ak])
                ki, kk = self.tf()
                P.dve(lambda e, a=a, ki=ki: e.tensor_scalar(out=ki[:R, :], in0=a[:R, :], scalar1=1.0 / TWO_PI, scalar2=None, op0=ALU.mult),
                      reads=[ak], writes=[kk])
                k2, k2k = self.tf()
                k2i = k2.bitcast(I32)
                P.dve(lambda e, ki=ki, k2i=k2i: e.tensor_copy(out=k2i[:R, :], in_=ki[:R, :]), reads=[kk], writes=[k2k])
                P.dve(lambda e, ki=ki, k2i=k2i: e.tensor_copy(out=ki[:R, :], in_=k2i[:R, :]), reads=[k2k], writes=[kk])
                P.dve(lambda e, a=a, ki=ki: e.scalar_tensor_tensor(out=a[:R, :], in0=ki[:R, :], scalar=-TWO_PI, in1=a[:R, :], op0=ALU.mult, op1=ALU.add),
                      reads=[ak, kk], writes=[ak])
                P.dve(lambda e, a=a, ki=ki: e.tensor_scalar(out=ki[:R, :], in0=a[:R, :], scalar1=math.pi, scalar2=-TWO_PI, op0=ALU.is_gt, op1=ALU.mult),
                      reads=[ak], writes=[kk])
                P.dve(lambda e, a=a, ki=ki: e.tensor_tensor(out=a[:R, :], in0=a[:R, :], in1=ki[:R, :], op=ALU.add), reads=[ak, kk], writes=[ak])
                P.dve(lambda e, a=a, ki=ki: e.tensor_scalar(out=ki[:R, :], in0=a[:R, :], scalar1=-math.pi, scalar2=TWO_PI, op0=ALU.is_lt, op1=ALU.mult),
                      reads=[ak], writes=[kk])
                P.dve(lambda e, a=a, ki=ki: e.tensor_tensor(out=a[:R, :], in0=a[:R, :], in1=ki[:R, :], op=ALU.add), reads=[ak, kk], writes=[ak])
                P.dve(lambda e, a=a: e.tensor_scalar(out=a[:R, :], in0=a[:R, :], scalar1=math.pi, scalar2=-math.pi, op0=ALU.min, op1=ALU.max),
                      reads=[ak], writes=[ak])
                P.act(lambda e, a=a, dst=dst, cs=cs: e.activation(out=dst[:R, cs], in_=a[:R, :], func=AF.Sin), reads=[ak],
                      writes=[("trig", which, tt)])

    def rope_apply(self, xb, xk, R, rot_lo, tt):
        P = self.P
        cs = slice(tt * 512, (tt + 1) * 512)
        b = 4 + (self.mmb % 2)
        self.mmb += 1
        P.pe(lambda e, xb=xb, b=b: e.matmul(self.ps[b][:R, :], lhsT=self.rotT[:R, rot_lo:rot_lo + R], rhs=xb[:R, :], start=True, stop=True),
             reads=[xk, ("rotT",)], writes=[("ps", b)])
        t1, t1k = self.tf()
        t2, t2k = self.tf()
        P.dve(lambda e, xb=xb, t1=t1, cs=cs: e.tensor_tensor(out=t1[:R, :], in0=xb[:R, :], in1=self.cosT[:R, cs], op=ALU.mult),
              reads=[xk, ("trig", 1, tt)], writes=[t1k])
        P.dve(lambda e, t2=t2, b=b, cs=cs: e.tensor_tensor(out=t2[:R, :], in0=self.ps[b][:R, :], in1=self.sinT[:R, cs], op=ALU.mult),
              reads=[("ps", b), ("trig", 0, tt)], writes=[t2k])
        P.pool(lambda e, xb=xb, t1=t1, t2=t2: e.tensor_tensor(out=xb[:R, :], in0=t1[:R, :], in1=t2[:R, :], op=ALU.add),
               reads=[t1k, t2k], writes=[xk])

    def phaseA_diff(self, dr):
        P = self.P
        self.eps_col()
        self.load_hT(dr["hT_in"])
        self.rope_tables(dr, 0, 32)
        if dr.get("ada_w") is not None:
            self.compute_mod(dr)
        if dr.get("modT_out") is not None:
            self.load(dr["modT_out"], self.modT[:], [("dram", "modT")], rkeys=[("modT", j) for j in range(96)], q="pool")
        A, Bc, abk = self.adaln_cols(dr["gmixT"], 0)
        self.norm_adaln(A, Bc, abk)
        Wv = dr["w_in"].rearrange("(kc p) n -> p kc n", p=128)
        qT = dr["qT"]
        kT = dr["kT"]

        def evac(ci, tt, ps, pk):
            xb, xk = self.tb()
            P.act(lambda e, xb=xb, ps=ps: e.copy(out=xb[:], in_=ps), reads=[pk], writes=[xk])
            self.rope_apply(xb, xk, 32, 0, tt)
            dst = (qT if ci < 16 else kT)[ci % 16, :, tt * 512:(tt + 1) * 512]
            self.load(dst, xb[:], [("dram", "qk", ci, tt)], rkeys=[xk], q="pool")

        self.linear_fm(Wv, KC, [(j * 128, 128) for j in range(32)], evac=evac, rhs_fn=self.rhs_act, n_tiles=2)
        self.v_token_major(Wv, 4096, dr["V"], KC, lambda kc, tb_: (self.actT[:, kc, tb_ * 128:(tb_ + 1) * 128], [("actT", kc, tb_ // 4)]))

    def v_token_major(self, Wv, col_base, Vd, n_kc, lhs_fn, ncols=2048, col_list=None):
        P = self.P
        maxcols = 4096 // n_kc
        if col_list is None:
            col_list = [(col_base + g * 256, 256, g * 256) for g in range(ncols // 256)]
        for (c0, w, d0) in col_list:
            (wv,), wk = self.wload([Wv[:, :, c0:c0 + w]], n_kc)
            for tb_ in range(8):
                b = self.mmbank(0, 4)
                for kc in range(n_kc):
                    lhs, lk = lhs_fn(kc, tb_)
                    P.pe(lambda e, b=b, kc=kc, lhs=lhs, wv=wv, w=w: e.matmul(self.ps[b][:, :w], lhsT=lhs, rhs=wv[:, kc, :],
                                                                            start=(kc == 0), stop=(kc == n_kc - 1)),
                         reads=[wk] + list(lk), writes=[("ps", b)])
                vt, vk = self.tb()
                P.dve(lambda e, vt=vt, b=b, w=w: e.tensor_copy(out=vt[:, :w], in_=self.ps[b][:, :w]), reads=[("ps", b)], writes=[vk])
                self.load(Vd[tb_ * 128:(tb_ + 1) * 128, d0:d0 + w], vt[:, :w], [("dram", "V", d0, tb_)], rkeys=[vk], q="pool")

    def phaseA_mla(self, dr):
        P = self.P
        self.eps_col()
        self.load_hT(dr["hT_in"])
        self.rope_tables(dr, 1, 64)
        if dr.get("ada_w") is not None:
            self.compute_mod(dr)
        if dr.get("modT_out") is not None:
            self.load(dr["modT_out"], self.modT[:], [("dram", "modT")], rkeys=[("modT", j) for j in range(96)], q="pool")
        A, Bc, abk = self.adaln_cols(dr["gmixT"], 0)
        self.norm_adaln(A, Bc, abk)
        Wv = dr["w_in"].rearrange("(kc p) n -> p kc n", p=128)
        krT = dr["krT"]
        qg = self.small[:, 160:164]
        kvg = self.small[:, 164:168]
        self.load(qg, dr["qgT"], [("qg",)])
        self.load(kvg, dr["kvgT"], [("kvg",)])
        latf = self.hT

        def evac_lat(ci, tt, ps, pk):
            cs = slice(tt * 512, (tt + 1) * 512)
            if ci < 8:
                P.dve(lambda e, ci=ci, cs=cs, ps=ps: e.tensor_copy(out=latf[:, ci, cs], in_=ps), reads=[pk], writes=[("hT", ci, tt)])
            else:
                xb, xk = self.tb()
                P.act(lambda e, xb=xb, ps=ps: e.copy(out=xb[:64, :], in_=ps), reads=[pk], writes=[xk])
                self.rope_apply(xb, xk, 64, 32, tt)
                self.load(krT[:, cs], xb[:64, :], [("dram", "kr", tt)], rkeys=[xk], q="pool")

        self.linear_fm(Wv, KC, [(j * 128, 128) for j in range(8)] + [(1024, 64)], evac=evac_lat, rhs_fn=self.rhs_act, n_tiles=2)
        latn = self.arena[:, 0:8192].rearrange("p (c t) -> p c t", t=T)
        for grp, gcol, gk in ((0, qg, ("qg",)), (1, kvg, ("kvg",))):
            for tt in range(2):
                cs = slice(tt * 512, (tt + 1) * 512)
                sb_ = 6 + tt
                for c in range(4):
                    ci = grp * 4 + c
                    sq, sqk = self.tb()
                    P.act(lambda e, sq=sq, ci=ci, cs=cs: e.activation(out=sq[:], in_=latf[:, ci, cs], func=AF.Square), reads=[("hT", ci, tt)], writes=[sqk])
                    P.pe(lambda e, sq=sq, c=c, sb_=sb_: e.matmul(self.ps[sb_][:], lhsT=self.ones_bf[:], rhs=sq[:], start=(c == 0), stop=(c == 3)),
                         reads=[sqk, ("ones",)], writes=[("ps", sb_)])
                rs, rsk = self.rs_tile()
                P.act(lambda e, rs=rs, sb_=sb_: e.activation(out=rs[:], in_=self.ps[sb_][:], func=AF.Sqrt, scale=1.0 / 512, bias=self.small[:, 255:256]),
                      reads=[("ps", sb_), ("epscol",)], writes=[rsk])
                P.dve(lambda e, rs=rs: e.reciprocal(out=rs[:], in_=rs[:]), reads=[rsk], writes=[rsk])
                for c in range(4):
                    ci = grp * 4 + c
                    t, tk = self.tf()
                    P.dve(lambda e, t=t, ci=ci, cs=cs, rs=rs: e.tensor_tensor(out=t[:], in0=latf[:, ci, cs], in1=rs[:], op=ALU.mult),
                          reads=[("hT", ci, tt), rsk], writes=[tk])
                    P.pool(lambda e, t=t, ci=ci, c=c, cs=cs, gcol=gcol: e.tensor_scalar(out=latn[:, ci, cs], in0=t[:], scalar1=gcol[:, c:c + 1], scalar2=None, op0=ALU.mult),
                           reads=[tk, gk], writes=[("latn", ci, tt)])
        Wq = dr["w_uq"].rearrange("(kc p) n -> p kc n", p=128)
        chunks = []
        for h in range(16):
            chunks.append((h * 192, 128))
            chunks.append((h * 192 + 128, 64))
        qnT, qrT, knT = dr["qnT"], dr["qrT"], dr["knT"]

        def rhs_q(kc, tt):
            return latn[:, kc, tt * 512:(tt + 1) * 512], [("latn", kc, tt)]

        def evac_q(ci, tt, ps, pk):
            h, isr = ci // 2, ci % 2
            cs = slice(tt * 512, (tt + 1) * 512)
            xb, xk = self.tb()
            if not isr:
                P.act(lambda e, xb=xb, ps=ps: e.copy(out=xb[:], in_=ps), reads=[pk], writes=[xk])
                self.load(qnT[h, :, cs], xb[:], [("dram", "qn", h, tt)], rkeys=[xk], q="pool")
            else:
                P.act(lambda e, xb=xb, ps=ps: e.copy(out=xb[:64, :], in_=ps), reads=[pk], writes=[xk])
                self.rope_apply(xb, xk, 64, 32, tt)
                self.load(qrT[h, :, cs], xb[:64, :], [("dram", "qr", h, tt)], rkeys=[xk], q="pool")

        self.linear_fm(Wq, 4, chunks, evac=evac_q, rhs_fn=rhs_q, n_tiles=2)
        Wkv = dr["w_ukv"].rearrange("(kc p) n -> p kc n", p=128)

        def rhs_kv(kc, tt):
            return latn[:, 4 + kc, tt * 512:(tt + 1) * 512], [("latn", 4 + kc, tt)]

        def evac_k(ci, tt, ps, pk):
            cs = slice(tt * 512, (tt + 1) * 512)
            xb, xk = self.tb()
            P.act(lambda e, xb=xb, ps=ps: e.copy(out=xb[:], in_=ps), reads=[pk], writes=[xk])
            self.load(knT[ci, :, cs], xb[:], [("dram", "kn", ci, tt)], rkeys=[xk], q="pool")

        self.linear_fm(Wkv, 4, [(h * 256, 128) for h in range(16)], evac=evac_k, rhs_fn=rhs_kv, n_tiles=2)
        self.v_token_major(Wkv, 0, dr["V"], 4,
                           lambda kc, tb_: (latn[:, 4 + kc, tb_ * 128:(tb_ + 1) * 128], [("latn", 4 + kc, tb_ // 4)]),
                           col_list=[(h * 256 + 128, 128, h * 128) for h in range(16)])

    def kc_list(self, qt, prev):
        out = []
        if prev:
            for kc in range(8):
                out.append((kc, 0, True, None))
        for oi in range(4 * qt + 4):
            r = oi - 4 * qt
            if r < 0:
                out.append((8 + oi, 0, False, None))
            else:
                out.append((8 + oi, 128 * r, False, r))
        return out

    def attn_scores_pv(self, qt, prev, bias_ap, bias_k, scale, s_mm, pv_list, den_bank):
        P = self.P
        cq = qt * 512
        lst = self.kc_list(qt, prev)
        for idx, (kc, col0, isp, r) in enumerate(lst):
            sbk = self.mmbank(0, 2)
            s_mm(kc, cq + col0, col0, sbk)
            pT, pk = self.tb()
            if isp and bias_ap is not None:
                P.act(lambda e, pT=pT, sbk=sbk, col0=col0: e.activation(out=pT[:, col0:], in_=self.ps[sbk][:, col0:], func=AF.Exp, scale=scale, bias=bias_ap),
                      reads=[("ps", sbk)] + list(bias_k), writes=[pk])
            else:
                P.act(lambda e, pT=pT, sbk=sbk, col0=col0: e.activation(out=pT[:, col0:], in_=self.ps[sbk][:, col0:], func=AF.Exp, scale=scale),
                      reads=[("ps", sbk)], writes=[pk])
            if r is not None:
                P.pool(lambda e, pT=pT, col0=col0: e.memset(pT[64:128, col0:col0 + 64], 0.0), reads=[pk], writes=[pk])
            first = idx == 0
            last = idx == len(lst) - 1
            for (bank, lfn) in pv_list:
                lhs, lk = lfn(kc)
                P.pe(lambda e, bank=bank, lhs=lhs, pT=pT, col0=col0, first=first, last=last:
                     e.matmul(self.ps[bank][:, col0:], lhsT=lhs, rhs=pT[:, col0:], start=first, stop=last),
                     reads=[pk] + list(lk), writes=[("ps", bank)])
            P.pe(lambda e, pT=pT, col0=col0, first=first, last=last:
                 e.matmul(self.ps[den_bank][:, col0:], lhsT=self.ones_bf[:], rhs=pT[:, col0:], start=first, stop=last),
                 reads=[pk, ("ones",)], writes=[("ps", den_bank)])

    def attn_diff(self, dr, prev, lam_init):
        P = self.P
        ar = self.arena
        q_sb = ar[:, 0:2048].rearrange("p (c t) -> p c t", t=T)
        k_sb = ar[:, 2048:6144].rearrange("p (c t) -> p c t", t=2 * T)
        v_sb = ar[:, 6144:10240].rearrange("p (k d) -> p k d", d=256)
        qT, kT, V = dr["qT"], dr["kT"], dr["V"]
        lamT = self.small[:, 168:172]
        self.load(lamT, dr["lamT"], [("lamT",)])
        prod = self.small[:, 172:174]
        P.dve(lambda e: e.tensor_tensor(out=prod[:, 0:1], in0=lamT[:, 0:1], in1=lamT[:, 1:2], op=ALU.mult), reads=[("lamT",)], writes=[("lprod", 0)])
        P.dve(lambda e: e.tensor_tensor(out=prod[:, 1:2], in0=lamT[:, 2:3], in1=lamT[:, 3:4], op=ALU.mult), reads=[("lamT",)], writes=[("lprod", 1)])
        of, ofk = self.tf()
        P.dve(lambda e, of=of: e.memset(of[:, :128], 1.0), writes=[ofk])
        P.pe(lambda e, of=of: e.matmul(self.ps[7][:, 0:2], lhsT=of[:, :128], rhs=prod, start=True, stop=True),
             reads=[ofk, ("lprod", 0), ("lprod", 1)], writes=[("ps", 7)])
        ee = self.small[:, 174:176]
        P.act(lambda e: e.activation(out=ee, in_=self.ps[7][:, 0:2], func=AF.Exp), reads=[("ps", 7)], writes=[("lexp",)])
        nlam = self.small[:, 176:177]
        P.dve(lambda e: e.tensor_tensor(out=nlam, in0=ee[:, 1:2], in1=ee[:, 0:1], op=ALU.subtract), reads=[("lexp",)], writes=[("nlam",)])
        P.dve(lambda e: e.tensor_scalar(out=nlam, in0=nlam, scalar1=-lam_init, scalar2=None, op0=ALU.add), reads=[("nlam",)], writes=[("nlam",)])
        sg = self.small[:, 177:179]
        self.load(sg, dr["sgT"], [("sg",)])
        P.dve(lambda e: e.tensor_scalar(out=sg, in0=sg, scalar1=(1.0 - lam_init), scalar2=None, op0=ALU.mult), reads=[("sg",)], writes=[("sg",)])
        scale = 128 ** -0.5
        bias_ap = self.small[:, 179:180] if prev == "data" else None
        if prev == "data":
            self.load(bias_ap, dr["pbias"], [("pbias",)])
        has_prev = prev in ("data", "static")
        it = 0
        for h in range(8):
            for c in range(2):
                self.load(q_sb[:, c, :], qT[2 * h + c], [("ar", "q", c)])
                if has_prev:
                    self.load(k_sb[:, c, 0:T], dr["kT_prev"][2 * h + c], [("ar", "kp", c)])
                self.load(k_sb[:, c, T:2 * T], kT[2 * h + c], [("ar", "k", c)])
            if has_prev:
                self.load(v_sb[:, 0:8, :], dr["V_prev"][:, h * 256:(h + 1) * 256].rearrange("(k p) d -> p k d", p=128), [("ar", "vp")])
            self.load(v_sb[:, 8:16, :], V[:, h * 256:(h + 1) * 256].rearrange("(k p) d -> p k d", p=128), [("ar", "v")])
            for qt in range(2):
                on = []
                for c in range(2):
                    banks = (2, 3, 4) if it % 2 == 0 else (5, 6, 7)
                    it += 1

                    def s_mm(kc, qcol, col0, sbk, c=c):
                        isp = kc < 8
                        P.pe(lambda e: e.matmul(self.ps[sbk][:, col0:], lhsT=k_sb[:, c, kc * 128:(kc + 1) * 128], rhs=q_sb[:, c, qcol:(qcol - col0) + 512],
                                                start=True, stop=True),
                             reads=[("ar", "kp" if isp else "k", c), ("ar", "q", c)], writes=[("ps", sbk)])

                    pv = [(banks[d], (lambda kc, d=d: (v_sb[:, kc, d * 128:(d + 1) * 128], [("ar", "vp" if kc < 8 else "v")]))) for d in range(2)]
                    self.attn_scores_pv(qt, has_prev, bias_ap, [("pbias",)], scale, s_mm, pv, banks[2])
                    rd, rdk = self.tf()
                    P.dve(lambda e, rd=rd, banks=banks: e.reciprocal(out=rd[:], in_=self.ps[banks[2]][:]), reads=[("ps", banks[2])], writes=[rdk])
                    for d in range(2):
                        o, ok = self.tf()
                        P.dve(lambda e, o=o, rd=rd, banks=banks, d=d: e.tensor_tensor(out=o[:], in0=self.ps[banks[d]][:], in1=rd[:], op=ALU.mult),
                              reads=[("ps", banks[d]), rdk], writes=[ok])
                        on.append((o, ok))
                os_ = []
                for d in range(2):
                    (o0, k0), (o1, k1) = on[d], on[2 + d]
                    P.dve(lambda e, o0=o0, o1=o1: e.scalar_tensor_tensor(out=o0[:], in0=o1[:], scalar=nlam, in1=o0[:], op0=ALU.mult, op1=ALU.add),
                           reads=[k0, k1, ("nlam",)], writes=[k0])
                    os_.append((o0, k0))
                for d in range(2):
                    sq, sqk = self.tb()
                    P.act(lambda e, sq=sq, o=os_[d][0]: e.activation(out=sq[:], in_=o[:], func=AF.Square), reads=[os_[d][1]], writes=[sqk])
                    P.pe(lambda e, sq=sq, d=d: e.matmul(self.ps[0][:], lhsT=self.ones_bf[:], rhs=sq[:], start=(d == 0), stop=(d == 1)),
                         reads=[sqk, ("ones",)], writes=[("ps", 0)])
                rs, rsk = self.rs_tile()
                P.act(lambda e, rs=rs: e.activation(out=rs[:], in_=self.ps[0][:], func=AF.Sqrt, scale=1.0 / 256, bias=self.small[:, 255:256]),
                      reads=[("ps", 0), ("epscol",)], writes=[rsk])
                P.dve(lambda e, rs=rs: e.reciprocal(out=rs[:], in_=rs[:]), reads=[rsk], writes=[rsk])
                for d in range(2):
                    o, ok = os_[d]
                    P.dve(lambda e, o=o, rs=rs: e.tensor_tensor(out=o[:], in0=o[:], in1=rs[:], op=ALU.mult), reads=[ok, rsk], writes=[ok])
                    P.pool(lambda e, o=o, d=d, h=h, qt=qt: e.tensor_scalar(out=self.actT[:, 2 * h + d, qt * 512:(qt + 1) * 512], in0=o[:],
                                                                            scalar1=sg[:, d:d + 1], scalar2=None, op0=ALU.mult),
                           reads=[ok, ("sg",)], writes=[("actT", 2 * h + d, qt)])

    def attn_mla(self, dr, prev):
        P = self.P
        ar = self.arena
        qn_sb = ar[:, 0:1024]
        qr_sb = ar[:, 1024:2048]
        kn_sb = ar[:, 2048:4096]
        kr_sb = ar[:, 4096:6144]
        v_sb = ar[:, 6144:8192].rearrange("p (k d) -> p k d", d=128)
        scale = 192 ** -0.5
        bias_ap = self.small[:, 179:180] if prev == "data" else None
        if prev == "data":
            self.load(bias_ap, dr["pbias"], [("pbias",)])
        has_prev = prev in ("data", "static")
        if has_prev:
            self.load(kr_sb[:64, 0:T], dr["krT_prev"], [("ar", "krp")])
        self.load(kr_sb[:64, T:2 * T], dr["krT"], [("ar", "kr")])
        it = 0
        for h in range(16):
            self.load(qn_sb, dr["qnT"][h], [("ar", "qn")])
            self.load(qr_sb[:64, :], dr["qrT"][h], [("ar", "qr")])
            if has_prev:
                self.load(kn_sb[:, 0:T], dr["knT_prev"][h], [("ar", "knp")])
                self.load(v_sb[:, 0:8, :], dr["V_prev"][:, h * 128:(h + 1) * 128].rearrange("(k p) d -> p k d", p=128), [("ar", "vp")])
            self.load(kn_sb[:, T:2 * T], dr["knT"][h], [("ar", "kn")])
            self.load(v_sb[:, 8:16, :], dr["V"][:, h * 128:(h + 1) * 128].rearrange("(k p) d -> p k d", p=128), [("ar", "v")])
            for qt in range(2):
                banks = (2, 4) if it % 2 == 0 else (5, 7)
                it += 1

                def s_mm(kc, qcol, col0, sbk):
                    isp = kc < 8
                    P.pe(lambda e: e.matmul(self.ps[sbk][:, col0:], lhsT=kn_sb[:, kc * 128:(kc + 1) * 128], rhs=qn_sb[:, qcol:(qcol - col0) + 512],
                                            start=True, stop=False),
                         reads=[("ar", "knp" if isp else "kn"), ("ar", "qn")], writes=[("ps", sbk)])
                    P.pe(lambda e: e.matmul(self.ps[sbk][:, col0:], lhsT=kr_sb[:64, kc * 128:(kc + 1) * 128], rhs=qr_sb[:64, qcol:(qcol - col0) + 512],
                                            start=False, stop=True),
                         reads=[("ar", "krp" if isp else "kr"), ("ar", "qr")], writes=[("ps", sbk)])

                pv = [(banks[0], (lambda kc: (v_sb[:, kc, :], [("ar", "vp" if kc < 8 else "v")])))]
                self.attn_scores_pv(qt, has_prev, bias_ap, [("pbias",)], scale, s_mm, pv, banks[1])
                rd, rdk = self.tf()
                P.dve(lambda e, rd=rd, banks=banks: e.reciprocal(out=rd[:], in_=self.ps[banks[1]][:]), reads=[("ps", banks[1])], writes=[rdk])
                P.dve(lambda e, rd=rd, banks=banks, h=h, qt=qt: e.tensor_tensor(out=self.actT[:, h, qt * 512:(qt + 1) * 512], in0=self.ps[banks[0]][:], in1=rd[:], op=ALU.mult),
                      reads=[("ps", banks[0]), rdk], writes=[("actT", h, qt)])

    def out_proj(self, w_out):
        P = self.P
        Wv = w_out.rearrange("(kc p) n -> p kc n", p=128)

        def evac(ci, tt, ps, pk):
            cs = slice(tt * 512, (tt + 1) * 512)
            P.dve(lambda e, ci=ci, cs=cs, ps=ps: e.scalar_tensor_tensor(out=self.hT[:, ci, cs], in0=ps, scalar=self.modT[:, 32 + ci:33 + ci],
                                                                        in1=self.hT[:, ci, cs], op0=ALU.mult, op1=ALU.add),
                  reads=[pk, ("modT", 32 + ci), ("hT", ci, tt)], writes=[("hT", ci, tt)])

        self.linear_fm(Wv, KC, [(j * 128, 128) for j in range(16)], evac=evac, rhs_fn=self.rhs_act, n_tiles=2)

    def moe(self, dr, n_exp=NE):
        P = self.P
        self.load(self.bgu[:], dr["bguT"], [("bgu",)])
        self.load(self.wr[:], dr["wrT"], [("wr",)])
        self.load(self.brr[:], dr["brr"], [("brr",)])
        bgu3 = self.bgu[:].rearrange("p (e j) -> p e j", j=16)
        P.dve(lambda e: e.tensor_scalar(out=bgu3[:, :, 8:16], in0=bgu3[:, :, 8:16], scalar1=1.0, scalar2=None, op0=ALU.add),
              reads=[("bgu",)], writes=[("bgu",)])
        wr3 = self.wr[:].rearrange("p (k e) -> p k e", e=NE)
        A, Bc, abk = self.adaln_cols(dr["gffnT"], 1)
        LB = 5

        def want32(kc, tt, u32, uk):
            for tb4 in range(4):
                tb_ = tt * 4 + tb4
                P.pe(lambda e, u32=u32, kc=kc, tb4=tb4, tb_=tb_: e.matmul(self.ps[LB][:, tb_ * 32:(tb_ + 1) * 32], lhsT=u32[:, tb4 * 128:(tb4 + 1) * 128],
                                                                          rhs=wr3[:, kc, :], start=(kc == 0 and tb_ == 0), stop=(kc == KC - 1), skip_group_check=True),
                     reads=[uk, ("wr",)], writes=[("ps", LB)])

        self.norm_adaln(A, Bc, abk, want32=want32)
        rt = self.rt
        for tb_ in range(8):
            lg = rt[:, 0:32]
            P.dve(lambda e, tb_=tb_: e.tensor_tensor(out=lg, in0=self.ps[LB][:, tb_ * 32:(tb_ + 1) * 32], in1=self.brr[:], op=ALU.add),
                  reads=[("ps", LB), ("brr",)], writes=[("rt", "lg")])
            m8 = rt[:, 32:40]
            P.dve(lambda e: e.max(out=m8, in_=lg), reads=[("rt", "lg")], writes=[("rt", "m8")])
            mk, mkk = self.tf()
            P.dve(lambda e, mk=mk: e.tensor_scalar(out=mk[:, 0:32], in0=lg, scalar1=m8[:, 3:4], scalar2=None, op0=ALU.is_ge),
                  reads=[("rt", "lg"), ("rt", "m8")], writes=[mkk])
            nm = rt[:, 40:41]
            P.dve(lambda e: e.tensor_scalar(out=nm, in0=m8[:, 0:1], scalar1=-1.0, scalar2=None, op0=ALU.mult), reads=[("rt", "m8")], writes=[("rt", "nm")])
            ex, exk = self.tf()
            P.act(lambda e, ex=ex: e.activation(out=ex[:, 0:32], in_=lg, func=AF.Exp, bias=nm, scale=1.0), reads=[("rt", "lg"), ("rt", "nm")], writes=[exk])
            P.dve(lambda e, ex=ex, mk=mk: e.tensor_tensor(out=ex[:, 0:32], in0=ex[:, 0:32], in1=mk[:, 0:32], op=ALU.mult), reads=[exk, mkk], writes=[exk])
            dn = rt[:, 41:42]
            P.dve(lambda e, ex=ex: e.reduce_sum(out=dn, in_=ex[:, 0:32], axis=AX.X), reads=[exk], writes=[("rt", "dn")])
            P.dve(lambda e: e.reciprocal(out=dn, in_=dn), reads=[("rt", "dn")], writes=[("rt", "dn")])
            P.dve(lambda e, ex=ex: e.tensor_scalar(out=ex[:, 0:32], in0=ex[:, 0:32], scalar1=dn, scalar2=None, op0=ALU.mult), reads=[exk, ("rt", "dn")], writes=[exk])
            P.pe(lambda e, ex=ex, tb_=tb_: e.transpose(out=self.ps[7][:32, (tb_ % 4) * 128:(tb_ % 4 + 1) * 128], in_=ex[:, 0:32], identity=self.ident[:]),
                 reads=[exk, ("ident",)], writes=[("ps", 7)])
            P.dve(lambda e, tb_=tb_: e.tensor_copy(out=self.GT[:, tb_ * 128:(tb_ + 1) * 128], in_=self.ps[7][:32, (tb_ % 4) * 128:(tb_ % 4 + 1) * 128]),
                  reads=[("ps", 7)], writes=[("GT", tb_ // 4)])
        Ap = self.arena[:, 0:8192].rearrange("p (f t) -> p f t", t=T)
        for ex_i in range(n_exp):
            P.dve(lambda e, ex_i=ex_i: e.tensor_scalar(out=self.sel[:], in0=self.pidx[:], scalar1=float(ex_i), scalar2=None, op0=ALU.is_equal),
                  reads=[("pidx",)], writes=[("sel",)])
            gbs = []
            for tt in range(2):
                P.pe(lambda e, tt=tt: e.matmul(self.ps[6 + tt][:], lhsT=self.sel[:], rhs=self.GT[:, tt * 512:(tt + 1) * 512], start=True, stop=True),
                     reads=[("sel",), ("GT", tt)], writes=[("ps", 6 + tt)])
                gb = self.gb[ex_i % 2][:, tt * 512:(tt + 1) * 512]
                gbk = ("gb", ex_i % 2, tt)
                P.act(lambda e, gb=gb, tt=tt: e.copy(out=gb, in_=self.ps[6 + tt][:]), reads=[("ps", 6 + tt)], writes=[gbk])
                gbs.append((gb, gbk))
            Wgu = dr["w_gu"][ex_i].rearrange("(kc p) n -> p kc n", p=128)
            for fc in range(8):
                (wg, wu), wk = self.wload([Wgu[:, :, fc * 128:(fc + 1) * 128], Wgu[:, :, 1024 + fc * 128:1024 + (fc + 1) * 128]], KC)
                for tt in range(2):
                    cs = slice(tt * 512, (tt + 1) * 512)
                    bg = 0 + 2 * (self.mmb % 2)
                    bu = bg + 1
                    self.mmb += 1
                    for kc in range(KC):
                        P.pe(lambda e, bg=bg, kc=kc, wg=wg, cs=cs: e.matmul(self.ps[bg][:], lhsT=wg[:, kc, :], rhs=self.actT[:, kc, cs], start=(kc == 0), stop=(kc == KC - 1)),
                             reads=[wk, ("actT", kc, tt)], writes=[("ps", bg)])
                    for kc in range(KC):
                        P.pe(lambda e, bu=bu, kc=kc, wu=wu, cs=cs: e.matmul(self.ps[bu][:], lhsT=wu[:, kc, :], rhs=self.actT[:, kc, cs], start=(kc == 0), stop=(kc == KC - 1)),
                             reads=[wk, ("actT", kc, tt)], writes=[("ps", bu)])
                    gc, gck = self.tf()
                    P.dve(lambda e, gc=gc, bg=bg, ex_i=ex_i, fc=fc: e.tensor_scalar(out=gc[:], in0=self.ps[bg][:], scalar1=bgu3[:, ex_i, fc:fc + 1], scalar2=7.0, op0=ALU.add, op1=ALU.min),
                          reads=[("ps", bg), ("bgu",)], writes=[gck])
                    sg_, sgk = self.tf()
                    P.act(lambda e, gc=gc, sg_=sg_: e.activation(out=sg_[:], in_=gc[:], func=AF.Sigmoid, scale=1.702), reads=[gck], writes=[sgk])
                    uc, uck = self.tf()
                    P.dve(lambda e, uc=uc, bu=bu, ex_i=ex_i, fc=fc: e.tensor_scalar(out=uc[:], in0=self.ps[bu][:], scalar1=bgu3[:, ex_i, 8 + fc:9 + fc], scalar2=-6.0, op0=ALU.add, op1=ALU.max),
                          reads=[("ps", bu), ("bgu",)], writes=[uck])
                    P.pool(lambda e, gc=gc, sg_=sg_: e.tensor_tensor(out=gc[:], in0=gc[:], in1=sg_[:], op=ALU.mult), reads=[gck, sgk], writes=[gck])
                    P.dve(lambda e, gc=gc, uc=uc: e.scalar_tensor_tensor(out=uc[:], in0=uc[:], scalar=8.0, in1=gc[:], op0=ALU.min, op1=ALU.mult), reads=[gck, uck], writes=[uck])
                    gb, gbk = gbs[tt]
                    P.pool(lambda e, uc=uc, gb=gb, fc=fc, cs=cs: e.tensor_tensor(out=Ap[:, fc, cs], in0=uc[:], in1=gb, op=ALU.mult), reads=[uck, gbk], writes=[("Ap", fc, tt)])
            Wd = dr["w_down"][ex_i].rearrange("(kc p) n -> p kc n", p=128)
            for dp in range(4):
                (wd,), wk = self.wload([Wd[:, :, dp * 512:(dp + 1) * 512]], 8)
                for dci in range(4):
                    dc = dp * 4 + dci
                    for tt in range(2):
                        cs = slice(tt * 512, (tt + 1) * 512)
                        b = 4 + (self.mmb % 2)
                        self.mmb += 1
                        for fk in range(8):
                            P.pe(lambda e, b=b, fk=fk, wd=wd, dci=dci, cs=cs: e.matmul(self.ps[b][:], lhsT=wd[:, fk, dci * 128:(dci + 1) * 128], rhs=Ap[:, fk, cs], start=(fk == 0), stop=(fk == 7)),
                                 reads=[wk, ("Ap", fk, tt)], writes=[("ps", b)])
                        P.dve(lambda e, b=b, dc=dc, cs=cs: e.scalar_tensor_tensor(out=self.hT[:, dc, cs], in0=self.ps[b][:], scalar=self.modT[:, 80 + dc:81 + dc], in1=self.hT[:, dc, cs],
                                                                                 op0=ALU.mult, op1=ALU.add),
                              reads=[("ps", b), ("modT", 80 + dc), ("hT", dc, tt)], writes=[("hT", dc, tt)])
        bdv = self.wst[0][:32, 0:D]
        self.load(bdv, dr["bd"], [("wst", 0, i) for i in range(4)])
        for dc in range(KC):
            for tt in range(2):
                cs = slice(tt * 512, (tt + 1) * 512)
                b = 4 + (self.mmb % 2)
                self.mmb += 1
                P.pe(lambda e, b=b, dc=dc, cs=cs: e.matmul(self.ps[b][:], lhsT=bdv[:, dc * 128:(dc + 1) * 128], rhs=self.GT[:, cs], start=True, stop=True),
                     reads=[("wst", 0, 0), ("GT", tt)], writes=[("ps", b)])
                P.dve(lambda e, b=b, dc=dc, cs=cs: e.scalar_tensor_tensor(out=self.hT[:, dc, cs], in0=self.ps[b][:], scalar=self.modT[:, 80 + dc:81 + dc], in1=self.hT[:, dc, cs],
                                                                         op0=ALU.mult, op1=ALU.add),
                      reads=[("ps", b), ("modT", 80 + dc), ("hT", dc, tt)], writes=[("hT", dc, tt)])

    def final_norm(self, dr):
        P = self.P
        g = self.small[:, 128:144]
        self.load(g, dr["gfinT"], [("gcol",)])
        outv = dr["outT"].rearrange("(kc p) t -> p kc t", p=128)
        for tt in range(2):
            cs = slice(tt * 512, (tt + 1) * 512)
            sb_ = 6 + tt
            for kc in range(KC):
                sq, sqk = self.tb()
                P.act(lambda e, sq=sq, kc=kc, cs=cs: e.activation(out=sq[:], in_=self.hT[:, kc, cs], func=AF.Square), reads=[("hT", kc, tt)], writes=[sqk])
                P.pe(lambda e, sq=sq, kc=kc, sb_=sb_: e.matmul(self.ps[sb_][:], lhsT=self.ones_bf[:], rhs=sq[:], start=(kc == 0), stop=(kc == KC - 1)),
                     reads=[sqk, ("ones",)], writes=[("ps", sb_)])
            rs, rsk = self.rs_tile()
            P.act(lambda e, rs=rs, sb_=sb_: e.activation(out=rs[:], in_=self.ps[sb_][:], func=AF.Sqrt, scale=1.0 / D, bias=self.small[:, 255:256]),
                  reads=[("ps", sb_), ("epscol",)], writes=[rsk])
            P.dve(lambda e, rs=rs: e.reciprocal(out=rs[:], in_=rs[:]), reads=[rsk], writes=[rsk])
            for kc in range(KC):
                t, tk = self.tf()
                P.dve(lambda e, t=t, kc=kc, cs=cs, rs=rs: e.tensor_tensor(out=t[:], in0=self.hT[:, kc, cs], in1=rs[:], op=ALU.mult), reads=[("hT", kc, tt), rsk], writes=[tk])
                P.pool(lambda e, t=t, kc=kc: e.tensor_scalar(out=t[:], in0=t[:], scalar1=g[:, kc:kc + 1], scalar2=None, op0=ALU.mult), reads=[tk, ("gcol",)], writes=[tk])
                self.load(outv[:, kc, cs], t[:], [("dram", "out", kc, tt)], rkeys=[tk], q="pool")

    def phaseB(self, dr, kind, prev, lam_init, final, n_exp=NE, do_attn=True, do_moe=True):
        self.eps_col()
        self.load_hT(dr["hT_in"])
        if dr.get("modT_in") is not None:
            self.load(self.modT[:], dr["modT_in"], [("modT", j) for j in range(96)])
        if do_attn:
            if kind == "diff":
                self.attn_diff(dr, prev, lam_init)
            else:
                self.attn_mla(dr, prev)
            self.out_proj(dr["w_out"])
        self.P.barrier()
        if do_moe:
            self.moe(dr, n_exp)
        if final:
            self.final_norm(dr)
        else:
            self.store_hT(dr["hT_out"])


def _dram(nc, name, shape, dt, kind):
    return nc.dram_tensor(name, list(shape), dt, kind=kind).ap()


COMMON_IN = dict(rotT=([64, 96], BF16), invf=([64, 2], F32), ident=([128, 128], F32), pidx=([32, 128], F32))

A_IN = {
    "diff": dict(hT_in=([D, T], F32), ccol=([128, 16], F32), pos=([64, T], I32), adabT=([128, 96], F32), ada_w=([D, 6 * D], F32),
                 gmixT=([128, 16], F32), w_in=([D, 6144], F32)),
    "mla": dict(hT_in=([D, T], F32), ccol=([128, 16], F32), pos=([64, T], I32), adabT=([128, 96], F32), ada_w=([D, 6 * D], F32),
                gmixT=([128, 16], F32), w_in=([D, 1088], F32), qgT=([128, 4], F32), kvgT=([128, 4], F32),
                w_uq=([512, 3072], F32), w_ukv=([512, 4096], F32)),
}
A_OUT = {
    "diff": dict(modT_out=([128, 96], F32), qT=([16, 128, T], BF16), kT=([16, 128, T], BF16), V=([T, D], BF16)),
    "mla": dict(modT_out=([128, 96], F32), qnT=([16, 128, T], BF16), qrT=([16, 64, T], BF16), knT=([16, 128, T], BF16),
                krT=([64, T], BF16), V=([T, D], BF16)),
}
B_IN_COMMON = dict(hT_in=([D, T], F32), modT_in=([128, 96], F32), w_out=([D, D], F32), gffnT=([128, 16], F32),
                   bguT=([128, NE * 16], F32), bd=([NE, D], F32), wrT=([128, KC * NE], F32), brr=([128, NE], F32),
                   w_gu=([NE, D, D], F32), w_down=([NE, D // 2, D], F32), pbias=([128, 1], F32))
B_IN = {
    "diff": dict(qT=([16, 128, T], BF16), kT=([16, 128, T], BF16), V=([T, D], BF16), kT_prev=([16, 128, T], BF16), V_prev=([T, D], BF16),
                 lamT=([128, 4], F32), sgT=([128, 2], F32)),
    "mla": dict(qnT=([16, 128, T], BF16), qrT=([16, 64, T], BF16), knT=([16, 128, T], BF16), krT=([64, T], BF16), V=([T, D], BF16),
                knT_prev=([16, 128, T], BF16), krT_prev=([64, T], BF16), V_prev=([T, D], BF16)),
}


def _finish(nc, es, B):
    B.P.analyze()
    sems = {e: [es.enter_context(nc.semaphore(f"s_{e}{i}")) for i in range(max(1, (B.P.cnt[e] + EPOCH - 1) // EPOCH))] for e in ENGS}
    dsems = {e: [es.enter_context(nc.semaphore(f"d_{e}{i}")) for i in range(NSLOT)] for e in ["sp", "pool", "act"]}
    block = es.enter_context(nc.Block())
    B.P.emit(block, sems, dsems)


def build_A(kind):
    nc = bass.Bass("TRN2", target_bir_lowering=False, dynamic_dma_scratch_size=4096)
    dr = {}
    for k, (s, dt) in {**COMMON_IN, **A_IN[kind]}.items():
        dr[k] = _dram(nc, k, s, dt, "ExternalInput")
    for k, (s, dt) in A_OUT[kind].items():
        dr[k] = _dram(nc, k, s, dt, "ExternalOutput")
    with ExitStack() as es:
        B = Bld(nc, es)
        B.consts(dr)
        if kind == "diff":
            B.phaseA_diff(dr)
        else:
            B.phaseA_mla(dr)
        _finish(nc, es, B)
    return nc


def build_B(kind, lam_init, final, n_exp=NE, do_attn=True, do_moe=True):
    nc = bass.Bass("TRN2", target_bir_lowering=False, dynamic_dma_scratch_size=4096)
    dr = {}
    spec = {**COMMON_IN, **B_IN_COMMON, **B_IN[kind]}
    if not do_moe:
        for k in ("w_gu", "w_down", "bguT", "bd", "wrT", "brr", "gffnT"):
            spec.pop(k)
    elif n_exp != NE:
        spec["w_gu"] = ([n_exp, D, D], F32)
        spec["w_down"] = ([n_exp, D // 2, D], F32)
    if not do_attn:
        for k in list(B_IN[kind].keys()) + ["w_out", "pbias"]:
            spec.pop(k)
    for k, (s, dt) in spec.items():
        dr[k] = _dram(nc, k, s, dt, "ExternalInput")
    if final:
        dr["gfinT"] = _dram(nc, "gfinT", [128, 16], F32, "ExternalInput")
        dr["outT"] = _dram(nc, "outT", [D, T], F32, "ExternalOutput")
    else:
        dr["hT_out"] = _dram(nc, "hT_out", [D, T], F32, "ExternalOutput")
    with ExitStack() as es:
        B = Bld(nc, es)
        B.consts(dr)
        B.phaseB(dr, kind, "data", lam_init, final, n_exp, do_attn, do_moe)
        _finish(nc, es, B)
    return nc


def build_fused(depth=4, n_exp=NE):
    nc = bass.Bass("TRN2", target_bir_lowering=False, dynamic_dma_scratch_size=4096)
    I = lambda name, shape, dt: _dram(nc, name, shape, dt, "ExternalInput")
    N = lambda name, shape, dt: nc.dram_tensor(name, list(shape), dt).ap()
    g = {}
    for k, (sh, dt) in COMMON_IN.items():
        g[k] = I(k, sh, dt)
    xT = I("xT", [2, D, T], F32)
    pos = I("pos", [2, 64, T], I32)
    ccol = I("ccol", [128, 16], F32)
    outT = _dram(nc, "outT", [2, D, T], F32, "ExternalOutput")
    hbuf = N("hbuf", [2, D, T], F32)
    scr = {
        "diff": [dict(qT=N(f"qT{h}", [16, 128, T], BF16), kT=N(f"kT{h}", [16, 128, T], BF16), V=N(f"Vd{h}", [T, D], BF16)) for h in range(2)],
        "mla": [dict(qnT=N(f"qnT{h}", [16, 128, T], BF16), qrT=N(f"qrT{h}", [16, 64, T], BF16), knT=N(f"knT{h}", [16, 128, T], BF16),
                     krT=N(f"krT{h}", [64, T], BF16), V=N(f"Vm{h}", [T, D], BF16)) for h in range(2)],
    }
    L = []
    for i in range(depth):
        kind = "diff" if i % 2 == 0 else "mla"
        w = dict(adabT=I(f"adabT{i}", [128, 96], F32), ada_w=I(f"ada_w{i}", [D, 6 * D], F32), gmixT=I(f"gmixT{i}", [128, 16], F32),
                 gffnT=I(f"gffnT{i}", [128, 16], F32), bguT=I(f"bguT{i}", [128, NE * 16], F32), bd=I(f"bd{i}", [NE, D], F32),
                 wrT=I(f"wrT{i}", [128, KC * NE], F32), brr=I(f"brr{i}", [128, NE], F32),
                 w_gu=I(f"w_gu{i}", [n_exp, D, D], F32), w_down=I(f"w_down{i}", [n_exp, D // 2, D], F32), w_out=I(f"w_out{i}", [D, D], F32))
        if kind == "diff":
            w.update(w_in=I(f"w_in{i}", [D, 6144], F32), lamT=I(f"lamT{i}", [128, 4], F32), sgT=I(f"sgT{i}", [128, 2], F32))
        else:
            w.update(w_in=I(f"w_in{i}", [D, 1088], F32), qgT=I(f"qgT{i}", [128, 4], F32), kvgT=I(f"kvgT{i}", [128, 4], F32),
                     w_uq=I(f"w_uq{i}", [512, 3072], F32), w_ukv=I(f"w_ukv{i}", [512, 4096], F32))
        L.append(w)
    gfinT = I("gfinT", [128, 16], F32)
    with ExitStack() as es:
        B = Bld(nc, es)
        B.consts(g)
        for i in range(depth):
            kind = "diff" if i % 2 == 0 else "mla"
            lam_init = 0.8 - 0.6 * math.exp(-0.3 * i)
            w = L[i]
            final = i == depth - 1
            for half in range(2):
                dr = dict(g)
                dr.update(w)
                dr.update(scr[kind][half])
                dr.update(hT_in=(xT[half] if i == 0 else hbuf[half]), pos=pos[half], ccol=ccol)
                if half == 1:
                    dr["ada_w"] = None
                if kind == "diff":
                    B.phaseA_diff(dr)
                else:
                    B.phaseA_mla(dr)
                B.P.barrier()
            for half in range(2):
                dr = dict(g)
                dr.update(w)
                dr.update(scr[kind][half])
                dr.update(hT_in=(xT[half] if i == 0 else hbuf[half]), hT_out=hbuf[half], gfinT=gfinT, outT=outT[half])
                if half == 1:
                    for k, v in scr[kind][0].items():
                        if k[0] in "kV":
                            dr[k + "_prev"] = v
                B.phaseB(dr, kind, "static" if half == 1 else "none", lam_init, final, n_exp)
                B.P.barrier()
        _finish(nc, es, B)
    return nc


def fused_inputs(inp, n_exp=NE, depth=4):
    hc = host_consts()
    f = lambda a: np.asarray(a, np.float32)
    shared = dict(hc)
    for i in range(depth):
        kind = "diff" if i % 2 == 0 else "mla"
        j = i // 2
        shared.update({
            f"adabT{i}": colT(inp["ada_b"][i], 96), f"ada_w{i}": f(inp["ada_w"][i]), f"gmixT{i}": colT(inp["mix_norm_g"][i], 16),
            f"gffnT{i}": colT(inp["ffn_norm_g"][i], 16),
            f"bguT{i}": np.ascontiguousarray(f(inp["moe_b_gate_up"][i]).reshape(NE, 16, 128).transpose(2, 0, 1).reshape(128, NE * 16)),
            f"bd{i}": f(inp["moe_b_down"][i]),
            f"wrT{i}": np.ascontiguousarray(f(inp["moe_w_router"][i]).reshape(KC, 128, NE).transpose(1, 0, 2).reshape(128, KC * NE)),
            f"brr{i}": np.ascontiguousarray(np.broadcast_to(f(inp["moe_b_router"][i])[None, :], (128, NE))),
            f"w_gu{i}": f(inp["moe_w_gate_up"][i])[:n_exp], f"w_down{i}": f(inp["moe_w_down"][i])[:n_exp],
        })
        if kind == "diff":
            shared.update({f"w_in{i}": f(inp["diff_w_in"][j]), f"lamT{i}": np.ascontiguousarray(f(inp["diff_lambda"][j]).T),
                           f"sgT{i}": colT(inp["diff_subln_g"][j], 2), f"w_out{i}": f(inp["diff_w_out"][j])})
        else:
            shared.update({f"w_in{i}": f(inp["mla_w_in"][j]), f"qgT{i}": colT(inp["mla_q_norm_g"][j], 4), f"kvgT{i}": colT(inp["mla_kv_norm_g"][j], 4),
                           f"w_uq{i}": f(inp["mla_w_uq"][j]), f"w_ukv{i}": f(inp["mla_w_ukv"][j]), f"w_out{i}": f(inp["mla_w_out"][j])})
    shared["gfinT"] = colT(inp["final_norm_g"], 16)
    x = f(inp["x"])
    maps = []
    for b in range(4):
        m = dict(shared)
        m["xT"] = np.ascontiguousarray(x[b].reshape(2, T, D).transpose(0, 2, 1))
        m["pos"] = np.ascontiguousarray(np.broadcast_to(np.asarray(inp["positions"])[b].reshape(2, 1, T), (2, 64, T))).astype(np.int32)
        m["ccol"] = colT(np.asarray(inp["c"])[b], 16)
        maps.append(m)
    return maps


def kernel_fused(**inp):
    nc = _prog(("fused",), lambda: build_fused())
    maps = fused_inputs(inp)
    cores = list(range(8))
    res = run_bass_kernel_spmd(nc, [maps[c // 2] for c in cores], core_ids=cores).results
    res = [res[2 * b] for b in range(4)]
    out = np.empty((4, 2 * T, D), np.float32)
    for b in range(4):
        o = np.asarray(res[b]["outT"])
        out[b] = o.transpose(0, 2, 1).reshape(2 * T, D)
    return out


def colT(v, n):
    return np.ascontiguousarray(np.asarray(v, np.float32).reshape(n, 128).T)


def host_consts():
    rot = np.zeros((64, 96), np.float32)
    for i in range(16):
        rot[i + 16, i] = -1.0
        rot[i, i + 16] = 1.0
    for i in range(32):
        rot[i + 32, 32 + i] = -1.0
        rot[i, 32 + i + 32] = 1.0
    invf = np.zeros((64, 2), np.float32)
    f32 = (500000.0 ** (-np.arange(0, 32, 2, dtype=np.float32) / 32)).astype(np.float32)
    f64 = (500000.0 ** (-np.arange(0, 64, 2, dtype=np.float32) / 64)).astype(np.float32)
    for p in range(32):
        invf[p, 0] = f32[p % 16]
    for p in range(64):
        invf[p, 1] = f64[p % 32]
    return dict(rotT=rot.astype(ml_dtypes.bfloat16), invf=invf, ident=np.eye(128, dtype=np.float32),
                pidx=np.ascontiguousarray(np.broadcast_to(np.arange(32, dtype=np.float32)[:, None], (32, 128))))


_PROGS = {}


def _prog(key, fn):
    if key not in _PROGS:
        _PROGS[key] = fn()
    return _PROGS[key]


def kernel_unfused(x, c, positions, ada_w, ada_b, mix_norm_g, ffn_norm_g, final_norm_g,
           diff_w_in, diff_lambda, diff_subln_g, diff_w_out,
           mla_w_in, mla_q_norm_g, mla_kv_norm_g, mla_w_uq, mla_w_ukv, mla_w_out,
           moe_w_router, moe_b_router, moe_w_gate_up, moe_b_gate_up, moe_w_down, moe_b_down):
    x = np.asarray(x, np.float32)
    NC = 8
    cores = list(range(NC))
    hc = host_consts()
    hT = [np.ascontiguousarray(x[cid // 2, (cid % 2) * T:(cid % 2 + 1) * T, :].T) for cid in cores]
    ccol = [colT(np.asarray(c)[cid // 2], 16) for cid in cores]
    pos = [np.ascontiguousarray(np.broadcast_to(np.asarray(positions)[cid // 2, (cid % 2) * T:(cid % 2 + 1) * T][None, :], (64, T))).astype(np.int32) for cid in cores]
    pbias = [np.full((128, 1), 0.0 if cid % 2 == 1 else -30000.0, np.float32) for cid in cores]
    out = None
    for i in range(4):
        kind = "diff" if i % 2 == 0 else "mla"
        j = i // 2
        lam_init = 0.8 - 0.6 * math.exp(-0.3 * i)
        shared = dict(hc)
        shared.update(adabT=colT(ada_b[i], 96), ada_w=np.asarray(ada_w[i], np.float32), gmixT=colT(mix_norm_g[i], 16))
        if kind == "diff":
            shared.update(w_in=np.asarray(diff_w_in[j], np.float32))
        else:
            shared.update(w_in=np.asarray(mla_w_in[j], np.float32), qgT=colT(mla_q_norm_g[j], 4), kvgT=colT(mla_kv_norm_g[j], 4),
                          w_uq=np.asarray(mla_w_uq[j], np.float32), w_ukv=np.asarray(mla_w_ukv[j], np.float32))
        ncA = _prog(("A", kind), lambda: build_A(kind))
        in_maps = [dict(shared, hT_in=hT[cid], ccol=ccol[cid], pos=pos[cid]) for cid in cores]
        rA = run_bass_kernel_spmd(ncA, in_maps, core_ids=cores).results
        final = i == 3
        sharedB = dict(hc)
        sharedB.update(w_out=np.asarray(diff_w_out[j] if kind == "diff" else mla_w_out[j], np.float32), gffnT=colT(ffn_norm_g[i], 16),
                       bguT=np.ascontiguousarray(np.asarray(moe_b_gate_up[i], np.float32).reshape(NE, 16, 128).transpose(2, 0, 1).reshape(128, NE * 16)),
                       bd=np.asarray(moe_b_down[i], np.float32),
                       wrT=np.ascontiguousarray(np.asarray(moe_w_router[i], np.float32).reshape(KC, 128, NE).transpose(1, 0, 2).reshape(128, KC * NE)),
                       brr=np.ascontiguousarray(np.broadcast_to(np.asarray(moe_b_router[i], np.float32)[None, :], (128, NE))),
                       w_gu=np.asarray(moe_w_gate_up[i], np.float32), w_down=np.asarray(moe_w_down[i], np.float32))
        if kind == "diff":
            sharedB.update(lamT=np.ascontiguousarray(np.asarray(diff_lambda[j], np.float32).T), sgT=colT(diff_subln_g[j], 2))
        if final:
            sharedB.update(gfinT=colT(final_norm_g, 16))
        ncB = _prog(("B", kind, i if kind == "diff" else 0, final), lambda: build_B(kind, lam_init, final))
        in_maps = []
        for cid in cores:
            m = dict(sharedB, hT_in=hT[cid], modT_in=rA[cid]["modT_out"], pbias=pbias[cid])
            partner = cid - 1 if cid % 2 == 1 else cid
            if kind == "diff":
                m.update(qT=rA[cid]["qT"], kT=rA[cid]["kT"], V=rA[cid]["V"], kT_prev=rA[partner]["kT"], V_prev=rA[partner]["V"])
            else:
                m.update(qnT=rA[cid]["qnT"], qrT=rA[cid]["qrT"], knT=rA[cid]["knT"], krT=rA[cid]["krT"], V=rA[cid]["V"],
                         knT_prev=rA[partner]["knT"], krT_prev=rA[partner]["krT"], V_prev=rA[partner]["V"])
            in_maps.append(m)
        rB = run_bass_kernel_spmd(ncB, in_maps, core_ids=cores).results
        if final:
            out = np.empty((4, 2048, D), np.float32)
            for cid in cores:
                out[cid // 2, (cid % 2) * T:(cid % 2 + 1) * T, :] = np.asarray(rB[cid]["outT"]).T
        else:
            hT = [np.asarray(rB[cid]["hT_out"]) for cid in cores]
    return out


def kernel(**inputs):
    return kernel_fused(**inputs)
```

```python
import math
from contextlib import ExitStack

import numpy as np
import ml_dtypes
import concourse.bass as bass
import concourse.mybir as mybir
from concourse.bass_utils import run_bass_kernel_spmd

F32 = mybir.dt.float32
BF16 = mybir.dt.bfloat16
I32 = mybir.dt.int32
AF = mybir.ActivationFunctionType
ALU = mybir.AluOpType
AX = mybir.AxisListType

D = 2048
T = 1024
KC = 16
EPS = 1e-6
NE = 32
ENGS = ["pe", "act", "dve", "pool", "sp"]
NSLOT = 8
EPOCH = 12000
NEPOCH = 24
NTF = 10
NTB = 8
TWO_PI = 2.0 * math.pi


class Prog:
    def __init__(self, nc):
        self.nc = nc
        self.ops = []

    def add(self, eng, fn, reads=(), writes=(), dma=False):
        self.ops.append(dict(eng=eng, fn=fn, reads=tuple(reads), writes=tuple(writes), dma=dma, extra=set()))

    def pe(self, fn, reads=(), writes=()):
        self.add("pe", fn, reads, writes)

    def act(self, fn, reads=(), writes=()):
        self.add("act", fn, reads, writes)

    def dve(self, fn, reads=(), writes=()):
        self.add("dve", fn, reads, writes)

    def pool(self, fn, reads=(), writes=()):
        self.add("pool", fn, reads, writes)

    def dma(self, fn, reads=(), writes=(), q="sp"):
        self.add(q, fn, reads, writes, dma=True)

    def barrier(self):
        self.ops.append(dict(bar=True))

    def analyze(self):
        last_w = {}
        readers = {}
        ops = [o for o in self.ops]
        real = []
        pending_bar = None
        last_comp = {}
        last_dmas = {e: [] for e in ENGS}
        seen_after_bar = set()
        for o in ops:
            if o.get("bar"):
                pending_bar = set()
                for e in ENGS:
                    if e in last_comp:
                        pending_bar.add(last_comp[e])
                    pending_bar.update(last_dmas[e][-NSLOT:])
                seen_after_bar = set()
                continue
            i = len(real)
            real.append(o)
            deps = set()
            for k in o["reads"]:
                if k in last_w:
                    deps.add(last_w[k])
            for k in o["writes"]:
                if k in last_w:
                    deps.add(last_w[k])
                rd = readers.get(k)
                if rd:
                    deps.update(rd[0].values())
                    deps.update(rd[1])
            deps.discard(i)
            if o["eng"] == "pe":
                deps = {d for d in deps if real[d]["eng"] != "pe" or real[d]["dma"]}
            if pending_bar is not None and o["eng"] not in seen_after_bar:
                deps.update(pending_bar)
                seen_after_bar.add(o["eng"])
            o["deps"] = deps
            for k in o["writes"]:
                last_w[k] = i
                readers[k] = ({}, [])
            for k in o["reads"]:
                rd = readers.setdefault(k, ({}, []))
                if o["dma"]:
                    rd[1].append(i)
                else:
                    rd[0][o["eng"]] = i
            if o["dma"]:
                last_dmas[o["eng"]].append(i)
            else:
                last_comp[o["eng"]] = i
        self.real = real
        for o in real:
            o["sig"] = False
        for o in real:
            for d in o["deps"]:
                real[d]["sig"] = True
        cnt = {e: 0 for e in ENGS}
        dcnt = {e: 0 for e in ENGS}
        for o in real:
            e = o["eng"]
            if o["dma"]:
                j = dcnt[e]
                dcnt[e] += 1
                o["slot"] = j % NSLOT
                o["tok"] = ("d", e, j % NSLOT, 16 * (j // NSLOT + 1))
                o["prev"] = 16 * (j // NSLOT)
            elif o["sig"]:
                c = cnt[e]
                cnt[e] += 1
                o["tok"] = ("c", e, c // EPOCH, c % EPOCH + 1)
        self.cnt = cnt
        self.dcnt = dcnt
        for e in ENGS:
            assert cnt[e] < EPOCH * NEPOCH, (e, cnt[e])

    def emit(self, block, sems, dsems):
        nc = self.nc
        ops = self.real
        engobj = {"pe": nc.tensor, "act": nc.scalar, "dve": nc.vector, "pool": nc.gpsimd, "sp": nc.sync}

        def semof(key):
            kind, e, idx = key
            return sems[e][idx] if kind == "c" else dsems[e][idx]

        def run_engine(ename):
            eng = engobj[ename]
            waited = {}
            last_dma_tok = {}
            for op in ops:
                if op["eng"] != ename:
                    continue
                need = {}
                for d in op["deps"]:
                    tok = ops[d]["tok"]
                    key = tok[:3]
                    need[key] = max(need.get(key, 0), tok[3])
                if op["dma"] and op["prev"] > 0:
                    key = ("d", ename, op["slot"])
                    need[key] = max(need.get(key, 0), op["prev"])
                for key, val in need.items():
                    if waited.get(key, 0) >= val:
                        continue
                    eng.wait_ge(semof(key), val)
                    waited[key] = val
                ins = op["fn"](eng)
                if op["dma"]:
                    ins.then_inc(semof(op["tok"][:3]), 16)
                    last_dma_tok[op["slot"]] = op["tok"]
                elif op["sig"]:
                    ins.then_inc(semof(op["tok"][:3]), 1)
            for slot, tok in last_dma_tok.items():
                eng.wait_ge(semof(tok[:3]), tok[3])

        @block.tensor
        def _(e):
            run_engine("pe")

        @block.scalar
        def _(e):
            run_engine("act")

        @block.vector
        def _(e):
            run_engine("dve")

        @block.gpsimd
        def _(e):
            run_engine("pool")

        @block.sync
        def _(e):
            run_engine("sp")


class Bld:
    def __init__(self, nc, es):
        self.nc = nc
        self.P = Prog(nc)
        sb = lambda name, shape, dt: es.enter_context(nc.sbuf_tensor("sb_" + name, shape, dt))
        self.hT = sb("hT", [128, KC, T], F32)
        self.actT = sb("actT", [128, KC, T], BF16)
        self.wst = [sb(f"wst{i}", [128, 4096], F32) for i in range(2)]
        self.wbf = [sb(f"wbf{i}", [128, 4096], BF16) for i in range(2)]
        self.arena = sb("arena", [128, 10240], BF16)
        self.tmpf = [sb(f"tf{i}", [128, 512], F32) for i in range(NTF)]
        self.tmpb = [sb(f"tb{i}", [128, 512], BF16) for i in range(NTB)]
        self.auxf = sb("auxf", [128, 2 * T], F32)
        self.cosT = self.auxf[:, 0:T]
        self.sinT = self.auxf[:, T:2 * T]
        self.gb = [self.auxf[:, 0:T], self.auxf[:, T:2 * T]]
        self.rstd = [sb(f"rstd{i}", [128, 512], F32) for i in range(2)]
        self.rsi = 0
        self.modT = sb("modT", [128, 96], F32)
        self.small = sb("small", [128, 256], F32)
        self.small_b = sb("small_b", [128, 64], BF16)
        self.ones_bf = sb("ones_bf", [128, 128], BF16)
        self.ones_f = sb("ones_f", [128, 128], F32)
        self.rotT = sb("rotT", [64, 96], BF16)
        self.invf = sb("invf", [64, 2], F32)
        self.ident = sb("ident", [128, 128], F32)
        self.pidx = sb("pidx", [32, 128], F32)
        self.bgu = sb("bgu", [128, NE * 16], F32)
        self.wr = sb("wr", [128, KC * NE], F32)
        self.brr = sb("brr", [128, NE], F32)
        self.GT = sb("GT", [32, T], F32)
        self.sel = sb("sel", [32, 128], F32)
        self.rt = sb("rt", [128, 64], F32)
        self.ps = [es.enter_context(nc.psum_tensor(f"ps{i}", [128, 512], F32)) for i in range(8)]
        self.tfi = 0
        self.tbi = 0
        self.wslot = 0
        self.mmb = 0
        self.uid = 0

    def tf(self):
        i = self.tfi
        self.tfi = (i + 1) % NTF
        return self.tmpf[i], ("tf", i)

    def rs_tile(self):
        i = self.rsi
        self.rsi ^= 1
        return self.rstd[i], ("rstd", i)

    def tb(self):
        i = self.tbi
        self.tbi = (i + 1) % NTB
        return self.tmpb[i], ("tb", i)

    def mmbank(self, lo=0, n=4):
        b = lo + self.mmb % n
        self.mmb += 1
        return b

    def load(self, dst, src, wkeys, rkeys=(), q="sp"):
        self.P.dma(lambda e, dst=dst, src=src: e.dma_start(out=dst, in_=src), reads=rkeys, writes=wkeys, q=q)

    def wload(self, regions, n_kc):
        P = self.P
        slot = self.wslot
        self.wslot ^= 1
        off = 0
        views = []
        for ri, r in enumerate(regions):
            w = r.shape[-1]
            n = n_kc * w
            dst = self.wst[slot][:, off:off + n].rearrange("p (k w) -> p k w", w=w)
            P.dma(lambda e, dst=dst, r=r: e.dma_start(out=dst, in_=r), writes=[("wst", slot, ri)])
            views.append(self.wbf[slot][:, off:off + n].rearrange("p (k w) -> p k w", w=w))
            off += n
        assert off <= 4096
        P.act(lambda e, slot=slot, off=off: e.copy(out=self.wbf[slot][:, :off], in_=self.wst[slot][:, :off]),
              reads=[("wst", slot, i) for i in range(4)], writes=[("wbf", slot)])
        return views, ("wbf", slot)

    def linear_fm(self, Wv, n_kc, chunks, rhs_fn, n_tiles, evac, banks=(0, 4)):
        P = self.P
        maxcols = 4096 // n_kc
        i = 0
        while i < len(chunks):
            c0 = chunks[i][0]
            j = i
            while j + 1 < len(chunks) and chunks[j + 1][0] + chunks[j + 1][1] - c0 <= maxcols \
                    and chunks[j + 1][0] == chunks[j][0] + chunks[j][1]:
                j += 1
            c1 = chunks[j][0] + chunks[j][1]
            (wv,), wk = self.wload([Wv[:, :, c0:c1]], n_kc)
            for ci in range(i, j + 1):
                col0, width = chunks[ci]
                o = col0 - c0
                for tt in range(n_tiles):
                    b = self.mmbank(*banks)
                    ncol = None
                    for kc in range(n_kc):
                        rhs, rk = rhs_fn(kc, tt)
                        ncol = rhs.shape[-1]
                        P.pe(lambda e, b=b, kc=kc, o=o, width=width, rhs=rhs, ncol=ncol, wv=wv:
                             e.matmul(self.ps[b][:width, :ncol], lhsT=wv[:rhs.shape[0], kc, o:o + width], rhs=rhs,
                                      start=(kc == 0), stop=(kc == n_kc - 1)),
                             reads=[wk] + list(rk), writes=[("ps", b)])
                    evac(ci, tt, self.ps[b][:width, :ncol], ("ps", b))
            i = j + 1

    def consts(self, dr):
        P = self.P
        P.dve(lambda e: e.memset(self.ones_bf[:], 1.0), writes=[("ones",)])
        P.dve(lambda e: e.memset(self.ones_f[:], 1.0), writes=[("onesf",)])
        self.load(self.rotT[:], dr["rotT"], [("rotT",)])
        self.load(self.invf[:], dr["invf"], [("invf",)])
        self.load(self.ident[:], dr["ident"], [("ident",)])
        self.load(self.pidx[:], dr["pidx"], [("pidx",)])

    def load_hT(self, src):
        v = src.rearrange("(kc p) t -> p kc t", p=128)
        for kc in range(KC):
            self.load(self.hT[:, kc, :], v[:, kc, :], [("hT", kc, 0), ("hT", kc, 1)])

    def store_hT(self, dst):
        v = dst.rearrange("(kc p) t -> p kc t", p=128)
        for kc in range(KC):
            self.load(v[:, kc, :], self.hT[:, kc, :], [("dram", "h", id(dst), kc)], rkeys=[("hT", kc, 0), ("hT", kc, 1)], q="pool")

    def norm_adaln(self, Acol, Bcol, abkeys, want32=None):
        P = self.P
        for tt in range(2):
            cs = slice(tt * 512, (tt + 1) * 512)
            sb_ = 6 + tt
            for kc in range(KC):
                sq, sqk = self.tf()
                P.act(lambda e, sq=sq, kc=kc, cs=cs: e.activation(out=sq[:], in_=self.hT[:, kc, cs], func=AF.Square),
                      reads=[("hT", kc, tt)], writes=[sqk])
                P.pe(lambda e, sq=sq, kc=kc, sb_=sb_: e.matmul(self.ps[sb_][:], lhsT=self.ones_f[:], rhs=sq[:],
                                                              start=(kc == 0), stop=(kc == KC - 1)),
                     reads=[sqk, ("onesf",)], writes=[("ps", sb_)])
            rs, rsk = self.rs_tile()
            P.act(lambda e, rs=rs, sb_=sb_: e.activation(out=rs[:], in_=self.ps[sb_][:], func=AF.Sqrt, scale=1.0 / D, bias=self.small[:, 255:256]),
                  reads=[("ps", sb_), ("epscol",)], writes=[rsk])
            P.dve(lambda e, rs=rs: e.reciprocal(out=rs[:], in_=rs[:]), reads=[rsk], writes=[rsk])
            for kc in range(KC):
                t, tk = self.tf()
                P.dve(lambda e, t=t, kc=kc, cs=cs, rs=rs: e.tensor_tensor(out=t[:], in0=self.hT[:, kc, cs], in1=rs[:], op=ALU.mult),
                      reads=[("hT", kc, tt), rsk], writes=[tk])
                if want32 is None:
                    P.pool(lambda e, t=t, kc=kc, cs=cs: e.tensor_scalar(out=self.actT[:, kc, cs], in0=t[:], scalar1=Acol[:, kc:kc + 1],
                                                                        scalar2=Bcol[:, kc:kc + 1], op0=ALU.mult, op1=ALU.add),
                           reads=[tk] + list(abkeys), writes=[("actT", kc, tt)])
                else:
                    u32, uk = self.tf()
                    P.pool(lambda e, t=t, kc=kc, u32=u32: e.tensor_scalar(out=u32[:], in0=t[:], scalar1=Acol[:, kc:kc + 1],
                                                                          scalar2=Bcol[:, kc:kc + 1], op0=ALU.mult, op1=ALU.add),
                           reads=[tk] + list(abkeys), writes=[uk])
                    P.act(lambda e, u32=u32, kc=kc, cs=cs: e.copy(out=self.actT[:, kc, cs], in_=u32[:]), reads=[uk], writes=[("actT", kc, tt)])
                    want32(kc, tt, u32, uk)

    def eps_col(self):
        self.P.dve(lambda e: e.memset(self.small[:, 255:256], EPS), writes=[("epscol",)])

    def rhs_act(self, kc, tt):
        return self.actT[:, kc, tt * 512:(tt + 1) * 512], [("actT", kc, tt)]

    def compute_mod(self, dr):
        P = self.P
        ccol = self.small[:, 0:16]
        cs_ = self.small[:, 16:32]
        self.load(ccol, dr["ccol"], [("ccol",)])
        P.act(lambda e: e.activation(out=cs_, in_=ccol, func=AF.Silu), reads=[("ccol",)], writes=[("csil",)])
        cb = self.small_b[:, 0:16]
        P.dve(lambda e: e.tensor_copy(out=cb, in_=cs_), reads=[("csil",)], writes=[("cbf",)])
        adab = self.small[:, 32:128]
        self.load(adab, dr["adabT"], [("adab",)])
        Wv = dr["ada_w"].rearrange("(kc p) n -> p kc n", p=128)

        def rhs(kc, tt):
            return cb[:, kc:kc + 1], [("cbf",)]

        def evac(ci, tt, ps, pk):
            P.dve(lambda e, ci=ci, ps=ps: e.tensor_tensor(out=self.modT[:, ci:ci + 1], in0=ps, in1=adab[:, ci:ci + 1], op=ALU.add),
                  reads=[pk, ("adab",)], writes=[("modT", ci)])

        self.linear_fm(Wv, KC, [(j * 128, 128) for j in range(96)], rhs, 1, evac)

    def adaln_cols(self, gT_dram, which):
        P = self.P
        base = 0 if which == 0 else 48
        g = self.small[:, 128:144]
        A = self.small[:, 144:160]
        self.load(g, gT_dram, [("gcol",)])
        P.dve(lambda e: e.tensor_scalar(out=A, in0=self.modT[:, base + 16:base + 32], scalar1=1.0, scalar2=None, op0=ALU.add),
              reads=[("modT", j) for j in range(base + 16, base + 32)], writes=[("Acol",)])
        P.dve(lambda e: e.tensor_tensor(out=A, in0=A, in1=g, op=ALU.mult), reads=[("Acol",), ("gcol",)], writes=[("Acol",)])
        return A, self.modT[:, base:base + 16], [("Acol",)] + [("modT", j) for j in range(base, base + 16)]

    def rope_tables(self, dr, col, R):
        P = self.P
        for tt in range(2):
            cs = slice(tt * 512, (tt + 1) * 512)
            pi_, pik = self.tf()
            pint = pi_.bitcast(I32)
            self.load(pint[:64, :], dr["pos"][:, cs], [pik])
            for which, shift, dst in ((0, 0.0, self.sinT), (1, math.pi / 2, self.cosT)):
                a, ak = self.tf()
                P.dve(lambda e, a=a, pint=pint: e.tensor_copy(out=a[:R, :], in_=pint[:R, :]), reads=[pik], writes=[ak])
                P.dve(lambda e, a=a, shift=shift: e.tensor_scalar(out=a[:R, :], in0=a[:R, :], scalar1=self.invf[:R, col:col + 1], scalar2=shift,
                                                                  op0=ALU.mult, op1=ALU.add), reads=[ak, ("invf",)], writes=[ak])
                ki, kk = self.tf()
                P.dve(lambda e, a=a, ki=ki: e.tensor_scalar(out=ki[:R, :], in0=a[:R, :], scalar1=1.0 / TWO_PI, scalar2=None, op0=ALU.mult),
                      reads=[ak], writes=[kk])
                k2, k2k = self.tf()
                k2i = k2.bitcast(I32)
                P.dve(lambda e, ki=ki, k2i=k2i: e.tensor_copy(out=k2i[:R, :], in_=ki[:R, :]), reads=[kk], writes=[k2k])
                P.dve(lambda e, ki=ki, k2i=k2i: e.tensor_copy(out=ki[:R, :], in_=k2i[:R, :]), reads=[k2k], writes=[kk])
                P.dve(lambda e, a=a, ki=ki: e.scalar_tensor_tensor(out=a[:R, :], in0=ki[:R, :], scalar=-TWO_PI, in1=a[:R, :], op0=ALU.mult, op1=ALU.add),
                      reads=[ak, kk], writes=[ak])
                P.dve(lambda e, a=a, ki=ki: e.tensor_scalar(out=ki[:R, :], in0=a[:R, :], scalar1=math.pi, scalar2=-TWO_PI, op0=ALU.is_gt, op1=ALU.mult),
                      reads=[ak], writes=[kk])
                P.dve(lambda e, a=a, ki=ki: e.tensor_tensor(out=a[:R, :], in0=a[:R, :], in1=ki[:R, :], op=ALU.add), reads=[ak, kk], writes=[ak])
                P.dve(lambda e, a=a, ki=ki: e.tensor_scalar(out=ki[:R, :], in0=a[:R, :], scalar1=-math.pi, scalar2=TWO_PI, op0=ALU.is_lt, op1=ALU.mult),
                      reads=[ak], writes=[kk])
                P.dve(lambda e, a=a, ki=ki: e.tensor_tensor(out=a[:R, :], in0=a[:R, :], in1=ki[:R, :], op=ALU.add), reads=[ak, kk], writes=[ak])
                P.dve(lambda e, a=a: e.tensor_scalar(out=a[:R, :], in0=a[:R, :], scalar1=math.pi, scalar2=-math.pi, op0=ALU.min, op1=ALU.max),
                      reads=[ak], writes=[ak])
                P.act(lambda e, a=a, dst=dst, cs=cs: e.activation(out=dst[:R, cs], in_=a[:R, :], func=AF.Sin), reads=[ak],
                      writes=[("trig", which, tt)])

    def rope_apply(self, xb, xk, R, rot_lo, tt):
        P = self.P
        cs = slice(tt * 512, (tt + 1) * 512)
        b = 4 + (self.mmb % 2)
        self.mmb += 1
        P.pe(lambda e, xb=xb, b=b: e.matmul(self.ps[b][:R, :], lhsT=self.rotT[:R, rot_lo:rot_lo + R], rhs=xb[:R, :], start=True, stop=True),
             reads=[xk, ("rotT",)], writes=[("ps", b)])
        t1, t1k = self.tf()
        t2, t2k = self.tf()
        P.dve(lambda e, xb=xb, t1=t1, cs=cs: e.tensor_tensor(out=t1[:R, :], in0=xb[:R, :], in1=self.cosT[:R, cs], op=ALU.mult),
              reads=[xk, ("trig", 1, tt)], writes=[t1k])
        P.dve(lambda e, t2=t2, b=b, cs=cs: e.tensor_tensor(out=t2[:R, :], in0=self.ps[b][:R, :], in1=self.sinT[:R, cs], op=ALU.mult),
              reads=[("ps", b), ("trig", 0, tt)], writes=[t2k])
        P.pool(lambda e, xb=xb, t1=t1, t2=t2: e.tensor_tensor(out=xb[:R, :], in0=t1[:R, :], in1=t2[:R, :], op=ALU.add),
               reads=[t1k, t2k], writes=[xk])

    def phaseA_diff(self, dr):
        P = self.P
        self.eps_col()
        self.load_hT(dr["hT_in"])
        self.rope_tables(dr, 0, 32)
        if dr.get("ada_w") is not None:
            self.compute_mod(dr)
        if dr.get("modT_out") is not None:
            self.load(dr["modT_out"], self.modT[:], [("dram", "modT")], rkeys=[("modT", j) for j in range(96)], q="pool")
        A, Bc, abk = self.adaln_cols(dr["gmixT"], 0)
        self.norm_adaln(A, Bc, abk)
        Wv = dr["w_in"].rearrange("(kc p) n -> p kc n", p=128)
        qT = dr["qT"]
        kT = dr["kT"]

        def evac(ci, tt, ps, pk):
            xb, xk = self.tb()
            P.act(lambda e, xb=xb, ps=ps: e.copy(out=xb[:], in_=ps), reads=[pk], writes=[xk])
            self.rope_apply(xb, xk, 32, 0, tt)
            dst = (qT if ci < 16 else kT)[ci % 16, :, tt * 512:(tt + 1) * 512]
            self.load(dst, xb[:], [("dram", "qk", ci, tt)], rkeys=[xk], q="pool")

        self.linear_fm(Wv, KC, [(j * 128, 128) for j in range(32)], evac=evac, rhs_fn=self.rhs_act, n_tiles=2)
        self.v_token_major(Wv, 4096, dr["V"], KC, lambda kc, tb_: (self.actT[:, kc, tb_ * 128:(tb_ + 1) * 128], [("actT", kc, tb_ // 4)]))

    def v_token_major(self, Wv, col_base, Vd, n_kc, lhs_fn, ncols=2048, col_list=None):
        P = self.P
        maxcols = 4096 // n_kc
        if col_list is None:
            col_list = [(col_base + g * 256, 256, g * 256) for g in range(ncols // 256)]
        for (c0, w, d0) in col_list:
            (wv,), wk = self.wload([Wv[:, :, c0:c0 + w]], n_kc)
            for tb_ in range(8):
                b = self.mmbank(0, 4)
                for kc in range(n_kc):
                    lhs, lk = lhs_fn(kc, tb_)
                    P.pe(lambda e, b=b, kc=kc, lhs=lhs, wv=wv, w=w: e.matmul(self.ps[b][:, :w], lhsT=lhs, rhs=wv[:, kc, :],
                                                                            start=(kc == 0), stop=(kc == n_kc - 1)),
                         reads=[wk] + list(lk), writes=[("ps", b)])
                vt, vk = self.tb()
                P.dve(lambda e, vt=vt, b=b, w=w: e.tensor_copy(out=vt[:, :w], in_=self.ps[b][:, :w]), reads=[("ps", b)], writes=[vk])
                self.load(Vd[tb_ * 128:(tb_ + 1) * 128, d0:d0 + w], vt[:, :w], [("dram", "V", d0, tb_)], rkeys=[vk], q="pool")

    def phaseA_mla(self, dr):
        P = self.P
        self.eps_col()
        self.load_hT(dr["hT_in"])
        self.rope_tables(dr, 1, 64)
        if dr.get("ada_w") is not None:
            self.compute_mod(dr)
        if dr.get("modT_out") is not None:
            self.load(dr["modT_out"], self.modT[:], [("dram", "modT")], rkeys=[("modT", j) for j in range(96)], q="pool")
        A, Bc, abk = self.adaln_cols(dr["gmixT"], 0)
        self.norm_adaln(A, Bc, abk)
        Wv = dr["w_in"].rearrange("(kc p) n -> p kc n", p=128)
        krT = dr["krT"]
        qg = self.small[:, 160:164]
        kvg = self.small[:, 164:168]
        self.load(qg, dr["qgT"], [("qg",)])
        self.load(kvg, dr["kvgT"], [("kvg",)])
        latf = self.hT

        def evac_lat(ci, tt, ps, pk):
            cs = slice(tt * 512, (tt + 1) * 512)
            if ci < 8:
                P.dve(lambda e, ci=ci, cs=cs, ps=ps: e.tensor_copy(out=latf[:, ci, cs], in_=ps), reads=[pk], writes=[("hT", ci, tt)])
            else:
                xb, xk = self.tb()
                P.act(lambda e, xb=xb, ps=ps: e.copy(out=xb[:64, :], in_=ps), reads=[pk], writes=[xk])
                self.rope_apply(xb, xk, 64, 32, tt)
                self.load(krT[:, cs], xb[:64, :], [("dram", "kr", tt)], rkeys=[xk], q="pool")

        self.linear_fm(Wv, KC, [(j * 128, 128) for j in range(8)] + [(1024, 64)], evac=evac_lat, rhs_fn=self.rhs_act, n_tiles=2)
        latn = self.arena[:, 0:8192].rearrange("p (c t) -> p c t", t=T)
        for grp, gcol, gk in ((0, qg, ("qg",)), (1, kvg, ("kvg",))):
            for tt in range(2):
                cs = slice(tt * 512, (tt + 1) * 512)
                sb_ = 6 + tt
                for c in range(4):
                    ci = grp * 4 + c
                    sq, sqk = self.tb()
                    P.act(lambda e, sq=sq, ci=ci, cs=cs: e.activation(out=sq[:], in_=latf[:, ci, cs], func=AF.Square), reads=[("hT", ci, tt)], writes=[sqk])
                    P.pe(lambda e, sq=sq, c=c, sb_=sb_: e.matmul(self.ps[sb_][:], lhsT=self.ones_bf[:], rhs=sq[:], start=(c == 0), stop=(c == 3)),
                         reads=[sqk, ("ones",)], writes=[("ps", sb_)])
                rs, rsk = self.rs_tile()
                P.act(lambda e, rs=rs, sb_=sb_: e.activation(out=rs[:], in_=self.ps[sb_][:], func=AF.Sqrt, scale=1.0 / 512, bias=self.small[:, 255:256]),
                      reads=[("ps", sb_), ("epscol",)], writes=[rsk])
                P.dve(lambda e, rs=rs: e.reciprocal(out=rs[:], in_=rs[:]), reads=[rsk], writes=[rsk])
                for c in range(4):
                    ci = grp * 4 + c
                    t, tk = self.tf()
                    P.dve(lambda e, t=t, ci=ci, cs=cs, rs=rs: e.tensor_tensor(out=t[:], in0=latf[:, ci, cs], in1=rs[:], op=ALU.mult),
                          reads=[("hT", ci, tt), rsk], writes=[tk])
                    P.pool(lambda e, t=t, ci=ci, c=c, cs=cs, gcol=gcol: e.tensor_scalar(out=latn[:, ci, cs], in0=t[:], scalar1=gcol[:, c:c + 1], scalar2=None, op0=ALU.mult),
                           reads=[tk, gk], writes=[("latn", ci, tt)])
        Wq = dr["w_uq"].rearrange("(kc p) n -> p kc n", p=128)
        chunks = []
        for h in range(16):
            chunks.append((h * 192, 128))
            chunks.append((h * 192 + 128, 64))
        qnT, qrT, knT = dr["qnT"], dr["qrT"], dr["knT"]

        def rhs_q(kc, tt):
            return latn[:, kc, tt * 512:(tt + 1) * 512], [("latn", kc, tt)]

        def evac_q(ci, tt, ps, pk):
            h, isr = ci // 2, ci % 2
            cs = slice(tt * 512, (tt + 1) * 512)
            xb, xk = self.tb()
            if not isr:
                P.act(lambda e, xb=xb, ps=ps: e.copy(out=xb[:], in_=ps), reads=[pk], writes=[xk])
                self.load(qnT[h, :, cs], xb[:], [("dram", "qn", h, tt)], rkeys=[xk], q="pool")
            else:
                P.act(lambda e, xb=xb, ps=ps: e.copy(out=xb[:64, :], in_=ps), reads=[pk], writes=[xk])
                self.rope_apply(xb, xk, 64, 32, tt)
                self.load(qrT[h, :, cs], xb[:64, :], [("dram", "qr", h, tt)], rkeys=[xk], q="pool")

        self.linear_fm(Wq, 4, chunks, evac=evac_q, rhs_fn=rhs_q, n_tiles=2)
        Wkv = dr["w_ukv"].rearrange("(kc p) n -> p kc n", p=128)

        def rhs_kv(kc, tt):
            return latn[:, 4 + kc, tt * 512:(tt + 1) * 512], [("latn", 4 + kc, tt)]

        def evac_k(ci, tt, ps, pk):
            cs = slice(tt * 512, (tt + 1) * 512)
            xb, xk = self.tb()
            P.act(lambda e, xb=xb, ps=ps: e.copy(out=xb[:], in_=ps), reads=[pk], writes=[xk])
            self.load(knT[ci, :, cs], xb[:], [("dram", "kn", ci, tt)], rkeys=[xk], q="pool")

        self.linear_fm(Wkv, 4, [(h * 256, 128) for h in range(16)], evac=evac_k, rhs_fn=rhs_kv, n_tiles=2)
        self.v_token_major(Wkv, 0, dr["V"], 4,
                           lambda kc, tb_: (latn[:, 4 + kc, tb_ * 128:(tb_ + 1) * 128], [("latn", 4 + kc, tb_ // 4)]),
                           col_list=[(h * 256 + 128, 128, h * 128) for h in range(16)])

    def kc_list(self, qt, prev):
        out = []
        if prev:
            for kc in range(8):
                out.append((kc, 0, True, None))
        for oi in range(4 * qt + 4):
            r = oi - 4 * qt
            if r < 0:
                out.append((8 + oi, 0, False, None))
            else:
                out.append((8 + oi, 128 * r, False, r))
        return out

    def attn_scores_pv(self, qt, prev, bias_ap, bias_k, scale, s_mm, pv_list, den_bank):
        P = self.P
        cq = qt * 512
        lst = self.kc_list(qt, prev)
        for idx, (kc, col0, isp, r) in enumerate(lst):
            sbk = self.mmbank(0, 2)
            s_mm(kc, cq + col0, col0, sbk)
            pT, pk = self.tb()
            if isp and bias_ap is not None:
                P.act(lambda e, pT=pT, sbk=sbk, col0=col0: e.activation(out=pT[:, col0:], in_=self.ps[sbk][:, col0:], func=AF.Exp, scale=scale, bias=bias_ap),
                      reads=[("ps", sbk)] + list(bias_k), writes=[pk])
            else:
                P.act(lambda e, pT=pT, sbk=sbk, col0=col0: e.activation(out=pT[:, col0:], in_=self.ps[sbk][:, col0:], func=AF.Exp, scale=scale),
                      reads=[("ps", sbk)], writes=[pk])
            if r is not None:
                P.pool(lambda e, pT=pT, col0=col0: e.memset(pT[64:128, col0:col0 + 64], 0.0), reads=[pk], writes=[pk])
            first = idx == 0
            last = idx == len(lst) - 1
            for (bank, lfn) in pv_list:
                lhs, lk = lfn(kc)
                P.pe(lambda e, bank=bank, lhs=lhs, pT=pT, col0=col0, first=first, last=last:
                     e.matmul(self.ps[bank][:, col0:], lhsT=lhs, rhs=pT[:, col0:], start=first, stop=last),
                     reads=[pk] + list(lk), writes=[("ps", bank)])
            P.pe(lambda e, pT=pT, col0=col0, first=first, last=last:
                 e.matmul(self.ps[den_bank][:, col0:], lhsT=self.ones_bf[:], rhs=pT[:, col0:], start=first, stop=last),
                 reads=[pk, ("ones",)], writes=[("ps", den_bank)])

    def attn_diff(self, dr, prev, lam_init):
        P = self.P
        ar = self.arena
        q_sb = ar[:, 0:2048].rearrange("p (c t) -> p c t", t=T)
        k_sb = ar[:, 2048:6144].rearrange("p (c t) -> p c t", t=2 * T)
        v_sb = ar[:, 6144:10240].rearrange("p (k d) -> p k d", d=256)
        qT, kT, V = dr["qT"], dr["kT"], dr["V"]
        lamT = self.small[:, 168:172]
        self.load(lamT, dr["lamT"], [("lamT",)])
        prod = self.small[:, 172:174]
        P.dve(lambda e: e.tensor_tensor(out=prod[:, 0:1], in0=lamT[:, 0:1], in1=lamT[:, 1:2], op=ALU.mult), reads=[("lamT",)], writes=[("lprod", 0)])
        P.dve(lambda e: e.tensor_tensor(out=prod[:, 1:2], in0=lamT[:, 2:3], in1=lamT[:, 3:4], op=ALU.mult), reads=[("lamT",)], writes=[("lprod", 1)])
        of, ofk = self.tf()
        P.dve(lambda e, of=of: e.memset(of[:, :128], 1.0), writes=[ofk])
        P.pe(lambda e, of=of: e.matmul(self.ps[7][:, 0:2], lhsT=of[:, :128], rhs=prod, start=True, stop=True),
             reads=[ofk, ("lprod", 0), ("lprod", 1)], writes=[("ps", 7)])
        ee = self.small[:, 174:176]
        P.act(lambda e: e.activation(out=ee, in_=self.ps[7][:, 0:2], func=AF.Exp), reads=[("ps", 7)], writes=[("lexp",)])
        nlam = self.small[:, 176:177]
        P.dve(lambda e: e.tensor_tensor(out=nlam, in0=ee[:, 1:2], in1=ee[:, 0:1], op=ALU.subtract), reads=[("lexp",)], writes=[("nlam",)])
        P.dve(lambda e: e.tensor_scalar(out=nlam, in0=nlam, scalar1=-lam_init, scalar2=None, op0=ALU.add), reads=[("nlam",)], writes=[("nlam",)])
        sg = self.small[:, 177:179]
        self.load(sg, dr["sgT"], [("sg",)])
        P.dve(lambda e: e.tensor_scalar(out=sg, in0=sg, scalar1=(1.0 - lam_init), scalar2=None, op0=ALU.mult), reads=[("sg",)], writes=[("sg",)])
        scale = 128 ** -0.5
        bias_ap = self.small[:, 179:180] if prev == "data" else None
        if prev == "data":
            self.load(bias_ap, dr["pbias"], [("pbias",)])
        has_prev = prev in ("data", "static")
        it = 0
        for h in range(8):
            for c in range(2):
                self.load(q_sb[:, c, :], qT[2 * h + c], [("ar", "q", c)])
                if has_prev:
                    self.load(k_sb[:, c, 0:T], dr["kT_prev"][2 * h + c], [("ar", "kp", c)])
                self.load(k_sb[:, c, T:2 * T], kT[2 * h + c], [("ar", "k", c)])
            if has_prev:
                self.load(v_sb[:, 0:8, :], dr["V_prev"][:, h * 256:(h + 1) * 256].rearrange("(k p) d -> p k d", p=128), [("ar", "vp")])
            self.load(v_sb[:, 8:16, :], V[:, h * 256:(h + 1) * 256].rearrange("(k p) d -> p k d", p=128), [("ar", "v")])
            for qt in range(2):
                on = []
                for c in range(2):
                    banks = (2, 3, 4) if it % 2 == 0 else (5, 6, 7)
                    it += 1

                    def s_mm(kc, qcol, col0, sbk, c=c):
                        isp = kc < 8
                        P.pe(lambda e: e.matmul(self.ps[sbk][:, col0:], lhsT=k_sb[:, c, kc * 128:(kc + 1) * 128], rhs=q_sb[:, c, qcol:(qcol - col0) + 512],
                                                start=True, stop=True),
                             reads=[("ar", "kp" if isp else "k", c), ("ar", "q", c)], writes=[("ps", sbk)])

                    pv = [(banks[d], (lambda kc, d=d: (v_sb[:, kc, d * 128:(d + 1) * 128], [("ar", "vp" if kc < 8 else "v")]))) for d in range(2)]
                    self.attn_scores_pv(qt, has_prev, bias_ap, [("pbias",)], scale, s_mm, pv, banks[2])
                    rd, rdk = self.tf()
                    P.dve(lambda e, rd=rd, banks=banks: e.reciprocal(out=rd[:], in_=self.ps[banks[2]][:]), reads=[("ps", banks[2])], writes=[rdk])
                    for d in range(2):
                        o, ok = self.tf()
                        P.dve(lambda e, o=o, rd=rd, banks=banks, d=d: e.tensor_tensor(out=o[:], in0=self.ps[banks[d]][:], in1=rd[:], op=ALU.mult),
                              reads=[("ps", banks[d]), rdk], writes=[ok])
                        on.append((o, ok))
                os_ = []
                for d in range(2):
                    (o0, k0), (o1, k1) = on[d], on[2 + d]
                    P.dve(lambda e, o0=o0, o1=o1: e.scalar_tensor_tensor(out=o0[:], in0=o1[:], scalar=nlam, in1=o0[:], op0=ALU.mult, op1=ALU.add),
                           reads=[k0, k1, ("nlam",)], writes=[k0])
                    os_.append((o0, k0))
                for d in range(2):
                    sq, sqk = self.tb()
                    P.act(lambda e, sq=sq, o=os_[d][0]: e.activation(out=sq[:], in_=o[:], func=AF.Square), reads=[os_[d][1]], writes=[sqk])
                    P.pe(lambda e, sq=sq, d=d: e.matmul(self.ps[0][:], lhsT=self.ones_bf[:], rhs=sq[:], start=(d == 0), stop=(d == 1)),
                         reads=[sqk, ("ones",)], writes=[("ps", 0)])
                rs, rsk = self.rs_tile()
                P.act(lambda e, rs=rs: e.activation(out=rs[:], in_=self.ps[0][:], func=AF.Sqrt, scale=1.0 / 256, bias=self.small[:, 255:256]),
                      reads=[("ps", 0), ("epscol",)], writes=[rsk])
                P.dve(lambda e, rs=rs: e.reciprocal(out=rs[:], in_=rs[:]), reads=[rsk], writes=[rsk])
                for d in range(2):
                    o, ok = os_[d]
                    P.dve(lambda e, o=o, rs=rs: e.tensor_tensor(out=o[:], in0=o[:], in1=rs[:], op=ALU.mult), reads=[ok, rsk], writes=[ok])
                    P.pool(lambda e, o=o, d=d, h=h, qt=qt: e.tensor_scalar(out=self.actT[:, 2 * h + d, qt * 512:(qt + 1) * 512], in0=o[:],
                                                                            scalar1=sg[:, d:d + 1], scalar2=None, op0=ALU.mult),
                           reads=[ok, ("sg",)], writes=[("actT", 2 * h + d, qt)])

    def attn_mla(self, dr, prev):
        P = self.P
        ar = self.arena
        qn_sb = ar[:, 0:1024]
        qr_sb = ar[:, 1024:2048]
        kn_sb = ar[:, 2048:4096]
        kr_sb = ar[:, 4096:6144]
        v_sb = ar[:, 6144:8192].rearrange("p (k d) -> p k d", d=128)
        scale = 192 ** -0.5
        bias_ap = self.small[:, 179:180] if prev == "data" else None
        if prev == "data":
            self.load(bias_ap, dr["pbias"], [("pbias",)])
        has_prev = prev in ("data", "static")
        if has_prev:
            self.load(kr_sb[:64, 0:T], dr["krT_prev"], [("ar", "krp")])
        self.load(kr_sb[:64, T:2 * T], dr["krT"], [("ar", "kr")])
        it = 0
        for h in range(16):
            self.load(qn_sb, dr["qnT"][h], [("ar", "qn")])
            self.load(qr_sb[:64, :], dr["qrT"][h], [("ar", "qr")])
            if has_prev:
                self.load(kn_sb[:, 0:T], dr["knT_prev"][h], [("ar", "knp")])
                self.load(v_sb[:, 0:8, :], dr["V_prev"][:, h * 128:(h + 1) * 128].rearrange("(k p) d -> p k d", p=128), [("ar", "vp")])
            self.load(kn_sb[:, T:2 * T], dr["knT"][h], [("ar", "kn")])
            self.load(v_sb[:, 8:16, :], dr["V"][:, h * 128:(h + 1) * 128].rearrange("(k p) d -> p k d", p=128), [("ar", "v")])
            for qt in range(2):
                banks = (2, 4) if it % 2 == 0 else (5, 7)
                it += 1

                def s_mm(kc, qcol, col0, sbk):
                    isp = kc < 8
                    P.pe(lambda e: e.matmul(self.ps[sbk][:, col0:], lhsT=kn_sb[:, kc * 128:(kc + 1) * 128], rhs=qn_sb[:, qcol:(qcol - col0) + 512],
                                            start=True, stop=False),
                         reads=[("ar", "knp" if isp else "kn"), ("ar", "qn")], writes=[("ps", sbk)])
                    P.pe(lambda e: e.matmul(self.ps[sbk][:, col0:], lhsT=kr_sb[:64, kc * 128:(kc + 1) * 128], rhs=qr_sb[:64, qcol:(qcol - col0) + 512],
                                            start=False, stop=True),
                         reads=[("ar", "krp" if isp else "kr"), ("ar", "qr")], writes=[("ps", sbk)])

                pv = [(banks[0], (lambda kc: (v_sb[:, kc, :], [("ar", "vp" if kc < 8 else "v")])))]
                self.attn_scores_pv(qt, has_prev, bias_ap, [("pbias",)], scale, s_mm, pv, banks[1])
                rd, rdk = self.tf()
                P.dve(lambda e, rd=rd, banks=banks: e.reciprocal(out=rd[:], in_=self.ps[banks[1]][:]), reads=[("ps", banks[1])], writes=[rdk])
                P.dve(lambda e, rd=rd, banks=banks, h=h, qt=qt: e.tensor_tensor(out=self.actT[:, h, qt * 512:(qt + 1) * 512], in0=self.ps[banks[0]][:], in1=rd[:], op=ALU.mult),
                      reads=[("ps", banks[0]), rdk], writes=[("actT", h, qt)])

    def out_proj(self, w_out):
        P = self.P
        Wv = w_out.rearrange("(kc p) n -> p kc n", p=128)

        def evac(ci, tt, ps, pk):
            cs = slice(tt * 512, (tt + 1) * 512)
            P.dve(lambda e, ci=ci, cs=cs, ps=ps: e.scalar_tensor_tensor(out=self.hT[:, ci, cs], in0=ps, scalar=self.modT[:, 32 + ci:33 + ci],
                                                                        in1=self.hT[:, ci, cs], op0=ALU.mult, op1=ALU.add),
                  reads=[pk, ("modT", 32 + ci), ("hT", ci, tt)], writes=[("hT", ci, tt)])

        self.linear_fm(Wv, KC, [(j * 128, 128) for j in range(16)], evac=evac, rhs_fn=self.rhs_act, n_tiles=2)

    def moe(self, dr, n_exp=NE):
        P = self.P
        self.load(self.bgu[:], dr["bguT"], [("bgu",)])
        self.load(self.wr[:], dr["wrT"], [("wr",)])
        self.load(self.brr[:], dr["brr"], [("brr",)])
        bgu3 = self.bgu[:].rearrange("p (e j) -> p e j", j=16)
        P.dve(lambda e: e.tensor_scalar(out=bgu3[:, :, 8:16], in0=bgu3[:, :, 8:16], scalar1=1.0, scalar2=None, op0=ALU.add),
              reads=[("bgu",)], writes=[("bgu",)])
        wr3 = self.wr[:].rearrange("p (k e) -> p k e", e=NE)
        A, Bc, abk = self.adaln_cols(dr["gffnT"], 1)
        LB = 5

        def want32(kc, tt, u32, uk):
            for tb4 in range(4):
                tb_ = tt * 4 + tb4
                P.pe(lambda e, u32=u32, kc=kc, tb4=tb4, tb_=tb_: e.matmul(self.ps[LB][:, tb_ * 32:(tb_ + 1) * 32], lhsT=u32[:, tb4 * 128:(tb4 + 1) * 128],
                                                                          rhs=wr3[:, kc, :], start=(kc == 0 and tb_ == 0), stop=(kc == KC - 1), skip_group_check=True),
                     reads=[uk, ("wr",)], writes=[("ps", LB)])

        self.norm_adaln(A, Bc, abk, want32=want32)
        rt = self.rt
        for tb_ in range(8):
            lg = rt[:, 0:32]
            P.dve(lambda e, tb_=tb_: e.tensor_tensor(out=lg, in0=self.ps[LB][:, tb_ * 32:(tb_ + 1) * 32], in1=self.brr[:], op=ALU.add),
                  reads=[("ps", LB), ("brr",)], writes=[("rt", "lg")])
            m8 = rt[:, 32:40]
            P.dve(lambda e: e.max(out=m8, in_=lg), reads=[("rt", "lg")], writes=[("rt", "m8")])
            mk, mkk = self.tf()
            P.dve(lambda e, mk=mk: e.tensor_scalar(out=mk[:, 0:32], in0=lg, scalar1=m8[:, 3:4], scalar2=None, op0=ALU.is_ge),
                  reads=[("rt", "lg"), ("rt", "m8")], writes=[mkk])
            nm = rt[:, 40:41]
            P.dve(lambda e: e.tensor_scalar(out=nm, in0=m8[:, 0:1], scalar1=-1.0, scalar2=None, op0=ALU.mult), reads=[("rt", "m8")], writes=[("rt", "nm")])
            ex, exk = self.tf()
            P.act(lambda e, ex=ex: e.activation(out=ex[:, 0:32], in_=lg, func=AF.Exp, bias=nm, scale=1.0), reads=[("rt", "lg"), ("rt", "nm")], writes=[exk])
            P.dve(lambda e, ex=ex, mk=mk: e.tensor_tensor(out=ex[:, 0:32], in0=ex[:, 0:32], in1=mk[:, 0:32], op=ALU.mult), reads=[exk, mkk], writes=[exk])
            dn = rt[:, 41:42]
            P.dve(lambda e, ex=ex: e.reduce_sum(out=dn, in_=ex[:, 0:32], axis=AX.X), reads=[exk], writes=[("rt", "dn")])
            P.dve(lambda e: e.reciprocal(out=dn, in_=dn), reads=[("rt", "dn")], writes=[("rt", "dn")])
            P.dve(lambda e, ex=ex: e.tensor_scalar(out=ex[:, 0:32], in0=ex[:, 0:32], scalar1=dn, scalar2=None, op0=ALU.mult), reads=[exk, ("rt", "dn")], writes=[exk])
            P.pe(lambda e, ex=ex, tb_=tb_: e.transpose(out=self.ps[7][:32, (tb_ % 4) * 128:(tb_ % 4 + 1) * 128], in_=ex[:, 0:32], identity=self.ident[:]),
                 reads=[exk, ("ident",)], writes=[("ps", 7)])
            P.dve(lambda e, tb_=tb_: e.tensor_copy(out=self.GT[:, tb_ * 128:(tb_ + 1) * 128], in_=self.ps[7][:32, (tb_ % 4) * 128:(tb_ % 4 + 1) * 128]),
                  reads=[("ps", 7)], writes=[("GT", tb_ // 4)])
        Ap = self.arena[:, 0:8192].rearrange("p (f t) -> p f t", t=T)
        for ex_i in range(n_exp):
            P.dve(lambda e, ex_i=ex_i: e.tensor_scalar(out=self.sel[:], in0=self.pidx[:], scalar1=float(ex_i), scalar2=None, op0=ALU.is_equal),
                  reads=[("pidx",)], writes=[("sel",)])
            gbs = []
            for tt in range(2):
                P.pe(lambda e, tt=tt: e.matmul(self.ps[6 + tt][:], lhsT=self.sel[:], rhs=self.GT[:, tt * 512:(tt + 1) * 512], start=True, stop=True),
                     reads=[("sel",), ("GT", tt)], writes=[("ps", 6 + tt)])
                gb = self.gb[ex_i % 2][:, tt * 512:(tt + 1) * 512]
                gbk = ("gb", ex_i % 2, tt)
                P.act(lambda e, gb=gb, tt=tt: e.copy(out=gb, in_=self.ps[6 + tt][:]), reads=[("ps", 6 + tt)], writes=[gbk])
                gbs.append((gb, gbk))
            Wgu = dr["w_gu"][ex_i].rearrange("(kc p) n -> p kc n", p=128)
            for fc in range(8):
                (wg, wu), wk = self.wload([Wgu[:, :, fc * 128:(fc + 1) * 128], Wgu[:, :, 1024 + fc * 128:1024 + (fc + 1) * 128]], KC)
                for tt in range(2):
                    cs = slice(tt * 512, (tt + 1) * 512)
                    bg = 0 + 2 * (self.mmb % 2)
                    bu = bg + 1
                    self.mmb += 1
                    for kc in range(KC):
                        P.pe(lambda e, bg=bg, kc=kc, wg=wg, cs=cs: e.matmul(self.ps[bg][:], lhsT=wg[:, kc, :], rhs=self.actT[:, kc, cs], start=(kc == 0), stop=(kc == KC - 1)),
                             reads=[wk, ("actT", kc, tt)], writes=[("ps", bg)])
                    for kc in range(KC):
                        P.pe(lambda e, bu=bu, kc=kc, wu=wu, cs=cs: e.matmul(self.ps[bu][:], lhsT=wu[:, kc, :], rhs=self.actT[:, kc, cs], start=(kc == 0), stop=(kc == KC - 1)),
                             reads=[wk, ("actT", kc, tt)], writes=[("ps", bu)])
                    gc, gck = self.tf()
                    P.dve(lambda e, gc=gc, bg=bg, ex_i=ex_i, fc=fc: e.tensor_scalar(out=gc[:], in0=self.ps[bg][:], scalar1=bgu3[:, ex_i, fc:fc + 1], scalar2=7.0, op0=ALU.add, op1=ALU.min),
                          reads=[("ps", bg), ("bgu",)], writes=[gck])
                    sg_, sgk = self.tf()
                    P.act(lambda e, gc=gc, sg_=sg_: e.activation(out=sg_[:], in_=gc[:], func=AF.Sigmoid, scale=1.702), reads=[gck], writes=[sgk])
                    uc, uck = self.tf()
                    P.dve(lambda e, uc=uc, bu=bu, ex_i=ex_i, fc=fc: e.tensor_scalar(out=uc[:], in0=self.ps[bu][:], scalar1=bgu3[:, ex_i, 8 + fc:9 + fc], scalar2=-6.0, op0=ALU.add, op1=ALU.max),
                          reads=[("ps", bu), ("bgu",)], writes=[uck])
                    P.pool(lambda e, gc=gc, sg_=sg_: e.tensor_tensor(out=gc[:], in0=gc[:], in1=sg_[:], op=ALU.mult), reads=[gck, sgk], writes=[gck])
                    P.dve(lambda e, gc=gc, uc=uc: e.scalar_tensor_tensor(out=uc[:], in0=uc[:], scalar=8.0, in1=gc[:], op0=ALU.min, op1=ALU.mult), reads=[gck, uck], writes=[uck])
                    gb, gbk = gbs[tt]
                    P.pool(lambda e, uc=uc, gb=gb, fc=fc, cs=cs: e.tensor_tensor(out=Ap[:, fc, cs], in0=uc[:], in1=gb, op=ALU.mult), reads=[uck, gbk], writes=[("Ap", fc, tt)])
            Wd = dr["w_down"][ex_i].rearrange("(kc p) n -> p kc n", p=128)
            for dp in range(4):
                (wd,), wk = self.wload([Wd[:, :, dp * 512:(dp + 1) * 512]], 8)
                for dci in range(4):
                    dc = dp * 4 + dci
                    for tt in range(2):
                        cs = slice(tt * 512, (tt + 1) * 512)
                        b = 4 + (self.mmb % 2)
                        self.mmb += 1
                        for fk in range(8):
                            P.pe(lambda e, b=b, fk=fk, wd=wd, dci=dci, cs=cs: e.matmul(self.ps[b][:], lhsT=wd[:, fk, dci * 128:(dci + 1) * 128], rhs=Ap[:, fk, cs], start=(fk == 0), stop=(fk == 7)),
                                 reads=[wk, ("Ap", fk, tt)], writes=[("ps", b)])
                        P.dve(lambda e, b=b, dc=dc, cs=cs: e.scalar_tensor_tensor(out=self.hT[:, dc, cs], in0=self.ps[b][:], scalar=self.modT[:, 80 + dc:81 + dc], in1=self.hT[:, dc, cs],
                                                                                 op0=ALU.mult, op1=ALU.add),
                              reads=[("ps", b), ("modT", 80 + dc), ("hT", dc, tt)], writes=[("hT", dc, tt)])
        bdv = self.wst[0][:32, 0:D]
        self.load(bdv, dr["bd"], [("wst", 0, i) for i in range(4)])
        for dc in range(KC):
            for tt in range(2):
                cs = slice(tt * 512, (tt + 1) * 512)
                b = 4 + (self.mmb % 2)
                self.mmb += 1
                P.pe(lambda e, b=b, dc=dc, cs=cs: e.matmul(self.ps[b][:], lhsT=bdv[:, dc * 128:(dc + 1) * 128], rhs=self.GT[:, cs], start=True, stop=True),
                     reads=[("wst", 0, 0), ("GT", tt)], writes=[("ps", b)])
                P.dve(lambda e, b=b, dc=dc, cs=cs: e.scalar_tensor_tensor(out=self.hT[:, dc, cs], in0=self.ps[b][:], scalar=self.modT[:, 80 + dc:81 + dc], in1=self.hT[:, dc, cs],
                                                                         op0=ALU.mult, op1=ALU.add),
                      reads=[("ps", b), ("modT", 80 + dc), ("hT", dc, tt)], writes=[("hT", dc, tt)])

    def final_norm(self, dr):
        P = self.P
        g = self.small[:, 128:144]
        self.load(g, dr["gfinT"], [("gcol",)])
        outv = dr["outT"].rearrange("(kc p) t -> p kc t", p=128)
        for tt in range(2):
            cs = slice(tt * 512, (tt + 1) * 512)
            sb_ = 6 + tt
            for kc in range(KC):
                sq, sqk = self.tb()
                P.act(lambda e, sq=sq, kc=kc, cs=cs: e.activation(out=sq[:], in_=self.hT[:, kc, cs], func=AF.Square), reads=[("hT", kc, tt)], writes=[sqk])
                P.pe(lambda e, sq=sq, kc=kc, sb_=sb_: e.matmul(self.ps[sb_][:], lhsT=self.ones_bf[:], rhs=sq[:], start=(kc == 0), stop=(kc == KC - 1)),
                     reads=[sqk, ("ones",)], writes=[("ps", sb_)])
            rs, rsk = self.rs_tile()
            P.act(lambda e, rs=rs, sb_=sb_: e.activation(out=rs[:], in_=self.ps[sb_][:], func=AF.Sqrt, scale=1.0 / D, bias=self.small[:, 255:256]),
                  reads=[("ps", sb_), ("epscol",)], writes=[rsk])
            P.dve(lambda e, rs=rs: e.reciprocal(out=rs[:], in_=rs[:]), reads=[rsk], writes=[rsk])
            for kc in range(KC):
                t, tk = self.tf()
                P.dve(lambda e, t=t, kc=kc, cs=cs, rs=rs: e.tensor_tensor(out=t[:], in0=self.hT[:, kc, cs], in1=rs[:], op=ALU.mult), reads=[("hT", kc, tt), rsk], writes=[tk])
                P.pool(lambda e, t=t, kc=kc: e.tensor_scalar(out=t[:], in0=t[:], scalar1=g[:, kc:kc + 1], scalar2=None, op0=ALU.mult), reads=[tk, ("gcol",)], writes=[tk])
                self.load(outv[:, kc, cs], t[:], [("dram", "out", kc, tt)], rkeys=[tk], q="pool")

    def phaseB(self, dr, kind, prev, lam_init, final, n_exp=NE, do_attn=True, do_moe=True):
        self.eps_col()
        self.load_hT(dr["hT_in"])
        if dr.get("modT_in") is not None:
            self.load(self.modT[:], dr["modT_in"], [("modT", j) for j in range(96)])
        if do_attn:
            if kind == "diff":
                self.attn_diff(dr, prev, lam_init)
            else:
                self.attn_mla(dr, prev)
            self.out_proj(dr["w_out"])
        self.P.barrier()
        if do_moe:
            self.moe(dr, n_exp)
        if final:
            self.final_norm(dr)
        else:
            self.store_hT(dr["hT_out"])


def _dram(nc, name, shape, dt, kind):
    return nc.dram_tensor(name, list(shape), dt, kind=kind).ap()


COMMON_IN = dict(rotT=([64, 96], BF16), invf=([64, 2], F32), ident=([128, 128], F32), pidx=([32, 128], F32))

A_IN = {
    "diff": dict(hT_in=([D, T], F32), ccol=([128, 16], F32), pos=([64, T], I32), adabT=([128, 96], F32), ada_w=([D, 6 * D], F32),
                 gmixT=([128, 16], F32), w_in=([D, 6144], F32)),
    "mla": dict(hT_in=([D, T], F32), ccol=([128, 16], F32), pos=([64, T], I32), adabT=([128, 96], F32), ada_w=([D, 6 * D], F32),
                gmixT=([128, 16], F32), w_in=([D, 1088], F32), qgT=([128, 4], F32), kvgT=([128, 4], F32),
                w_uq=([512, 3072], F32), w_ukv=([512, 4096], F32)),
}
A_OUT = {
    "diff": dict(modT_out=([128, 96], F32), qT=([16, 128, T], BF16), kT=([16, 128, T], BF16), V=([T, D], BF16)),
    "mla": dict(modT_out=([128, 96], F32), qnT=([16, 128, T], BF16), qrT=([16, 64, T], BF16), knT=([16, 128, T], BF16),
                krT=([64, T], BF16), V=([T, D], BF16)),
}
B_IN_COMMON = dict(hT_in=([D, T], F32), modT_in=([128, 96], F32), w_out=([D, D], F32), gffnT=([128, 16], F32),
                   bguT=([128, NE * 16], F32), bd=([NE, D], F32), wrT=([128, KC * NE], F32), brr=([128, NE], F32),
                   w_gu=([NE, D, D], F32), w_down=([NE, D // 2, D], F32), pbias=([128, 1], F32))
B_IN = {
    "diff": dict(qT=([16, 128, T], BF16), kT=([16, 128, T], BF16), V=([T, D], BF16), kT_prev=([16, 128, T], BF16), V_prev=([T, D], BF16),
                 lamT=([128, 4], F32), sgT=([128, 2], F32)),
    "mla": dict(qnT=([16, 128, T], BF16), qrT=([16, 64, T], BF16), knT=([16, 128, T], BF16), krT=([64, T], BF16), V=([T, D], BF16),
                knT_prev=([16, 128, T], BF16), krT_prev=([64, T], BF16), V_prev=([T, D], BF16)),
}


def _finish(nc, es, B):
    B.P.analyze()
    sems = {e: [es.enter_context(nc.semaphore(f"s_{e}{i}")) for i in range(max(1, (B.P.cnt[e] + EPOCH - 1) // EPOCH))] for e in ENGS}
    dsems = {e: [es.enter_context(nc.semaphore(f"d_{e}{i}")) for i in range(NSLOT)] for e in ["sp", "pool", "act"]}
    block = es.enter_context(nc.Block())
    B.P.emit(block, sems, dsems)


def build_A(kind):
    nc = bass.Bass("TRN2", target_bir_lowering=False, dynamic_dma_scratch_size=4096)
    dr = {}
    for k, (s, dt) in {**COMMON_IN, **A_IN[kind]}.items():
        dr[k] = _dram(nc, k, s, dt, "ExternalInput")
    for k, (s, dt) in A_OUT[kind].items():
        dr[k] = _dram(nc, k, s, dt, "ExternalOutput")
    with ExitStack() as es:
        B = Bld(nc, es)
        B.consts(dr)
        if kind == "diff":
            B.phaseA_diff(dr)
        else:
            B.phaseA_mla(dr)
        _finish(nc, es, B)
    return nc


def build_B(kind, lam_init, final, n_exp=NE, do_attn=True, do_moe=True):
    nc = bass.Bass("TRN2", target_bir_lowering=False, dynamic_dma_scratch_size=4096)
    dr = {}
    spec = {**COMMON_IN, **B_IN_COMMON, **B_IN[kind]}
    if not do_moe:
        for k in ("w_gu", "w_down", "bguT", "bd", "wrT", "brr", "gffnT"):
            spec.pop(k)
    elif n_exp != NE:
        spec["w_gu"] = ([n_exp, D, D], F32)
        spec["w_down"] = ([n_exp, D // 2, D], F32)
    if not do_attn:
        for k in list(B_IN[kind].keys()) + ["w_out", "pbias"]:
            spec.pop(k)
    for k, (s, dt) in spec.items():
        dr[k] = _dram(nc, k, s, dt, "ExternalInput")
    if final:
        dr["gfinT"] = _dram(nc, "gfinT", [128, 16], F32, "ExternalInput")
        dr["outT"] = _dram(nc, "outT", [D, T], F32, "ExternalOutput")
    else:
        dr["hT_out"] = _dram(nc, "hT_out", [D, T], F32, "ExternalOutput")
    with ExitStack() as es:
        B = Bld(nc, es)
        B.consts(dr)
        B.phaseB(dr, kind, "data", lam_init, final, n_exp, do_attn, do_moe)
        _finish(nc, es, B)
    return nc


def build_fused(depth=4, n_exp=NE):
    nc = bass.Bass("TRN2", target_bir_lowering=False, dynamic_dma_scratch_size=4096)
    I = lambda name, shape, dt: _dram(nc, name, shape, dt, "ExternalInput")
    N = lambda name, shape, dt: nc.dram_tensor(name, list(shape), dt).ap()
    g = {}
    for k, (sh, dt) in COMMON_IN.items():
        g[k] = I(k, sh, dt)
    xT = I("xT", [2, D, T], F32)
    pos = I("pos", [2, 64, T], I32)
    ccol = I("ccol", [128, 16], F32)
    outT = _dram(nc, "outT", [2, D, T], F32, "ExternalOutput")
    hbuf = N("hbuf", [2, D, T], F32)
    scr = {
        "diff": [dict(qT=N(f"qT{h}", [16, 128, T], BF16), kT=N(f"kT{h}", [16, 128, T], BF16), V=N(f"Vd{h}", [T, D], BF16)) for h in range(2)],
        "mla": [dict(qnT=N(f"qnT{h}", [16, 128, T], BF16), qrT=N(f"qrT{h}", [16, 64, T], BF16), knT=N(f"knT{h}", [16, 128, T], BF16),
                     krT=N(f"krT{h}", [64, T], BF16), V=N(f"Vm{h}", [T, D], BF16)) for h in range(2)],
    }
    L = []
    for i in range(depth):
        kind = "diff" if i % 2 == 0 else "mla"
        w = dict(adabT=I(f"adabT{i}", [128, 96], F32), ada_w=I(f"ada_w{i}", [D, 6 * D], F32), gmixT=I(f"gmixT{i}", [128, 16], F32),
                 gffnT=I(f"gffnT{i}", [128, 16], F32), bguT=I(f"bguT{i}", [128, NE * 16], F32), bd=I(f"bd{i}", [NE, D], F32),
                 wrT=I(f"wrT{i}", [128, KC * NE], F32), brr=I(f"brr{i}", [128, NE], F32),
                 w_gu=I(f"w_gu{i}", [n_exp, D, D], F32), w_down=I(f"w_down{i}", [n_exp, D // 2, D], F32), w_out=I(f"w_out{i}", [D, D], F32))
        if kind == "diff":
            w.update(w_in=I(f"w_in{i}", [D, 6144], F32), lamT=I(f"lamT{i}", [128, 4], F32), sgT=I(f"sgT{i}", [128, 2], F32))
        else:
            w.update(w_in=I(f"w_in{i}", [D, 1088], F32), qgT=I(f"qgT{i}", [128, 4], F32), kvgT=I(f"kvgT{i}", [128, 4], F32),
                     w_uq=I(f"w_uq{i}", [512, 3072], F32), w_ukv=I(f"w_ukv{i}", [512, 4096], F32))
        L.append(w)
    gfinT = I("gfinT", [128, 16], F32)
    with ExitStack() as es:
        B = Bld(nc, es)
        B.consts(g)
        for i in range(depth):
            kind = "diff" if i % 2 == 0 else "mla"
            lam_init = 0.8 - 0.6 * math.exp(-0.3 * i)
            w = L[i]
            final = i == depth - 1
            for half in range(2):
                dr = dict(g)
                dr.update(w)
                dr.update(scr[kind][half])
                dr.update(hT_in=(xT[half] if i == 0 else hbuf[half]), pos=pos[half], ccol=ccol)
                if half == 1:
                    dr["ada_w"] = None
                if kind == "diff":
                    B.phaseA_diff(dr)
                else:
                    B.phaseA_mla(dr)
                B.P.barrier()
            for half in range(2):
                dr = dict(g)
                dr.update(w)
                dr.update(scr[kind][half])
                dr.update(hT_in=(xT[half] if i == 0 else hbuf[half]), hT_out=hbuf[half], gfinT=gfinT, outT=outT[half])
                if half == 1:
                    for k, v in scr[kind][0].items():
                        if k[0] in "kV":
                            dr[k + "_prev"] = v
                B.phaseB(dr, kind, "static" if half == 1 else "none", lam_init, final, n_exp)
                B.P.barrier()
        _finish(nc, es, B)
    return nc


def fused_inputs(inp, n_exp=NE, depth=4):
    hc = host_consts()
    f = lambda a: np.asarray(a, np.float32)
    shared = dict(hc)
    for i in range(depth):
        kind = "diff" if i % 2 == 0 else "mla"
        j = i // 2
        shared.update({
            f"adabT{i}": colT(inp["ada_b"][i], 96), f"ada_w{i}": f(inp["ada_w"][i]), f"gmixT{i}": colT(inp["mix_norm_g"][i], 16),
            f"gffnT{i}": colT(inp["ffn_norm_g"][i], 16),
            f"bguT{i}": np.ascontiguousarray(f(inp["moe_b_gate_up"][i]).reshape(NE, 16, 128).transpose(2, 0, 1).reshape(128, NE * 16)),
            f"bd{i}": f(inp["moe_b_down"][i]),
            f"wrT{i}": np.ascontiguousarray(f(inp["moe_w_router"][i]).reshape(KC, 128, NE).transpose(1, 0, 2).reshape(128, KC * NE)),
            f"brr{i}": np.ascontiguousarray(np.broadcast_to(f(inp["moe_b_router"][i])[None, :], (128, NE))),
            f"w_gu{i}": f(inp["moe_w_gate_up"][i])[:n_exp], f"w_down{i}": f(inp["moe_w_down"][i])[:n_exp],
        })
        if kind == "diff":
            shared.update({f"w_in{i}": f(inp["diff_w_in"][j]), f"lamT{i}": np.ascontiguousarray(f(inp["diff_lambda"][j]).T),
                           f"sgT{i}": colT(inp["diff_subln_g"][j], 2), f"w_out{i}": f(inp["diff_w_out"][j])})
        else:
            shared.update({f"w_in{i}": f(inp["mla_w_in"][j]), f"qgT{i}": colT(inp["mla_q_norm_g"][j], 4), f"kvgT{i}": colT(inp["mla_kv_norm_g"][j], 4),
                           f"w_uq{i}": f(inp["mla_w_uq"][j]), f"w_ukv{i}": f(inp["mla_w_ukv"][j]), f"w_out{i}": f(inp["mla_w_out"][j])})
    shared["gfinT"] = colT(inp["final_norm_g"], 16)
    x = f(inp["x"])
    maps = []
    for b in range(4):
        m = dict(shared)
        m["xT"] = np.ascontiguousarray(x[b].reshape(2, T, D).transpose(0, 2, 1))
        m["pos"] = np.ascontiguousarray(np.broadcast_to(np.asarray(inp["positions"])[b].reshape(2, 1, T), (2, 64, T))).astype(np.int32)
        m["ccol"] = colT(np.asarray(inp["c"])[b], 16)
        maps.append(m)
    return maps


def kernel_fused(**inp):
    nc = _prog(("fused",), lambda: build_fused())
    maps = fused_inputs(inp)
    cores = list(range(4))
    res = run_bass_kernel_spmd(nc, [maps[c] for c in cores], core_ids=cores).results
    out = np.empty((4, 2 * T, D), np.float32)
    for b in range(4):
        o = np.asarray(res[b]["outT"])
        out[b] = o.transpose(0, 2, 1).reshape(2 * T, D)
    return out


def colT(v, n):
    return np.ascontiguousarray(np.asarray(v, np.float32).reshape(n, 128).T)


def host_consts():
    rot = np.zeros((64, 96), np.float32)
    for i in range(16):
        rot[i + 16, i] = -1.0
        rot[i, i + 16] = 1.0
    for i in range(32):
        rot[i + 32, 32 + i] = -1.0
        rot[i, 32 + i + 32] = 1.0
    invf = np.zeros((64, 2), np.float32)
    f32 = (500000.0 ** (-np.arange(0, 32, 2, dtype=np.float32) / 32)).astype(np.float32)
    f64 = (500000.0 ** (-np.arange(0, 64, 2, dtype=np.float32) / 64)).astype(np.float32)
    for p in range(32):
        invf[p, 0] = f32[p % 16]
    for p in range(64):
        invf[p, 1] = f64[p % 32]
    return dict(rotT=rot.astype(ml_dtypes.bfloat16), invf=invf, ident=np.eye(128, dtype=np.float32),
                pidx=np.ascontiguousarray(np.broadcast_to(np.arange(32, dtype=np.float32)[:, None], (32, 128))))


_PROGS = {}


def _prog(key, fn):
    if key not in _PROGS:
        _PROGS[key] = fn()
    return _PROGS[key]


def kernel_unfused(x, c, positions, ada_w, ada_b, mix_norm_g, ffn_norm_g, final_norm_g,
           diff_w_in, diff_lambda, diff_subln_g, diff_w_out,
           mla_w_in, mla_q_norm_g, mla_kv_norm_g, mla_w_uq, mla_w_ukv, mla_w_out,
           moe_w_router, moe_b_router, moe_w_gate_up, moe_b_gate_up, moe_w_down, moe_b_down):
    x = np.asarray(x, np.float32)
    NC = 8
    cores = list(range(NC))
    hc = host_consts()
    hT = [np.ascontiguousarray(x[cid // 2, (cid % 2) * T:(cid % 2 + 1) * T, :].T) for cid in cores]
    ccol = [colT(np.asarray(c)[cid // 2], 16) for cid in cores]
    pos = [np.ascontiguousarray(np.broadcast_to(np.asarray(positions)[cid // 2, (cid % 2) * T:(cid % 2 + 1) * T][None, :], (64, T))).astype(np.int32) for cid in cores]
    pbias = [np.full((128, 1), 0.0 if cid % 2 == 1 else -30000.0, np.float32) for cid in cores]
    out = None
    for i in range(4):
        kind = "diff" if i % 2 == 0 else "mla"
        j = i // 2
        lam_init = 0.8 - 0.6 * math.exp(-0.3 * i)
        shared = dict(hc)
        shared.update(adabT=colT(ada_b[i], 96), ada_w=np.asarray(ada_w[i], np.float32), gmixT=colT(mix_norm_g[i], 16))
        if kind == "diff":
            shared.update(w_in=np.asarray(diff_w_in[j], np.float32))
        else:
            shared.update(w_in=np.asarray(mla_w_in[j], np.float32), qgT=colT(mla_q_norm_g[j], 4), kvgT=colT(mla_kv_norm_g[j], 4),
                          w_uq=np.asarray(mla_w_uq[j], np.float32), w_ukv=np.asarray(mla_w_ukv[j], np.float32))
        ncA = _prog(("A", kind), lambda: build_A(kind))
        in_maps = [dict(shared, hT_in=hT[cid], ccol=ccol[cid], pos=pos[cid]) for cid in cores]
        rA = run_bass_kernel_spmd(ncA, in_maps, core_ids=cores).results
        final = i == 3
        sharedB = dict(hc)
        sharedB.update(w_out=np.asarray(diff_w_out[j] if kind == "diff" else mla_w_out[j], np.float32), gffnT=colT(ffn_norm_g[i], 16),
                       bguT=np.ascontiguousarray(np.asarray(moe_b_gate_up[i], np.float32).reshape(NE, 16, 128).transpose(2, 0, 1).reshape(128, NE * 16)),
                       bd=np.asarray(moe_b_down[i], np.float32),
                       wrT=np.ascontiguousarray(np.asarray(moe_w_router[i], np.float32).reshape(KC, 128, NE).transpose(1, 0, 2).reshape(128, KC * NE)),
                       brr=np.ascontiguousarray(np.broadcast_to(np.asarray(moe_b_router[i], np.float32)[None, :], (128, NE))),
                       w_gu=np.asarray(moe_w_gate_up[i], np.float32), w_down=np.asarray(moe_w_down[i], np.float32))
        if kind == "diff":
            sharedB.update(lamT=np.ascontiguousarray(np.asarray(diff_lambda[j], np.float32).T), sgT=colT(diff_subln_g[j], 2))
        if final:
            sharedB.update(gfinT=colT(final_norm_g, 16))
        ncB = _prog(("B", kind, i if kind == "diff" else 0, final), lambda: build_B(kind, lam_init, final))
        in_maps = []
        for cid in cores:
            m = dict(sharedB, hT_in=hT[cid], modT_in=rA[cid]["modT_out"], pbias=pbias[cid])
            partner = cid - 1 if cid % 2 == 1 else cid
            if kind == "diff":
                m.update(qT=rA[cid]["qT"], kT=rA[cid]["kT"], V=rA[cid]["V"], kT_prev=rA[partner]["kT"], V_prev=rA[partner]["V"])
            else:
                m.update(qnT=rA[cid]["qnT"], qrT=rA[cid]["qrT"], knT=rA[cid]["knT"], krT=rA[cid]["krT"], V=rA[cid]["V"],
                         knT_prev=rA[partner]["knT"], krT_prev=rA[partner]["krT"], V_prev=rA[partner]["V"])
            in_maps.append(m)
        rB = run_bass_kernel_spmd(ncB, in_maps, core_ids=cores).results
        if final:
            out = np.empty((4, 2048, D), np.float32)
            for cid in cores:
                out[cid // 2, (cid % 2) * T:(cid % 2 + 1) * T, :] = np.asarray(rB[cid]["outT"]).T
        else:
            hT = [np.asarray(rB[cid]["hT_out"]) for cid in cores]
    return out


def kernel(**inputs):
    return kernel_fused(**inputs)
```

```python
import math
from contextlib import ExitStack

import numpy as np
import ml_dtypes
import concourse.bass as bass
import concourse.mybir as mybir
from concourse.bass_utils import run_bass_kernel_spmd

F32 = mybir.dt.float32
BF16 = mybir.dt.bfloat16
I32 = mybir.dt.int32
AF = mybir.ActivationFunctionType
ALU = mybir.AluOpType
AX = mybir.AxisListType

D = 2048
T = 1024
KC = 16
EPS = 1e-6
NE = 32
ENGS = ["pe", "act", "dve", "pool", "sp"]
NSLOT = 8
EPOCH = 12000
NEPOCH = 24
NTF = 10
NTB = 8
TWO_PI = 2.0 * math.pi


class Prog:
    def __init__(self, nc):
        self.nc = nc
        self.ops = []

    def add(self, eng, fn, reads=(), writes=(), dma=False):
        self.ops.append(dict(eng=eng, fn=fn, reads=tuple(reads), writes=tuple(writes), dma=dma, extra=set()))

    def pe(self, fn, reads=(), writes=()):
        self.add("pe", fn, reads, writes)

    def act(self, fn, reads=(), writes=()):
        self.add("act", fn, reads, writes)

    def dve(self, fn, reads=(), writes=()):
        self.add("dve", fn, reads, writes)

    def pool(self, fn, reads=(), writes=()):
        self.add("pool", fn, reads, writes)

    def dma(self, fn, reads=(), writes=(), q="sp"):
        self.add(q, fn, reads, writes, dma=True)

    def barrier(self):
        self.ops.append(dict(bar=True))

    def analyze(self):
        last_w = {}
        readers = {}
        ops = [o for o in self.ops]
        real = []
        pending_bar = None
        last_comp = {}
        last_dmas = {e: [] for e in ENGS}
        seen_after_bar = set()
        for o in ops:
            if o.get("bar"):
                pending_bar = set()
                for e in ENGS:
                    if e in last_comp:
                        pending_bar.add(last_comp[e])
                    pending_bar.update(last_dmas[e][-NSLOT:])
                seen_after_bar = set()
                continue
            i = len(real)
            real.append(o)
            deps = set()
            for k in o["reads"]:
                if k in last_w:
                    deps.add(last_w[k])
            for k in o["writes"]:
                if k in last_w:
                    deps.add(last_w[k])
                rd = readers.get(k)
                if rd:
                    deps.update(rd[0].values())
                    deps.update(rd[1])
            deps.discard(i)
            if o["eng"] == "pe":
                deps = {d for d in deps if real[d]["eng"] != "pe" or real[d]["dma"]}
            if pending_bar is not None and o["eng"] not in seen_after_bar:
                deps.update(pending_bar)
                seen_after_bar.add(o["eng"])
            o["deps"] = deps
            for k in o["writes"]:
                last_w[k] = i
                readers[k] = ({}, [])
            for k in o["reads"]:
                rd = readers.setdefault(k, ({}, []))
                if o["dma"]:
                    rd[1].append(i)
                else:
                    rd[0][o["eng"]] = i
            if o["dma"]:
                last_dmas[o["eng"]].append(i)
            else:
                last_comp[o["eng"]] = i
        self.real = real
        for o in real:
            o["sig"] = False
        for o in real:
            for d in o["deps"]:
                real[d]["sig"] = True
        cnt = {e: 0 for e in ENGS}
        dcnt = {e: 0 for e in ENGS}
        for o in real:
            e = o["eng"]
            if o["dma"]:
                j = dcnt[e]
                dcnt[e] += 1
                o["slot"] = j % NSLOT
                o["tok"] = ("d", e, j % NSLOT, 16 * (j // NSLOT + 1))
                o["prev"] = 16 * (j // NSLOT)
            elif o["sig"]:
                c = cnt[e]
                cnt[e] += 1
                o["tok"] = ("c", e, c // EPOCH, c % EPOCH + 1)
        self.cnt = cnt
        self.dcnt = dcnt
        for e in ENGS:
            assert cnt[e] < EPOCH * NEPOCH, (e, cnt[e])

    def emit(self, block, sems, dsems):
        nc = self.nc
        ops = self.real
        engobj = {"pe": nc.tensor, "act": nc.scalar, "dve": nc.vector, "pool": nc.gpsimd, "sp": nc.sync}

        def semof(key):
            kind, e, idx = key
            return sems[e][idx] if kind == "c" else dsems[e][idx]

        def run_engine(ename):
            eng = engobj[ename]
            waited = {}
            last_dma_tok = {}
            for op in ops:
                if op["eng"] != ename:
                    continue
                need = {}
                for d in op["deps"]:
                    tok = ops[d]["tok"]
                    key = tok[:3]
                    need[key] = max(need.get(key, 0), tok[3])
                if op["dma"] and op["prev"] > 0:
                    key = ("d", ename, op["slot"])
                    need[key] = max(need.get(key, 0), op["prev"])
                for key, val in need.items():
                    if waited.get(key, 0) >= val:
                        continue
                    eng.wait_ge(semof(key), val)
                    waited[key] = val
                ins = op["fn"](eng)
                if op["dma"]:
                    ins.then_inc(semof(op["tok"][:3]), 16)
                    last_dma_tok[op["slot"]] = op["tok"]
                elif op["sig"]:
                    ins.then_inc(semof(op["tok"][:3]), 1)
            for slot, tok in last_dma_tok.items():
                eng.wait_ge(semof(tok[:3]), tok[3])

        @block.tensor
        def _(e):
            run_engine("pe")

        @block.scalar
        def _(e):
            run_engine("act")

        @block.vector
        def _(e):
            run_engine("dve")

        @block.gpsimd
        def _(e):
            run_engine("pool")

        @block.sync
        def _(e):
            run_engine("sp")


class Bld:
    def __init__(self, nc, es):
        self.nc = nc
        self.P = Prog(nc)
        sb = lambda name, shape, dt: es.enter_context(nc.sbuf_tensor("sb_" + name, shape, dt))
        self.hT = sb("hT", [128, KC, T], F32)
        self.actT = sb("actT", [128, KC, T], BF16)
        self.wst = [sb(f"wst{i}", [128, 4096], F32) for i in range(2)]
        self.wbf = [sb(f"wbf{i}", [128, 4096], BF16) for i in range(2)]
        self.arena = sb("arena", [128, 10240], BF16)
        self.tmpf = [sb(f"tf{i}", [128, 512], F32) for i in range(NTF)]
        self.tmpb = [sb(f"tb{i}", [128, 512], BF16) for i in range(NTB)]
        self.auxf = sb("auxf", [128, 2 * T], F32)
        self.cosT = self.auxf[:, 0:T]
        self.sinT = self.auxf[:, T:2 * T]
        self.gb = [self.auxf[:, 0:T], self.auxf[:, T:2 * T]]
        self.rstd = [sb(f"rstd{i}", [128, 512], F32) for i in range(2)]
        self.rsi = 0
        self.modT = sb("modT", [128, 96], F32)
        self.small = sb("small", [128, 256], F32)
        self.small_b = sb("small_b", [128, 64], BF16)
        self.ones_bf = sb("ones_bf", [128, 128], BF16)
        self.ones_f = sb("ones_f", [128, 128], F32)
        self.rotT = sb("rotT", [64, 96], BF16)
        self.invf = sb("invf", [64, 2], F32)
        self.ident = sb("ident", [128, 128], F32)
        self.pidx = sb("pidx", [32, 128], F32)
        self.bgu = sb("bgu", [128, NE * 16], F32)
        self.wr = sb("wr", [128, KC * NE], F32)
        self.brr = sb("brr", [128, NE], F32)
        self.GT = sb("GT", [32, T], F32)
        self.sel = sb("sel", [32, 128], F32)
        self.rt = sb("rt", [128, 64], F32)
        self.ps = [es.enter_context(nc.psum_tensor(f"ps{i}", [128, 512], F32)) for i in range(8)]
        self.tfi = 0
        self.tbi = 0
        self.wslot = 0
        self.mmb = 0
        self.uid = 0

    def tf(self):
        i = self.tfi
        self.tfi = (i + 1) % NTF
        return self.tmpf[i], ("tf", i)

    def rs_tile(self):
        i = self.rsi
        self.rsi ^= 1
        return self.rstd[i], ("rstd", i)

    def tb(self):
        i = self.tbi
        self.tbi = (i + 1) % NTB
        return self.tmpb[i], ("tb", i)

    def mmbank(self, lo=0, n=4):
        b = lo + self.mmb % n
        self.mmb += 1
        return b

    def load(self, dst, src, wkeys, rkeys=(), q="sp"):
        self.P.dma(lambda e, dst=dst, src=src: e.dma_start(out=dst, in_=src), reads=rkeys, writes=wkeys, q=q)

    def wload(self, regions, n_kc):
        P = self.P
        slot = self.wslot
        self.wslot ^= 1
        off = 0
        views = []
        for ri, r in enumerate(regions):
            w = r.shape[-1]
            n = n_kc * w
            dst = self.wst[slot][:, off:off + n].rearrange("p (k w) -> p k w", w=w)
            P.dma(lambda e, dst=dst, r=r: e.dma_start(out=dst, in_=r), writes=[("wst", slot, ri)])
            views.append(self.wbf[slot][:, off:off + n].rearrange("p (k w) -> p k w", w=w))
            off += n
        assert off <= 4096
        P.act(lambda e, slot=slot, off=off: e.copy(out=self.wbf[slot][:, :off], in_=self.wst[slot][:, :off]),
              reads=[("wst", slot, i) for i in range(4)], writes=[("wbf", slot)])
        return views, ("wbf", slot)

    def linear_fm(self, Wv, n_kc, chunks, rhs_fn, n_tiles, evac, banks=(0, 4)):
        P = self.P
        maxcols = 4096 // n_kc
        i = 0
        while i < len(chunks):
            c0 = chunks[i][0]
            j = i
            while j + 1 < len(chunks) and chunks[j + 1][0] + chunks[j + 1][1] - c0 <= maxcols \
                    and chunks[j + 1][0] == chunks[j][0] + chunks[j][1]:
                j += 1
            c1 = chunks[j][0] + chunks[j][1]
            (wv,), wk = self.wload([Wv[:, :, c0:c1]], n_kc)
            for ci in range(i, j + 1):
                col0, width = chunks[ci]
                o = col0 - c0
                for tt in range(n_tiles):
                    b = self.mmbank(*banks)
                    ncol = None
                    for kc in range(n_kc):
                        rhs, rk = rhs_fn(kc, tt)
                        ncol = rhs.shape[-1]
                        P.pe(lambda e, b=b, kc=kc, o=o, width=width, rhs=rhs, ncol=ncol, wv=wv:
                             e.matmul(self.ps[b][:width, :ncol], lhsT=wv[:rhs.shape[0], kc, o:o + width], rhs=rhs,
                                      start=(kc == 0), stop=(kc == n_kc - 1)),
                             reads=[wk] + list(rk), writes=[("ps", b)])
                    evac(ci, tt, self.ps[b][:width, :ncol], ("ps", b))
            i = j + 1

    def consts(self, dr):
        P = self.P
        P.dve(lambda e: e.memset(self.ones_bf[:], 1.0), writes=[("ones",)])
        P.dve(lambda e: e.memset(self.ones_f[:], 1.0), writes=[("onesf",)])
        self.load(self.rotT[:], dr["rotT"], [("rotT",)])
        self.load(self.invf[:], dr["invf"], [("invf",)])
        self.load(self.ident[:], dr["ident"], [("ident",)])
        self.load(self.pidx[:], dr["pidx"], [("pidx",)])

    def load_hT(self, src):
        v = src.rearrange("(kc p) t -> p kc t", p=128)
        for kc in range(KC):
            self.load(self.hT[:, kc, :], v[:, kc, :], [("hT", kc, 0), ("hT", kc, 1)])

    def store_hT(self, dst):
        v = dst.rearrange("(kc p) t -> p kc t", p=128)
        for kc in range(KC):
            self.load(v[:, kc, :], self.hT[:, kc, :], [("dram", "h", id(dst), kc)], rkeys=[("hT", kc, 0), ("hT", kc, 1)], q="pool")

    def norm_adaln(self, Acol, Bcol, abkeys, want32=None):
        P = self.P
        for tt in range(2):
            cs = slice(tt * 512, (tt + 1) * 512)
            sb_ = 6 + tt
            for kc in range(KC):
                sq, sqk = self.tf()
                P.act(lambda e, sq=sq, kc=kc, cs=cs: e.activation(out=sq[:], in_=self.hT[:, kc, cs], func=AF.Square),
                      reads=[("hT", kc, tt)], writes=[sqk])
                P.pe(lambda e, sq=sq, kc=kc, sb_=sb_: e.matmul(self.ps[sb_][:], lhsT=self.ones_f[:], rhs=sq[:],
                                                              start=(kc == 0), stop=(kc == KC - 1)),
                     reads=[sqk, ("onesf",)], writes=[("ps", sb_)])
            rs, rsk = self.rs_tile()
            P.act(lambda e, rs=rs, sb_=sb_: e.activation(out=rs[:], in_=self.ps[sb_][:], func=AF.Sqrt, scale=1.0 / D, bias=self.small[:, 255:256]),
                  reads=[("ps", sb_), ("epscol",)], writes=[rsk])
            P.dve(lambda e, rs=rs: e.reciprocal(out=rs[:], in_=rs[:]), reads=[rsk], writes=[rsk])
            for kc in range(KC):
                t, tk = self.tf()
                P.dve(lambda e, t=t, kc=kc, cs=cs, rs=rs: e.tensor_tensor(out=t[:], in0=self.hT[:, kc, cs], in1=rs[:], op=ALU.mult),
                      reads=[("hT", kc, tt), rsk], writes=[tk])
                if want32 is None:
                    P.pool(lambda e, t=t, kc=kc, cs=cs: e.tensor_scalar(out=self.actT[:, kc, cs], in0=t[:], scalar1=Acol[:, kc:kc + 1],
                                                                        scalar2=Bcol[:, kc:kc + 1], op0=ALU.mult, op1=ALU.add),
                           reads=[tk] + list(abkeys), writes=[("actT", kc, tt)])
                else:
                    u32, uk = self.tf()
                    P.pool(lambda e, t=t, kc=kc, u32=u32: e.tensor_scalar(out=u32[:], in0=t[:], scalar1=Acol[:, kc:kc + 1],
                                                                          scalar2=Bcol[:, kc:kc + 1], op0=ALU.mult, op1=ALU.add),
                           reads=[tk] + list(abkeys), writes=[uk])
                    P.act(lambda e, u32=u32, kc=kc, cs=cs: e.copy(out=self.actT[:, kc, cs], in_=u32[:]), reads=[uk], writes=[("actT", kc, tt)])
                    want32(kc, tt, u32, uk)

    def eps_col(self):
        self.P.dve(lambda e: e.memset(self.small[:, 255:256], EPS), writes=[("epscol",)])

    def rhs_act(self, kc, tt):
        return self.actT[:, kc, tt * 512:(tt + 1) * 512], [("actT", kc, tt)]

    def compute_mod(self, dr):
        P = self.P
        ccol = self.small[:, 0:16]
        cs_ = self.small[:, 16:32]
        self.load(ccol, dr["ccol"], [("ccol",)])
        P.act(lambda e: e.activation(out=cs_, in_=ccol, func=AF.Silu), reads=[("ccol",)], writes=[("csil",)])
        cb = self.small_b[:, 0:16]
        P.dve(lambda e: e.tensor_copy(out=cb, in_=cs_), reads=[("csil",)], writes=[("cbf",)])
        adab = self.small[:, 32:128]
        self.load(adab, dr["adabT"], [("adab",)])
        Wv = dr["ada_w"].rearrange("(kc p) n -> p kc n", p=128)

        def rhs(kc, tt):
            return cb[:, kc:kc + 1], [("cbf",)]

        def evac(ci, tt, ps, pk):
            P.dve(lambda e, ci=ci, ps=ps: e.tensor_tensor(out=self.modT[:, ci:ci + 1], in0=ps, in1=adab[:, ci:ci + 1], op=ALU.add),
                  reads=[pk, ("adab",)], writes=[("modT", ci)])

        self.linear_fm(Wv, KC, [(j * 128, 128) for j in range(96)], rhs, 1, evac)

    def adaln_cols(self, gT_dram, which):
        P = self.P
        base = 0 if which == 0 else 48
        g = self.small[:, 128:144]
        A = self.small[:, 144:160]
        self.load(g, gT_dram, [("gcol",)])
        P.dve(lambda e: e.tensor_scalar(out=A, in0=self.modT[:, base + 16:base + 32], scalar1=1.0, scalar2=None, op0=ALU.add),
              reads=[("modT", j) for j in range(base + 16, base + 32)], writes=[("Acol",)])
        P.dve(lambda e: e.tensor_tensor(out=A, in0=A, in1=g, op=ALU.mult), reads=[("Acol",), ("gcol",)], writes=[("Acol",)])
        return A, self.modT[:, base:base + 16], [("Acol",)] + [("modT", j) for j in range(base, base + 16)]

    def rope_tables(self, dr, col, R):
        P = self.P
        for tt in range(2):
            cs = slice(tt * 512, (tt + 1) * 512)
            pi_, pik = self.tf()
            pint = pi_.bitcast(I32)
            self.load(pint[:64, :], dr["pos"][:, cs], [pik])
            for which, shift, dst in ((0, 0.0, self.sinT), (1, math.pi / 2, self.cosT)):
                a, ak = self.tf()
                P.dve(lambda e, a=a, pint=pint: e.tensor_copy(out=a[:R, :], in_=pint[:R, :]), reads=[pik], writes=[ak])
                P.dve(lambda e, a=a, shift=shift: e.tensor_scalar(out=a[:R, :], in0=a[:R, :], scalar1=self.invf[:R, col:col + 1], scalar2=shift,
                                                                  op0=ALU.mult, op1=ALU.add), reads=[ak, ("invf",)], writes=[ak])
                ki, kk = self.tf()
                P.dve(lambda e, a=a, ki=ki: e.tensor_scalar(out=ki[:R, :], in0=a[:R, :], scalar1=1.0 / TWO_PI, scalar2=None, op0=ALU.mult),
                      reads=[ak], writes=[kk])
                k2, k2k = self.tf()
                k2i = k2.bitcast(I32)
                P.dve(lambda e, ki=ki, k2i=k2i: e.tensor_copy(out=k2i[:R, :], in_=ki[:R, :]), reads=[kk], writes=[k2k])
                P.dve(lambda e, ki=ki, k2i=k2i: e.tensor_copy(out=ki[:R, :], in_=k2i[:R, :]), reads=[k2k], writes=[kk])
                P.dve(lambda e, a=a, ki=ki: e.scalar_tensor_tensor(out=a[:R, :], in0=ki[:R, :], scalar=-TWO_PI, in1=a[:R, :], op0=ALU.mult, op1=ALU.add),
                      reads=[ak, kk], writes=[ak])
                P.dve(lambda e, a=a, ki=ki: e.tensor_scalar(out=ki[:R, :], in0=a[:R, :], scalar1=math.pi, scalar2=-TWO_PI, op0=ALU.is_gt, op1=ALU.mult),
                      reads=[ak], writes=[kk])
                P.dve(lambda e, a=a, ki=ki: e.tensor_tensor(out=a[:R, :], in0=a[:R, :], in1=ki[:R, :], op=ALU.add), reads=[ak, kk], writes=[ak])
                P.dve(lambda e, a=a, ki=ki: e.tensor_scalar(out=ki[:R, :], in0=a[:R, :], scalar1=-math.pi, scalar2=TWO_PI, op0=ALU.is_lt, op1=ALU.mult),
                      reads=[ak], writes=[kk])
                P.dve(lambda e, a=a, ki=ki: e.tensor_tensor(out=a[:R, :], in0=a[:R, :], in1=ki[:R, :], op=ALU.add), reads=[ak, kk], writes=[ak])
                P.dve(lambda e, a=a: e.tensor_scalar(out=a[:R, :], in0=a[:R, :], scalar1=math.pi, scalar2=-math.pi, op0=ALU.min, op1=ALU.max),
                      reads=[ak], writes=[ak])
                P.act(lambda e, a=a, dst=dst, cs=cs: e.activation(out=dst[:R, cs], in_=a[:R, :], func=AF.Sin), reads=[ak],
                      writes=[("trig", which, tt)])

    def rope_apply(self, xb, xk, R, rot_lo, tt):
        P = self.P
        cs = slice(tt * 512, (tt + 1) * 512)
        b = 4 + (self.mmb % 2)
        self.mmb += 1
        P.pe(lambda e, xb=xb, b=b: e.matmul(self.ps[b][:R, :], lhsT=self.rotT[:R, rot_lo:rot_lo + R], rhs=xb[:R, :], start=True, stop=True),
             reads=[xk, ("rotT",)], writes=[("ps", b)])
        t1, t1k = self.tf()
        t2, t2k = self.tf()
        P.dve(lambda e, xb=xb, t1=t1, cs=cs: e.tensor_tensor(out=t1[:R, :], in0=xb[:R, :], in1=self.cosT[:R, cs], op=ALU.mult),
              reads=[xk, ("trig", 1, tt)], writes=[t1k])
        P.dve(lambda e, t2=t2, b=b, cs=cs: e.tensor_tensor(out=t2[:R, :], in0=self.ps[b][:R, :], in1=self.sinT[:R, cs], op=ALU.mult),
              reads=[("ps", b), ("trig", 0, tt)], writes=[t2k])
        P.pool(lambda e, xb=xb, t1=t1, t2=t2: e.tensor_tensor(out=xb[:R, :], in0=t1[:R, :], in1=t2[:R, :], op=ALU.add),
               reads=[t1k, t2k], writes=[xk])

    def phaseA_diff(self, dr):
        P = self.P
        self.eps_col()
        self.load_hT(dr["hT_in"])
        self.rope_tables(dr, 0, 32)
        if dr.get("ada_w") is not None:
            self.compute_mod(dr)
        if dr.get("modT_out") is not None:
            self.load(dr["modT_out"], self.modT[:], [("dram", "modT")], rkeys=[("modT", j) for j in range(96)], q="pool")
        A, Bc, abk = self.adaln_cols(dr["gmixT"], 0)
        self.norm_adaln(A, Bc, abk)
        Wv = dr["w_in"].rearrange("(kc p) n -> p kc n", p=128)
        qT = dr["qT"]
        kT = dr["kT"]

        def evac(ci, tt, ps, pk):
            xb, xk = self.tb()
            P.act(lambda e, xb=xb, ps=ps: e.copy(out=xb[:], in_=ps), reads=[pk], writes=[xk])
            self.rope_apply(xb, xk, 32, 0, tt)
            dst = (qT if ci < 16 else kT)[ci % 16, :, tt * 512:(tt + 1) * 512]
            self.load(dst, xb[:], [("dram", "qk", ci, tt)], rkeys=[xk], q="pool")

        self.linear_fm(Wv, KC, [(j * 128, 128) for j in range(32)], evac=evac, rhs_fn=self.rhs_act, n_tiles=2)
        self.v_token_major(Wv, 4096, dr["V"], KC, lambda kc, tb_: (self.actT[:, kc, tb_ * 128:(tb_ + 1) * 128], [("actT", kc, tb_ // 4)]))

    def v_token_major(self, Wv, col_base, Vd, n_kc, lhs_fn, ncols=2048, col_list=None):
        P = self.P
        maxcols = 4096 // n_kc
        if col_list is None:
            col_list = [(col_base + g * 256, 256, g * 256) for g in range(ncols // 256)]
        for (c0, w, d0) in col_list:
            (wv,), wk = self.wload([Wv[:, :, c0:c0 + w]], n_kc)
            for tb_ in range(8):
                b = self.mmbank(0, 4)
                for kc in range(n_kc):
                    lhs, lk = lhs_fn(kc, tb_)
                    P.pe(lambda e, b=b, kc=kc, lhs=lhs, wv=wv, w=w: e.matmul(self.ps[b][:, :w], lhsT=lhs, rhs=wv[:, kc, :],
                                                                            start=(kc == 0), stop=(kc == n_kc - 1)),
                         reads=[wk] + list(lk), writes=[("ps", b)])
                vt, vk = self.tb()
                P.dve(lambda e, vt=vt, b=b, w=w: e.tensor_copy(out=vt[:, :w], in_=self.ps[b][:, :w]), reads=[("ps", b)], writes=[vk])
                self.load(Vd[tb_ * 128:(tb_ + 1) * 128, d0:d0 + w], vt[:, :w], [("dram", "V", d0, tb_)], rkeys=[vk], q="pool")

    def phaseA_mla(self, dr):
        P = self.P
        self.eps_col()
        self.load_hT(dr["hT_in"])
        self.rope_tables(dr, 1, 64)
        if dr.get("ada_w") is not None:
            self.compute_mod(dr)
        if dr.get("modT_out") is not None:
            self.load(dr["modT_out"], self.modT[:], [("dram", "modT")], rkeys=[("modT", j) for j in range(96)], q="pool")
        A, Bc, abk = self.adaln_cols(dr["gmixT"], 0)
        self.norm_adaln(A, Bc, abk)
        Wv = dr["w_in"].rearrange("(kc p) n -> p kc n", p=128)
        krT = dr["krT"]
        qg = self.small[:, 160:164]
        kvg = self.small[:, 164:168]
        self.load(qg, dr["qgT"], [("qg",)])
        self.load(kvg, dr["kvgT"], [("kvg",)])
        latf = self.hT

        def evac_lat(ci, tt, ps, pk):
            cs = slice(tt * 512, (tt + 1) * 512)
            if ci < 8:
                P.dve(lambda e, ci=ci, cs=cs, ps=ps: e.tensor_copy(out=latf[:, ci, cs], in_=ps), reads=[pk], writes=[("hT", ci, tt)])
            else:
                xb, xk = self.tb()
                P.act(lambda e, xb=xb, ps=ps: e.copy(out=xb[:64, :], in_=ps), reads=[pk], writes=[xk])
                self.rope_apply(xb, xk, 64, 32, tt)
                self.load(krT[:, cs], xb[:64, :], [("dram", "kr", tt)], rkeys=[xk], q="pool")

        self.linear_fm(Wv, KC, [(j * 128, 128) for j in range(8)] + [(1024, 64)], evac=evac_lat, rhs_fn=self.rhs_act, n_tiles=2)
        latn = self.arena[:, 0:8192].rearrange("p (c t) -> p c t", t=T)
        for grp, gcol, gk in ((0, qg, ("qg",)), (1, kvg, ("kvg",))):
            for tt in range(2):
                cs = slice(tt * 512, (tt + 1) * 512)
                sb_ = 6 + tt
                for c in range(4):
                    ci = grp * 4 + c
                    sq, sqk = self.tb()
                    P.act(lambda e, sq=sq, ci=ci, cs=cs: e.activation(out=sq[:], in_=latf[:, ci, cs], func=AF.Square), reads=[("hT", ci, tt)], writes=[sqk])
                    P.pe(lambda e, sq=sq, c=c, sb_=sb_: e.matmul(self.ps[sb_][:], lhsT=self.ones_bf[:], rhs=sq[:], start=(c == 0), stop=(c == 3)),
                         reads=[sqk, ("ones",)], writes=[("ps", sb_)])
                rs, rsk = self.rs_tile()
                P.act(lambda e, rs=rs, sb_=sb_: e.activation(out=rs[:], in_=self.ps[sb_][:], func=AF.Sqrt, scale=1.0 / 512, bias=self.small[:, 255:256]),
                      reads=[("ps", sb_), ("epscol",)], writes=[rsk])
                P.dve(lambda e, rs=rs: e.reciprocal(out=rs[:], in_=rs[:]), reads=[rsk], writes=[rsk])
                for c in range(4):
                    ci = grp * 4 + c
                    t, tk = self.tf()
                    P.dve(lambda e, t=t, ci=ci, cs=cs, rs=rs: e.tensor_tensor(out=t[:], in0=latf[:, ci, cs], in1=rs[:], op=ALU.mult),
                          reads=[("hT", ci, tt), rsk], writes=[tk])
                    P.pool(lambda e, t=t, ci=ci, c=c, cs=cs, gcol=gcol: e.tensor_scalar(out=latn[:, ci, cs], in0=t[:], scalar1=gcol[:, c:c + 1], scalar2=None, op0=ALU.mult),
                           reads=[tk, gk], writes=[("latn", ci, tt)])
        Wq = dr["w_uq"].rearrange("(kc p) n -> p kc n", p=128)
        chunks = []
        for h in range(16):
            chunks.append((h * 192, 128))
            chunks.append((h * 192 + 128, 64))
        qnT, qrT, knT = dr["qnT"], dr["qrT"], dr["knT"]

        def rhs_q(kc, tt):
            return latn[:, kc, tt * 512:(tt + 1) * 512], [("latn", kc, tt)]

        def evac_q(ci, tt, ps, pk):
            h, isr = ci // 2, ci % 2
            cs = slice(tt * 512, (tt + 1) * 512)
            xb, xk = self.tb()
            if not isr:
                P.act(lambda e, xb=xb, ps=ps: e.copy(out=xb[:], in_=ps), reads=[pk], writes=[xk])
                self.load(qnT[h, :, cs], xb[:], [("dram", "qn", h, tt)], rkeys=[xk], q="pool")
            else:
                P.act(lambda e, xb=xb, ps=ps: e.copy(out=xb[:64, :], in_=ps), reads=[pk], writes=[xk])
                self.rope_apply(xb, xk, 64, 32, tt)
                self.load(qrT[h, :, cs], xb[:64, :], [("dram", "qr", h, tt)], rkeys=[xk], q="pool")

        self.linear_fm(Wq, 4, chunks, evac=evac_q, rhs_fn=rhs_q, n_tiles=2)
        Wkv = dr["w_ukv"].rearrange("(kc p) n -> p kc n", p=128)

        def rhs_kv(kc, tt):
            return latn[:, 4 + kc, tt * 512:(tt + 1) * 512], [("latn", 4 + kc, tt)]

        def evac_k(ci, tt, ps, pk):
            cs = slice(tt * 512, (tt + 1) * 512)
            xb, xk = self.tb()
            P.act(lambda e, xb=xb, ps=ps: e.copy(out=xb[:], in_=ps), reads=[pk], writes=[xk])
            self.load(knT[ci, :, cs], xb[:], [("dram", "kn", ci, tt)], rkeys=[xk], q="pool")

        self.linear_fm(Wkv, 4, [(h * 256, 128) for h in range(16)], evac=evac_k, rhs_fn=rhs_kv, n_tiles=2)
        self.v_token_major(Wkv, 0, dr["V"], 4,
                           lambda kc, tb_: (latn[:, 4 + kc, tb_ * 128:(tb_ + 1) * 128], [("latn", 4 + kc, tb_ // 4)]),
                           col_list=[(h * 256 + 128, 128, h * 128) for h in range(16)])

    def kc_list(self, qt, prev):
        out = []
        if prev:
            for kc in range(8):
                out.append((kc, 0, True, None))
        for oi in range(4 * qt + 4):
            r = oi - 4 * qt
            if r < 0:
                out.append((8 + oi, 0, False, None))
            else:
                out.append((8 + oi, 128 * r, False, r))
        return out

    def attn_scores_pv(self, qt, prev, bias_ap, bias_k, scale, s_mm, pv_list, den_bank):
        P = self.P
        cq = qt * 512
        lst = self.kc_list(qt, prev)
        for idx, (kc, col0, isp, r) in enumerate(lst):
            sbk = self.mmbank(0, 2)
            s_mm(kc, cq + col0, col0, sbk)
            pT, pk = self.tb()
            if isp and bias_ap is not None:
                P.act(lambda e, pT=pT, sbk=sbk, col0=col0: e.activation(out=pT[:, col0:], in_=self.ps[sbk][:, col0:], func=AF.Exp, scale=scale, bias=bias_ap),
                      reads=[("ps", sbk)] + list(bias_k), writes=[pk])
            else:
                P.act(lambda e, pT=pT, sbk=sbk, col0=col0: e.activation(out=pT[:, col0:], in_=self.ps[sbk][:, col0:], func=AF.Exp, scale=scale),
                      reads=[("ps", sbk)], writes=[pk])
            if r is not None:
                P.pool(lambda e, pT=pT, col0=col0: e.memset(pT[64:128, col0:col0 + 64], 0.0), reads=[pk], writes=[pk])
            first = idx == 0
            last = idx == len(lst) - 1
            for (bank, lfn) in pv_list:
                lhs, lk = lfn(kc)
                P.pe(lambda e, bank=bank, lhs=lhs, pT=pT, col0=col0, first=first, last=last:
                     e.matmul(self.ps[bank][:, col0:], lhsT=lhs, rhs=pT[:, col0:], start=first, stop=last),
                     reads=[pk] + list(lk), writes=[("ps", bank)])
            P.pe(lambda e, pT=pT, col0=col0, first=first, last=last:
                 e.matmul(self.ps[den_bank][:, col0:], lhsT=self.ones_bf[:], rhs=pT[:, col0:], start=first, stop=last),
                 reads=[pk, ("ones",)], writes=[("ps", den_bank)])

    def attn_diff(self, dr, prev, lam_init):
        P = self.P
        ar = self.arena
        q_sb = ar[:, 0:2048].rearrange("p (c t) -> p c t", t=T)
        k_sb = ar[:, 2048:6144].rearrange("p (c t) -> p c t", t=2 * T)
        v_sb = ar[:, 6144:10240].rearrange("p (k d) -> p k d", d=256)
        qT, kT, V = dr["qT"], dr["kT"], dr["V"]
        lamT = self.small[:, 168:172]
        self.load(lamT, dr["lamT"], [("lamT",)])
        prod = self.small[:, 172:174]
        P.dve(lambda e: e.tensor_tensor(out=prod[:, 0:1], in0=lamT[:, 0:1], in1=lamT[:, 1:2], op=ALU.mult), reads=[("lamT",)], writes=[("lprod", 0)])
        P.dve(lambda e: e.tensor_tensor(out=prod[:, 1:2], in0=lamT[:, 2:3], in1=lamT[:, 3:4], op=ALU.mult), reads=[("lamT",)], writes=[("lprod", 1)])
        of, ofk = self.tf()
        P.dve(lambda e, of=of: e.memset(of[:, :128], 1.0), writes=[ofk])
        P.pe(lambda e, of=of: e.matmul(self.ps[7][:, 0:2], lhsT=of[:, :128], rhs=prod, start=True, stop=True),
             reads=[ofk, ("lprod", 0), ("lprod", 1)], writes=[("ps", 7)])
        ee = self.small[:, 174:176]
        P.act(lambda e: e.activation(out=ee, in_=self.ps[7][:, 0:2], func=AF.Exp), reads=[("ps", 7)], writes=[("lexp",)])
        nlam = self.small[:, 176:177]
        P.dve(lambda e: e.tensor_tensor(out=nlam, in0=ee[:, 1:2], in1=ee[:, 0:1], op=ALU.subtract), reads=[("lexp",)], writes=[("nlam",)])
        P.dve(lambda e: e.tensor_scalar(out=nlam, in0=nlam, scalar1=-lam_init, scalar2=None, op0=ALU.add), reads=[("nlam",)], writes=[("nlam",)])
        sg = self.small[:, 177:179]
        self.load(sg, dr["sgT"], [("sg",)])
        P.dve(lambda e: e.tensor_scalar(out=sg, in0=sg, scalar1=(1.0 - lam_init), scalar2=None, op0=ALU.mult), reads=[("sg",)], writes=[("sg",)])
        scale = 128 ** -0.5
        bias_ap = self.small[:, 179:180] if prev == "data" else None
        if prev == "data":
            self.load(bias_ap, dr["pbias"], [("pbias",)])
        has_prev = prev in ("data", "static")
        it = 0
        for h in range(8):
            for c in range(2):
                self.load(q_sb[:, c, :], qT[2 * h + c], [("ar", "q", c)])
                if has_prev:
                    self.load(k_sb[:, c, 0:T], dr["kT_prev"][2 * h + c], [("ar", "kp", c)])
                self.load(k_sb[:, c, T:2 * T], kT[2 * h + c], [("ar", "k", c)])
            if has_prev:
                self.load(v_sb[:, 0:8, :], dr["V_prev"][:, h * 256:(h + 1) * 256].rearrange("(k p) d -> p k d", p=128), [("ar", "vp")])
            self.load(v_sb[:, 8:16, :], V[:, h * 256:(h + 1) * 256].rearrange("(k p) d -> p k d", p=128), [("ar", "v")])
            for qt in range(2):
                on = []
                for c in range(2):
                    banks = (2, 3, 4) if it % 2 == 0 else (5, 6, 7)
                    it += 1

                    def s_mm(kc, qcol, col0, sbk, c=c):
                        isp = kc < 8
                        P.pe(lambda e: e.matmul(self.ps[sbk][:, col0:], lhsT=k_sb[:, c, kc * 128:(kc + 1) * 128], rhs=q_sb[:, c, qcol:(qcol - col0) + 512],
                                                start=True, stop=True),
                             reads=[("ar", "kp" if isp else "k", c), ("ar", "q", c)], writes=[("ps", sbk)])

                    pv = [(banks[d], (lambda kc, d=d: (v_sb[:, kc, d * 128:(d + 1) * 128], [("ar", "vp" if kc < 8 else "v")]))) for d in range(2)]
                    self.attn_scores_pv(qt, has_prev, bias_ap, [("pbias",)], scale, s_mm, pv, banks[2])
                    rd, rdk = self.tf()
                    P.dve(lambda e, rd=rd, banks=banks: e.reciprocal(out=rd[:], in_=self.ps[banks[2]][:]), reads=[("ps", banks[2])], writes=[rdk])
                    for d in range(2):
                        o, ok = self.tf()
                        P.dve(lambda e, o=o, rd=rd, banks=banks, d=d: e.tensor_tensor(out=o[:], in0=self.ps[banks[d]][:], in1=rd[:], op=ALU.mult),
                              reads=[("ps", banks[d]), rdk], writes=[ok])
                        on.append((o, ok))
                os_ = []
                for d in range(2):
                    (o0, k0), (o1, k1) = on[d], on[2 + d]
                    P.dve(lambda e, o0=o0, o1=o1: e.scalar_tensor_tensor(out=o0[:], in0=o1[:], scalar=nlam, in1=o0[:], op0=ALU.mult, op1=ALU.add),
                           reads=[k0, k1, ("nlam",)], writes=[k0])
                    os_.append((o0, k0))
                for d in range(2):
                    sq, sqk = self.tb()
                    P.act(lambda e, sq=sq, o=os_[d][0]: e.activation(out=sq[:], in_=o[:], func=AF.Square), reads=[os_[d][1]], writes=[sqk])
                    P.pe(lambda e, sq=sq, d=d: e.matmul(self.ps[0][:], lhsT=self.ones_bf[:], rhs=sq[:], start=(d == 0), stop=(d == 1)),
                         reads=[sqk, ("ones",)], writes=[("ps", 0)])
                rs, rsk = self.rs_tile()
                P.act(lambda e, rs=rs: e.activation(out=rs[:], in_=self.ps[0][:], func=AF.Sqrt, scale=1.0 / 256, bias=self.small[:, 255:256]),
                      reads=[("ps", 0), ("epscol",)], writes=[rsk])
                P.dve(lambda e, rs=rs: e.reciprocal(out=rs[:], in_=rs[:]), reads=[rsk], writes=[rsk])
                for d in range(2):
                    o, ok = os_[d]
                    P.dve(lambda e, o=o, rs=rs: e.tensor_tensor(out=o[:], in0=o[:], in1=rs[:], op=ALU.mult), reads=[ok, rsk], writes=[ok])
                    P.pool(lambda e, o=o, d=d, h=h, qt=qt: e.tensor_scalar(out=self.actT[:, 2 * h + d, qt * 512:(qt + 1) * 512], in0=o[:],
                                                                            scalar1=sg[:, d:d + 1], scalar2=None, op0=ALU.mult),
                           reads=[ok, ("sg",)], writes=[("actT", 2 * h + d, qt)])

    def attn_mla(self, dr, prev):
        P = self.P
        ar = self.arena
        qn_sb = ar[:, 0:1024]
        qr_sb = ar[:, 1024:2048]
        kn_sb = ar[:, 2048:4096]
        kr_sb = ar[:, 4096:6144]
        v_sb = ar[:, 6144:8192].rearrange("p (k d) -> p k d", d=128)
        scale = 192 ** -0.5
        bias_ap = self.small[:, 179:180] if prev == "data" else None
        if prev == "data":
            self.load(bias_ap, dr["pbias"], [("pbias",)])
        has_prev = prev in ("data", "static")
        if has_prev:
            self.load(kr_sb[:64, 0:T], dr["krT_prev"], [("ar", "krp")])
        self.load(kr_sb[:64, T:2 * T], dr["krT"], [("ar", "kr")])
        it = 0
        for h in range(16):
            self.load(qn_sb, dr["qnT"][h], [("ar", "qn")])
            self.load(qr_sb[:64, :], dr["qrT"][h], [("ar", "qr")])
            if has_prev:
                self.load(kn_sb[:, 0:T], dr["knT_prev"][h], [("ar", "knp")])
                self.load(v_sb[:, 0:8, :], dr["V_prev"][:, h * 128:(h + 1) * 128].rearrange("(k p) d -> p k d", p=128), [("ar", "vp")])
            self.load(kn_sb[:, T:2 * T], dr["knT"][h], [("ar", "kn")])
            self.load(v_sb[:, 8:16, :], dr["V"][:, h * 128:(h + 1) * 128].rearrange("(k p) d -> p k d", p=128), [("ar", "v")])
            for qt in range(2):
                banks = (2, 4) if it % 2 == 0 else (5, 7)
                it += 1

                def s_mm(kc, qcol, col0, sbk):
                    isp = kc < 8
                    P.pe(lambda e: e.matmul(self.ps[sbk][:, col0:], lhsT=kn_sb[:, kc * 128:(kc + 1) * 128], rhs=qn_sb[:, qcol:(qcol - col0) + 512],
                                            start=True, stop=False),
                         reads=[("ar", "knp" if isp else "kn"), ("ar", "qn")], writes=[("ps", sbk)])
                    P.pe(lambda e: e.matmul(self.ps[sbk][:, col0:], lhsT=kr_sb[:64, kc * 128:(kc + 1) * 128], rhs=qr_sb[:64, qcol:(qcol - col0) + 512],
                                            start=False, stop=True),
                         reads=[("ar", "krp" if isp else "kr"), ("ar", "qr")], writes=[("ps", sbk)])

                pv = [(banks[0], (lambda kc: (v_sb[:, kc, :], [("ar", "vp" if kc < 8 else "v")])))]
                self.attn_scores_pv(qt, has_prev, bias_ap, [("pbias",)], scale, s_mm, pv, banks[1])
                rd, rdk = self.tf()
                P.dve(lambda e, rd=rd, banks=banks: e.reciprocal(out=rd[:], in_=self.ps[banks[1]][:]), reads=[("ps", banks[1])], writes=[rdk])
                P.dve(lambda e, rd=rd, banks=banks, h=h, qt=qt: e.tensor_tensor(out=self.actT[:, h, qt * 512:(qt + 1) * 512], in0=self.ps[banks[0]][:], in1=rd[:], op=ALU.mult),
                      reads=[("ps", banks[0]), rdk], writes=[("actT", h, qt)])

    def out_proj(self, w_out):
        P = self.P
        Wv = w_out.rearrange("(kc p) n -> p kc n", p=128)

        def evac(ci, tt, ps, pk):
            cs = slice(tt * 512, (tt + 1) * 512)
            P.dve(lambda e, ci=ci, cs=cs, ps=ps: e.scalar_tensor_tensor(out=self.hT[:, ci, cs], in0=ps, scalar=self.modT[:, 32 + ci:33 + ci],
                                                                        in1=self.hT[:, ci, cs], op0=ALU.mult, op1=ALU.add),
                  reads=[pk, ("modT", 32 + ci), ("hT", ci, tt)], writes=[("hT", ci, tt)])

        self.linear_fm(Wv, KC, [(j * 128, 128) for j in range(16)], evac=evac, rhs_fn=self.rhs_act, n_tiles=2)

    def moe(self, dr, n_exp=NE):
        P = self.P
        self.load(self.bgu[:], dr["bguT"], [("bgu",)])
        self.load(self.wr[:], dr["wrT"], [("wr",)])
        self.load(self.brr[:], dr["brr"], [("brr",)])
        bgu3 = self.bgu[:].rearrange("p (e j) -> p e j", j=16)
        P.dve(lambda e: e.tensor_scalar(out=bgu3[:, :, 8:16], in0=bgu3[:, :, 8:16], scalar1=1.0, scalar2=None, op0=ALU.add),
              reads=[("bgu",)], writes=[("bgu",)])
        wr3 = self.wr[:].rearrange("p (k e) -> p k e", e=NE)
        A, Bc, abk = self.adaln_cols(dr["gffnT"], 1)
        LB = 5

        def want32(kc, tt, u32, uk):
            for tb4 in range(4):
                tb_ = tt * 4 + tb4
                P.pe(lambda e, u32=u32, kc=kc, tb4=tb4, tb_=tb_: e.matmul(self.ps[LB][:, tb_ * 32:(tb_ + 1) * 32], lhsT=u32[:, tb4 * 128:(tb4 + 1) * 128],
                                                                          rhs=wr3[:, kc, :], start=(kc == 0 and tb_ == 0), stop=(kc == KC - 1), skip_group_check=True),
                     reads=[uk, ("wr",)], writes=[("ps", LB)])

        self.norm_adaln(A, Bc, abk, want32=want32)
        rt = self.rt
        for tb_ in range(8):
            lg = rt[:, 0:32]
            P.dve(lambda e, tb_=tb_: e.tensor_tensor(out=lg, in0=self.ps[LB][:, tb_ * 32:(tb_ + 1) * 32], in1=self.brr[:], op=ALU.add),
                  reads=[("ps", LB), ("brr",)], writes=[("rt", "lg")])
            m8 = rt[:, 32:40]
            P.dve(lambda e: e.max(out=m8, in_=lg), reads=[("rt", "lg")], writes=[("rt", "m8")])
            mk, mkk = self.tf()
            P.dve(lambda e, mk=mk: e.tensor_scalar(out=mk[:, 0:32], in0=lg, scalar1=m8[:, 3:4], scalar2=None, op0=ALU.is_ge),
                  reads=[("rt", "lg"), ("rt", "m8")], writes=[mkk])
            nm = rt[:, 40:41]
            P.dve(lambda e: e.tensor_scalar(out=nm, in0=m8[:, 0:1], scalar1=-1.0, scalar2=None, op0=ALU.mult), reads=[("rt", "m8")], writes=[("rt", "nm")])
            ex, exk = self.tf()
            P.act(lambda e, ex=ex: e.activation(out=ex[:, 0:32], in_=lg, func=AF.Exp, bias=nm, scale=1.0), reads=[("rt", "lg"), ("rt", "nm")], writes=[exk])
            P.dve(lambda e, ex=ex, mk=mk: e.tensor_tensor(out=ex[:, 0:32], in0=ex[:, 0:32], in1=mk[:, 0:32], op=ALU.mult), reads=[exk, mkk], writes=[exk])
            dn = rt[:, 41:42]
            P.dve(lambda e, ex=ex: e.reduce_sum(out=dn, in_=ex[:, 0:32], axis=AX.X), reads=[exk], writes=[("rt", "dn")])
            P.dve(lambda e: e.reciprocal(out=dn, in_=dn), reads=[("rt", "dn")], writes=[("rt", "dn")])
            P.dve(lambda e, ex=ex: e.tensor_scalar(out=ex[:, 0:32], in0=ex[:, 0:32], scalar1=dn, scalar2=None, op0=ALU.mult), reads=[exk, ("rt", "dn")], writes=[exk])
            P.pe(lambda e, ex=ex, tb_=tb_: e.transpose(out=self.ps[7][:32, (tb_ % 4) * 128:(tb_ % 4 + 1) * 128], in_=ex[:, 0:32], identity=self.ident[:]),
                 reads=[exk, ("ident",)], writes=[("ps", 7)])
            P.dve(lambda e, tb_=tb_: e.tensor_copy(out=self.GT[:, tb_ * 128:(tb_ + 1) * 128], in_=self.ps[7][:32, (tb_ % 4) * 128:(tb_ % 4 + 1) * 128]),
                  reads=[("ps", 7)], writes=[("GT", tb_ // 4)])
        Ap = self.arena[:, 0:8192].rearrange("p (f t) -> p f t", t=T)
        for ex_i in range(n_exp):
            P.dve(lambda e, ex_i=ex_i: e.tensor_scalar(out=self.sel[:], in0=self.pidx[:], scalar1=float(ex_i), scalar2=None, op0=ALU.is_equal),
                  reads=[("pidx",)], writes=[("sel",)])
            gbs = []
            for tt in range(2):
                P.pe(lambda e, tt=tt: e.matmul(self.ps[6 + tt][:], lhsT=self.sel[:], rhs=self.GT[:, tt * 512:(tt + 1) * 512], start=True, stop=True),
                     reads=[("sel",), ("GT", tt)], writes=[("ps", 6 + tt)])
                gb = self.gb[ex_i % 2][:, tt * 512:(tt + 1) * 512]
                gbk = ("gb", ex_i % 2, tt)
                P.act(lambda e, gb=gb, tt=tt: e.copy(out=gb, in_=self.ps[6 + tt][:]), reads=[("ps", 6 + tt)], writes=[gbk])
                gbs.append((gb, gbk))
            Wgu = dr["w_gu"][ex_i].rearrange("(kc p) n -> p kc n", p=128)
            for fc in range(8):
                (wg, wu), wk = self.wload([Wgu[:, :, fc * 128:(fc + 1) * 128], Wgu[:, :, 1024 + fc * 128:1024 + (fc + 1) * 128]], KC)
                for tt in range(2):
                    cs = slice(tt * 512, (tt + 1) * 512)
                    bg = 0 + 2 * (self.mmb % 2)
                    bu = bg + 1
                    self.mmb += 1
                    for kc in range(KC):
                        P.pe(lambda e, bg=bg, kc=kc, wg=wg, cs=cs: e.matmul(self.ps[bg][:], lhsT=wg[:, kc, :], rhs=self.actT[:, kc, cs], start=(kc == 0), stop=(kc == KC - 1)),
                             reads=[wk, ("actT", kc, tt)], writes=[("ps", bg)])
                    for kc in range(KC):
                        P.pe(lambda e, bu=bu, kc=kc, wu=wu, cs=cs: e.matmul(self.ps[bu][:], lhsT=wu[:, kc, :], rhs=self.actT[:, kc, cs], start=(kc == 0), stop=(kc == KC - 1)),
                             reads=[wk, ("actT", kc, tt)], writes=[("ps", bu)])
                    gc, gck = self.tf()
                    P.dve(lambda e, gc=gc, bg=bg, ex_i=ex_i, fc=fc: e.tensor_scalar(out=gc[:], in0=self.ps[bg][:], scalar1=bgu3[:, ex_i, fc:fc + 1], scalar2=7.0, op0=ALU.add, op1=ALU.min),
                          reads=[("ps", bg), ("bgu",)], writes=[gck])
                    sg_, sgk = self.tf()
                    P.act(lambda e, gc=gc, sg_=sg_: e.activation(out=sg_[:], in_=gc[:], func=AF.Sigmoid, scale=1.702), reads=[gck], writes=[sgk])
                    uc, uck = self.tf()
                    P.dve(lambda e, uc=uc, bu=bu, ex_i=ex_i, fc=fc: e.tensor_scalar(out=uc[:], in0=self.ps[bu][:], scalar1=bgu3[:, ex_i, 8 + fc:9 + fc], scalar2=-6.0, op0=ALU.add, op1=ALU.max),
                          reads=[("ps", bu), ("bgu",)], writes=[uck])
                    P.pool(lambda e, gc=gc, sg_=sg_: e.tensor_tensor(out=gc[:], in0=gc[:], in1=sg_[:], op=ALU.mult), reads=[gck, sgk], writes=[gck])
                    P.dve(lambda e, gc=gc, uc=uc: e.scalar_tensor_tensor(out=uc[:], in0=uc[:], scalar=8.0, in1=gc[:], op0=ALU.min, op1=ALU.mult), reads=[gck, uck], writes=[uck])
                    gb, gbk = gbs[tt]
                    P.pool(lambda e, uc=uc, gb=gb, fc=fc, cs=cs: e.tensor_tensor(out=Ap[:, fc, cs], in0=uc[:], in1=gb, op=ALU.mult), reads=[uck, gbk], writes=[("Ap", fc, tt)])
            Wd = dr["w_down"][ex_i].rearrange("(kc p) n -> p kc n", p=128)
            for dp in range(4):
                (wd,), wk = self.wload([Wd[:, :, dp * 512:(dp + 1) * 512]], 8)
                for dci in range(4):
                    dc = dp * 4 + dci
                    for tt in range(2):
                        cs = slice(tt * 512, (tt + 1) * 512)
                        b = 4 + (self.mmb % 2)
                        self.mmb += 1
                        for fk in range(8):
                            P.pe(lambda e, b=b, fk=fk, wd=wd, dci=dci, cs=cs: e.matmul(self.ps[b][:], lhsT=wd[:, fk, dci * 128:(dci + 1) * 128], rhs=Ap[:, fk, cs], start=(fk == 0), stop=(fk == 7)),
                                 reads=[wk, ("Ap", fk, tt)], writes=[("ps", b)])
                        P.dve(lambda e, b=b, dc=dc, cs=cs: e.scalar_tensor_tensor(out=self.hT[:, dc, cs], in0=self.ps[b][:], scalar=self.modT[:, 80 + dc:81 + dc], in1=self.hT[:, dc, cs],
                                                                                 op0=ALU.mult, op1=ALU.add),
                              reads=[("ps", b), ("modT", 80 + dc), ("hT", dc, tt)], writes=[("hT", dc, tt)])
        bdv = self.wst[0][:32, 0:D]
        self.load(bdv, dr["bd"], [("wst", 0, i) for i in range(4)])
        for dc in range(KC):
            for tt in range(2):
                cs = slice(tt * 512, (tt + 1) * 512)
                b = 4 + (self.mmb % 2)
                self.mmb += 1
                P.pe(lambda e, b=b, dc=dc, cs=cs: e.matmul(self.ps[b][:], lhsT=bdv[:, dc * 128:(dc + 1) * 128], rhs=self.GT[:, cs], start=True, stop=True),
                     reads=[("wst", 0, 0), ("GT", tt)], writes=[("ps", b)])
                P.dve(lambda e, b=b, dc=dc, cs=cs: e.scalar_tensor_tensor(out=self.hT[:, dc, cs], in0=self.ps[b][:], scalar=self.modT[:, 80 + dc:81 + dc], in1=self.hT[:, dc, cs],
                                                                         op0=ALU.mult, op1=ALU.add),
                      reads=[("ps", b), ("modT", 80 + dc), ("hT", dc, tt)], writes=[("hT", dc, tt)])

    def final_norm(self, dr):
        P = self.P
        g = self.small[:, 128:144]
        self.load(g, dr["gfinT"], [("gcol",)])
        outv = dr["outT"].rearrange("(kc p) t -> p kc t", p=128)
        for tt in range(2):
            cs = slice(tt * 512, (tt + 1) * 512)
            sb_ = 6 + tt
            for kc in range(KC):
                sq, sqk = self.tb()
                P.act(lambda e, sq=sq, kc=kc, cs=cs: e.activation(out=sq[:], in_=self.hT[:, kc, cs], func=AF.Square), reads=[("hT", kc, tt)], writes=[sqk])
                P.pe(lambda e, sq=sq, kc=kc, sb_=sb_: e.matmul(self.ps[sb_][:], lhsT=self.ones_bf[:], rhs=sq[:], start=(kc == 0), stop=(kc == KC - 1)),
                     reads=[sqk, ("ones",)], writes=[("ps", sb_)])
            rs, rsk = self.rs_tile()
            P.act(lambda e, rs=rs, sb_=sb_: e.activation(out=rs[:], in_=self.ps[sb_][:], func=AF.Sqrt, scale=1.0 / D, bias=self.small[:, 255:256]),
                  reads=[("ps", sb_), ("epscol",)], writes=[rsk])
            P.dve(lambda e, rs=rs: e.reciprocal(out=rs[:], in_=rs[:]), reads=[rsk], writes=[rsk])
            for kc in range(KC):
                t, tk = self.tf()
                P.dve(lambda e, t=t, kc=kc, cs=cs, rs=rs: e.tensor_tensor(out=t[:], in0=self.hT[:, kc, cs], in1=rs[:], op=ALU.mult), reads=[("hT", kc, tt), rsk], writes=[tk])
                P.pool(lambda e, t=t, kc=kc: e.tensor_scalar(out=t[:], in0=t[:], scalar1=g[:, kc:kc + 1], scalar2=None, op0=ALU.mult), reads=[tk, ("gcol",)], writes=[tk])
                self.load(outv[:, kc, cs], t[:], [("dram", "out", kc, tt)], rkeys=[tk], q="pool")

    def phaseB(self, dr, kind, prev, lam_init, final, n_exp=NE, do_attn=True, do_moe=True):
        self.eps_col()
        self.load_hT(dr["hT_in"])
        if dr.get("modT_in") is not None:
            self.load(self.modT[:], dr["modT_in"], [("modT", j) for j in range(96)])
        if do_attn:
            if kind == "diff":
                self.attn_diff(dr, prev, lam_init)
            else:
                self.attn_mla(dr, prev)
            self.out_proj(dr["w_out"])
        self.P.barrier()
        if do_moe:
            self.moe(dr, n_exp)
        if final:
            self.final_norm(dr)
        else:
            self.store_hT(dr["hT_out"])


def _dram(nc, name, shape, dt, kind):
    return nc.dram_tensor(name, list(shape), dt, kind=kind).ap()


COMMON_IN = dict(rotT=([64, 96], BF16), invf=([64, 2], F32), ident=([128, 128], F32), pidx=([32, 128], F32))

A_IN = {
    "diff": dict(hT_in=([D, T], F32), ccol=([128, 16], F32), pos=([64, T], I32), adabT=([128, 96], F32), ada_w=([D, 6 * D], F32),
                 gmixT=([128, 16], F32), w_in=([D, 6144], F32)),
    "mla": dict(hT_in=([D, T], F32), ccol=([128, 16], F32), pos=([64, T], I32), adabT=([128, 96], F32), ada_w=([D, 6 * D], F32),
                gmixT=([128, 16], F32), w_in=([D, 1088], F32), qgT=([128, 4], F32), kvgT=([128, 4], F32),
                w_uq=([512, 3072], F32), w_ukv=([512, 4096], F32)),
}
A_OUT = {
    "diff": dict(modT_out=([128, 96], F32), qT=([16, 128, T], BF16), kT=([16, 128, T], BF16), V=([T, D], BF16)),
    "mla": dict(modT_out=([128, 96], F32), qnT=([16, 128, T], BF16), qrT=([16, 64, T], BF16), knT=([16, 128, T], BF16),
                krT=([64, T], BF16), V=([T, D], BF16)),
}
B_IN_COMMON = dict(hT_in=([D, T], F32), modT_in=([128, 96], F32), w_out=([D, D], F32), gffnT=([128, 16], F32),
                   bguT=([128, NE * 16], F32), bd=([NE, D], F32), wrT=([128, KC * NE], F32), brr=([128, NE], F32),
                   w_gu=([NE, D, D], F32), w_down=([NE, D // 2, D], F32), pbias=([128, 1], F32))
B_IN = {
    "diff": dict(qT=([16, 128, T], BF16), kT=([16, 128, T], BF16), V=([T, D], BF16), kT_prev=([16, 128, T], BF16), V_prev=([T, D], BF16),
                 lamT=([128, 4], F32), sgT=([128, 2], F32)),
    "mla": dict(qnT=([16, 128, T], BF16), qrT=([16, 64, T], BF16), knT=([16, 128, T], BF16), krT=([64, T], BF16), V=([T, D], BF16),
                knT_prev=([16, 128, T], BF16), krT_prev=([64, T], BF16), V_prev=([T, D], BF16)),
}


def _finish(nc, es, B):
    B.P.analyze()
    sems = {e: [es.enter_context(nc.semaphore(f"s_{e}{i}")) for i in range(max(1, (B.P.cnt[e] + EPOCH - 1) // EPOCH))] for e in ENGS}
    dsems = {e: [es.enter_context(nc.semaphore(f"d_{e}{i}")) for i in range(NSLOT)] for e in ["sp", "pool", "act"]}
    block = es.enter_context(nc.Block())
    B.P.emit(block, sems, dsems)


def build_A(kind):
    nc = bass.Bass("TRN2", target_bir_lowering=False, dynamic_dma_scratch_size=4096)
    dr = {}
    for k, (s, dt) in {**COMMON_IN, **A_IN[kind]}.items():
        dr[k] = _dram(nc, k, s, dt, "ExternalInput")
    for k, (s, dt) in A_OUT[kind].items():
        dr[k] = _dram(nc, k, s, dt, "ExternalOutput")
    with ExitStack() as es:
        B = Bld(nc, es)
        B.consts(dr)
        if kind == "diff":
            B.phaseA_diff(dr)
        else:
            B.phaseA_mla(dr)
        _finish(nc, es, B)
    return nc


def build_B(kind, lam_init, final, n_exp=NE, do_attn=True, do_moe=True):
    nc = bass.Bass("TRN2", target_bir_lowering=False, dynamic_dma_scratch_size=4096)
    dr = {}
    spec = {**COMMON_IN, **B_IN_COMMON, **B_IN[kind]}
    if not do_moe:
        for k in ("w_gu", "w_down", "bguT", "bd", "wrT", "brr", "gffnT"):
            spec.pop(k)
    elif n_exp != NE:
        spec["w_gu"] = ([n_exp, D, D], F32)
        spec["w_down"] = ([n_exp, D // 2, D], F32)
    if not do_attn:
        for k in list(B_IN[kind].keys()) + ["w_out", "pbias"]:
            spec.pop(k)
    for k, (s, dt) in spec.items():
        dr[k] = _dram(nc, k, s, dt, "ExternalInput")
    if final:
        dr["gfinT"] = _dram(nc, "gfinT", [128, 16], F32, "ExternalInput")
        dr["outT"] = _dram(nc, "outT", [D, T], F32, "ExternalOutput")
    else:
        dr["hT_out"] = _dram(nc, "hT_out", [D, T], F32, "ExternalOutput")
    with ExitStack() as es:
        B = Bld(nc, es)
        B.consts(dr)
        B.phaseB(dr, kind, "data", lam_init, final, n_exp, do_attn, do_moe)
        _finish(nc, es, B)
    return nc


def build_fused(depth=4, n_exp=NE):
    nc = bass.Bass("TRN2", target_bir_lowering=False, dynamic_dma_scratch_size=4096)
    I = lambda name, shape, dt: _dram(nc, name, shape, dt, "ExternalInput")
    N = lambda name, shape, dt: nc.dram_tensor(name, list(shape), dt).ap()
    g = {}
    for k, (sh, dt) in COMMON_IN.items():
        g[k] = I(k, sh, dt)
    xT = I("xT", [2, D, T], F32)
    pos = I("pos", [2, 64, T], I32)
    ccol = I("ccol", [128, 16], F32)
    outT = _dram(nc, "outT", [2, D, T], F32, "ExternalOutput")
    hbuf = N("hbuf", [2, D, T], F32)
    scr = {
        "diff": [dict(qT=N(f"qT{h}", [16, 128, T], BF16), kT=N(f"kT{h}", [16, 128, T], BF16), V=N(f"Vd{h}", [T, D], BF16)) for h in range(2)],
        "mla": [dict(qnT=N(f"qnT{h}", [16, 128, T], BF16), qrT=N(f"qrT{h}", [16, 64, T], BF16), knT=N(f"knT{h}", [16, 128, T], BF16),
                     krT=N(f"krT{h}", [64, T], BF16), V=N(f"Vm{h}", [T, D], BF16)) for h in range(2)],
    }
    L = []
    for i in range(depth):
        kind = "diff" if i % 2 == 0 else "mla"
        w = dict(adabT=I(f"adabT{i}", [128, 96], F32), ada_w=I(f"ada_w{i}", [D, 6 * D], F32), gmixT=I(f"gmixT{i}", [128, 16], F32),
                 gffnT=I(f"gffnT{i}", [128, 16], F32), bguT=I(f"bguT{i}", [128, NE * 16], F32), bd=I(f"bd{i}", [NE, D], F32),
                 wrT=I(f"wrT{i}", [128, KC * NE], F32), brr=I(f"brr{i}", [128, NE], F32),
                 w_gu=I(f"w_gu{i}", [n_exp, D, D], F32), w_down=I(f"w_down{i}", [n_exp, D // 2, D], F32), w_out=I(f"w_out{i}", [D, D], F32))
        if kind == "diff":
            w.update(w_in=I(f"w_in{i}", [D, 6144], F32), lamT=I(f"lamT{i}", [128, 4], F32), sgT=I(f"sgT{i}", [128, 2], F32))
        else:
            w.update(w_in=I(f"w_in{i}", [D, 1088], F32), qgT=I(f"qgT{i}", [128, 4], F32), kvgT=I(f"kvgT{i}", [128, 4], F32),
                     w_uq=I(f"w_uq{i}", [512, 3072], F32), w_ukv=I(f"w_ukv{i}", [512, 4096], F32))
        L.append(w)
    gfinT = I("gfinT", [128, 16], F32)
    with ExitStack() as es:
        B = Bld(nc, es)
        B.consts(g)
        for i in range(depth):
            kind = "diff" if i % 2 == 0 else "mla"
            lam_init = 0.8 - 0.6 * math.exp(-0.3 * i)
            w = L[i]
            final = i == depth - 1
            for half in range(2):
                dr = dict(g)
                dr.update(w)
                dr.update(scr[kind][half])
                dr.update(hT_in=(xT[half] if i == 0 else hbuf[half]), pos=pos[half], ccol=ccol)
                if half == 1:
                    dr["ada_w"] = None
                if kind == "diff":
                    B.phaseA_diff(dr)
                else:
                    B.phaseA_mla(dr)
                B.P.barrier()
            for half in range(2):
                dr = dict(g)
                dr.update(w)
                dr.update(scr[kind][half])
                dr.update(hT_in=(xT[half] if i == 0 else hbuf[half]), hT_out=hbuf[half], gfinT=gfinT, outT=outT[half])
                if half == 1:
                    for k, v in scr[kind][0].items():
                        if k[0] in "kV":
                            dr[k + "_prev"] = v
                B.phaseB(dr, kind, "static" if half == 1 else "none", lam_init, final, n_exp)
                B.P.barrier()
        _finish(nc, es, B)
    return nc


def fused_inputs(inp, n_exp=NE, depth=4):
    hc = host_consts()
    f = lambda a: np.asarray(a, np.float32)
    shared = dict(hc)
    for i in range(depth):
        kind = "diff" if i % 2 == 0 else "mla"
        j = i // 2
        shared.update({
            f"adabT{i}": colT(inp["ada_b"][i], 96), f"ada_w{i}": f(inp["ada_w"][i]), f"gmixT{i}": colT(inp["mix_norm_g"][i], 16),
            f"gffnT{i}": colT(inp["ffn_norm_g"][i], 16),
            f"bguT{i}": np.ascontiguousarray(f(inp["moe_b_gate_up"][i]).reshape(NE, 16, 128).transpose(2, 0, 1).reshape(128, NE * 16)),
            f"bd{i}": f(inp["moe_b_down"][i]),
            f"wrT{i}": np.ascontiguousarray(f(inp["moe_w_router"][i]).reshape(KC, 128, NE).transpose(1, 0, 2).reshape(128, KC * NE)),
            f"brr{i}": np.ascontiguousarray(np.broadcast_to(f(inp["moe_b_router"][i])[None, :], (128, NE))),
            f"w_gu{i}": f(inp["moe_w_gate_up"][i])[:n_exp], f"w_down{i}": f(inp["moe_w_down"][i])[:n_exp],
        })
        if kind == "diff":
            shared.update({f"w_in{i}": f(inp["diff_w_in"][j]), f"lamT{i}": np.ascontiguousarray(f(inp["diff_lambda"][j]).T),
                           f"sgT{i}": colT(inp["diff_subln_g"][j], 2), f"w_out{i}": f(inp["diff_w_out"][j])})
        else:
            shared.update({f"w_in{i}": f(inp["mla_w_in"][j]), f"qgT{i}": colT(inp["mla_q_norm_g"][j], 4), f"kvgT{i}": colT(inp["mla_kv_norm_g"][j], 4),
                           f"w_uq{i}": f(inp["mla_w_uq"][j]), f"w_ukv{i}": f(inp["mla_w_ukv"][j]), f"w_out{i}": f(inp["mla_w_out"][j])})
    shared["gfinT"] = colT(inp["final_norm_g"], 16)
    x = f(inp["x"])
    maps = []
    for b in range(4):
        m = dict(shared)
        m["xT"] = np.ascontiguousarray(x[b].reshape(2, T, D).transpose(0, 2, 1))
        m["pos"] = np.ascontiguousarray(np.broadcast_to(np.asarray(inp["positions"])[b].reshape(2, 1, T), (2, 64, T))).astype(np.int32)
        m["ccol"] = colT(np.asarray(inp["c"])[b], 16)
        maps.append(m)
    return maps


def kernel_fused(**inp):
    nc = _prog(("fused",), lambda: build_fused())
    maps = fused_inputs(inp)
    cores = list(range(8))
    res = run_bass_kernel_spmd(nc, [maps[c // 2] for c in cores], core_ids=cores).results
    res = [res[2 * b] for b in range(4)]
    out = np.empty((4, 2 * T, D), np.float32)
    for b in range(4):
        o = np.asarray(res[b]["outT"])
        out[b] = o.transpose(0, 2, 1).reshape(2 * T, D)
    return out


def colT(v, n):
    return np.ascontiguousarray(np.asarray(v, np.float32).reshape(n, 128).T)


def host_consts():
    rot = np.zeros((64, 96), np.float32)
    for i in range(16):
        rot[i + 16, i] = -1.0
        rot[i, i + 16] = 1.0
    for i in range(32):
        rot[i + 32, 32 + i] = -1.0
        rot[i, 32 + i + 32] = 1.0
    invf = np.zeros((64, 2), np.float32)
    f32 = (500000.0 ** (-np.arange(0, 32, 2, dtype=np.float32) / 32)).astype(np.float32)
    f64 = (500000.0 ** (-np.arange(0, 64, 2, dtype=np.float32) / 64)).astype(np.float32)
    for p in range(32):
        invf[p, 0] = f32[p % 16]
    for p in range(64):
        invf[p, 1] = f64[p % 32]
    return dict(rotT=rot.astype(ml_dtypes.bfloat16), invf=invf, ident=np.eye(128, dtype=np.float32),
                pidx=np.ascontiguousarray(np.broadcast_to(np.arange(32, dtype=np.float32)[:, None], (32, 128))))


_PROGS = {}


def _prog(key, fn):
    if key not in _PROGS:
        _PROGS[key] = fn()
    return _PROGS[key]


def kernel_unfused(x, c, positions, ada_w, ada_b, mix_norm_g, ffn_norm_g, final_norm_g,
           diff_w_in, diff_lambda, diff_subln_g, diff_w_out,
           mla_w_in, mla_q_norm_g, mla_kv_norm_g, mla_w_uq, mla_w_ukv, mla_w_out,
           moe_w_router, moe_b_router, moe_w_gate_up, moe_b_gate_up, moe_w_down, moe_b_down):
    x = np.asarray(x, np.float32)
    NC = 8
    cores = list(range(NC))
    hc = host_consts()
    hT = [np.ascontiguousarray(x[cid // 2, (cid % 2) * T:(cid % 2 + 1) * T, :].T) for cid in cores]
    ccol = [colT(np.asarray(c)[cid // 2], 16) for cid in cores]
    pos = [np.ascontiguousarray(np.broadcast_to(np.asarray(positions)[cid // 2, (cid % 2) * T:(cid % 2 + 1) * T][None, :], (64, T))).astype(np.int32) for cid in cores]
    pbias = [np.full((128, 1), 0.0 if cid % 2 == 1 else -30000.0, np.float32) for cid in cores]
    out = None
    for i in range(4):
        kind = "diff" if i % 2 == 0 else "mla"
        j = i // 2
        lam_init = 0.8 - 0.6 * math.exp(-0.3 * i)
        shared = dict(hc)
        shared.update(adabT=colT(ada_b[i], 96), ada_w=np.asarray(ada_w[i], np.float32), gmixT=colT(mix_norm_g[i], 16))
        if kind == "diff":
            shared.update(w_in=np.asarray(diff_w_in[j], np.float32))
        else:
            shared.update(w_in=np.asarray(mla_w_in[j], np.float32), qgT=colT(mla_q_norm_g[j], 4), kvgT=colT(mla_kv_norm_g[j], 4),
                          w_uq=np.asarray(mla_w_uq[j], np.float32), w_ukv=np.asarray(mla_w_ukv[j], np.float32))
        ncA = _prog(("A", kind), lambda: build_A(kind))
        in_maps = [dict(shared, hT_in=hT[cid], ccol=ccol[cid], pos=pos[cid]) for cid in cores]
        rA = run_bass_kernel_spmd(ncA, in_maps, core_ids=cores).results
        final = i == 3
        sharedB = dict(hc)
        sharedB.update(w_out=np.asarray(diff_w_out[j] if kind == "diff" else mla_w_out[j], np.float32), gffnT=colT(ffn_norm_g[i], 16),
                       bguT=np.ascontiguousarray(np.asarray(moe_b_gate_up[i], np.float32).reshape(NE, 16, 128).transpose(2, 0, 1).reshape(128, NE * 16)),
                       bd=np.asarray(moe_b_down[i], np.float32),
                       wrT=np.ascontiguousarray(np.asarray(moe_w_router[i], np.float32).reshape(KC, 128, NE).transpose(1, 0, 2).reshape(128, KC * NE)),
                       brr=np.ascontiguousarray(np.broadcast_to(np.asarray(moe_b_router[i], np.float32)[None, :], (128, NE))),
                       w_gu=np.asarray(moe_w_gate_up[i], np.float32), w_down=np.asarray(moe_w_down[i], np.float32))
        if kind == "diff":
            sharedB.update(lamT=np.ascontiguousarray(np.asarray(diff_lambda[j], np.float32).T), sgT=colT(diff_subln_g[j], 2))
        if final:
            sharedB.update(gfinT=colT(final_norm_g, 16))
        ncB = _prog(("B", kind, i if kind == "diff" else 0, final), lambda: build_B(kind, lam_init, final))
        in_maps = []
        for cid in cores:
            m = dict(sharedB, hT_in=hT[cid], modT_in=rA[cid]["modT_out"], pbias=pbias[cid])
            partner = cid - 1 if cid % 2 == 1 else cid
            if kind == "diff":
                m.update(qT=rA[cid]["qT"], kT=rA[cid]["kT"], V=rA[cid]["V"], kT_prev=rA[partner]["kT"], V_prev=rA[partner]["V"])
            else:
                m.update(qnT=rA[cid]["qnT"], qrT=rA[cid]["qrT"], knT=rA[cid]["knT"], krT=rA[cid]["krT"], V=rA[cid]["V"],
                         knT_prev=rA[partner]["knT"], krT_prev=rA[partner]["krT"], V_prev=rA[partner]["V"])
            in_maps.append(m)
        rB = run_bass_kernel_spmd(ncB, in_maps, core_ids=cores).results
        if final:
            out = np.empty((4, 2048, D), np.float32)
            for cid in cores:
                out[cid // 2, (cid % 2) * T:(cid % 2 + 1) * T, :] = np.asarray(rB[cid]["outT"]).T
        else:
            hT = [np.asarray(rB[cid]["hT_out"]) for cid in cores]
    return out


def kernel(**inputs):
    return kernel_unfused(**inputs)
```
